# Optimizing a Trainium2 kernel written in Bass

```python
import math
import jax, jax.numpy as jnp
from jax import lax
import numpy as np

D_MODEL = 1024
BATCH = 8
SEQ = 4096
DEPTH = 2

GRID_W = 64
CTX_LEN = 256
NORM_EPS = 1e-6
N_HEADS = 8
N_KV_HEADS = 2
HEAD_DIM = 128
ATT_WIDTH = N_HEADS * HEAD_DIM
KV_WIDTH = N_KV_HEADS * HEAD_DIM
ROPE_THETA = 10000.0
Q_BLOCK = 128
ATT_SCALE = HEAD_DIM ** -0.5
HY_WIDTH = D_MODEL // 2
HY_ORDER = 2
HY_SHORT = 3
HY_FILTER_HIDDEN = 64
HY_BANDS = 16
HY_PE_DIM = 1 + 2 * HY_BANDS
HY_FAST_DECAY = 0.3
HY_SLOW_DECAY = 1.5
HY_DECAY_TARGET = 1e-2
N_EXPERTS = 64
N_GROUPS = 8
EXPERTS_PER_GROUP = N_EXPERTS // N_GROUPS
TOP_K = 2
EXPERT_DIM = 512
MOE_BLOCK = 256
IN_WIDTH = ATT_WIDTH + 2 * KV_WIDTH + 3 * HY_WIDTH + 2 * D_MODEL
SPLITS = (ATT_WIDTH, ATT_WIDTH + KV_WIDTH, ATT_WIDTH + 2 * KV_WIDTH,
          ATT_WIDTH + 2 * KV_WIDTH + 3 * HY_WIDTH)

kernel_name = 'hybrid_attn_hyena_moe_diffusion_trunk'


def rms_norm(x, w):
    xf = x.astype(jnp.float32)
    y = xf * lax.rsqrt(jnp.mean(xf * xf, axis=-1, keepdims=True) + NORM_EPS)
    return (y * w.astype(jnp.float32)).astype(x.dtype)


def modulate(h, shift, scale):
    return h * (1 + scale) + shift


def head_norm(t, w, n_heads):
    B, L = t.shape[:2]
    return rms_norm(t.reshape(B, L, n_heads, HEAD_DIM), w)


def axial_rope(L):
    rows = L // GRID_W
    row = jnp.broadcast_to(jnp.arange(rows)[:, None], (rows, GRID_W)).reshape(-1).astype(jnp.float32)
    col = jnp.broadcast_to(jnp.arange(GRID_W)[None, :], (rows, GRID_W)).reshape(-1).astype(jnp.float32)
    n = HEAD_DIM // 4
    inv = ROPE_THETA ** (-jnp.arange(n, dtype=jnp.float32) / n)
    ang = jnp.concatenate([row[:, None] * inv, col[:, None] * inv], axis=-1)
    return jnp.cos(ang), jnp.sin(ang)


def apply_rope(x, cos, sin):
    x1, x2 = jnp.split(x, 2, axis=-1)
    c = cos[None, :, None, :].astype(x.dtype)
    s = sin[None, :, None, :].astype(x.dtype)
    return jnp.concatenate([x1 * c - x2 * s, x1 * s + x2 * c], axis=-1)


def gqa_attention(q, k, v, block):
    B, L = q.shape[:2]
    G = N_HEADS // N_KV_HEADS
    nb = L // block
    qb = jnp.moveaxis(q.reshape(B, nb, block, N_KV_HEADS, G, HEAD_DIM), 1, 0)

    def attend(qblk):
        s = jnp.einsum('bqhgd,bkhd->bhgqk', qblk, k).astype(jnp.float32) * ATT_SCALE
        p = jax.nn.softmax(s, axis=-1).astype(v.dtype)
        return jnp.einsum('bhgqk,bkhd->bqhgd', p, v)

    o = lax.map(attend, qb)
    return jnp.moveaxis(o, 0, 1).reshape(B, L, ATT_WIDTH)


def hyena_filters(L, pe_w1, pe_b1, freq1, pe_w2, pe_b2, freq2, pe_w3):
    t01 = jnp.linspace(0.0, 1.0, L, dtype=jnp.float32)[:, None]
    pos = jnp.arange(L, dtype=jnp.float32)[:, None]
    bands = jnp.linspace(1e-4, HY_BANDS - 1, HY_BANDS, dtype=jnp.float32)[None, :]
    f = 2.0 * math.pi * pos * bands / L
    z = jnp.concatenate([t01, jnp.cos(f), -jnp.sin(f)], axis=-1).astype(pe_w1.dtype)
    h = jnp.sin(freq1 * (z @ pe_w1 + pe_b1))
    h = jnp.sin(freq2 * (h @ pe_w2 + pe_b2))
    h = h @ pe_w3
    max_decay = math.log(HY_DECAY_TARGET) / HY_FAST_DECAY
    min_decay = math.log(HY_DECAY_TARGET) / HY_SLOW_DECAY
    deltas = jnp.abs(jnp.linspace(min_decay, max_decay, HY_WIDTH, dtype=jnp.float32))
    window = jnp.exp(-t01 * deltas[None, :]).astype(h.dtype)
    return h.reshape(L, HY_ORDER, 2, HY_WIDTH) * window[:, None, None, :]


def bidir_long_conv(u, h_fwd, h_bwd):
    L, C = h_fwd.shape
    k = jnp.concatenate([h_fwd, jnp.zeros((1, C), h_fwd.dtype), h_bwd[:0:-1]], axis=0).astype(jnp.float32)
    U = jnp.fft.rfft(u.astype(jnp.float32), n=2 * L, axis=1)
    K = jnp.fft.rfft(k, n=2 * L, axis=0)
    y = jnp.fft.irfft(U * K[None], n=2 * L, axis=1)[:, :L]
    return y.astype(u.dtype)


def short_conv(u, w, b):
    up = jnp.pad(u, ((0, 0), (1, 1), (0, 0)))
    return up[:, :-2] * w[0] + up[:, 1:-1] * w[1] + up[:, 2:] * w[2] + b


def hyena_operator(u_proj, filt, conv_w, conv_b, skip):
    u = short_conv(u_proj, conv_w, conv_b)
    x1, x2, v = jnp.split(u, 3, axis=-1)
    z = x1 * (bidir_long_conv(v, filt[:, 0, 0], filt[:, 0, 1]) + skip[0] * v)
    return x2 * (bidir_long_conv(z, filt[:, 1, 0], filt[:, 1, 1]) + skip[1] * z)


def merge_branches(y_att, y_hy, g, w_att_proj, w_hy_proj, w_out):
    g_att, g_hy = jnp.split(jax.nn.sigmoid(g), 2, axis=-1)
    return (g_att * (y_att @ w_att_proj) + g_hy * (y_hy @ w_hy_proj)) @ w_out


def route(h, router_w, router_b):
    T = h.shape[0]
    scores = jax.nn.sigmoid((h @ router_w).astype(jnp.float32))
    biased = (scores + router_b.astype(jnp.float32)).reshape(T, N_GROUPS, EXPERTS_PER_GROUP)
    grp_score = jnp.sum(lax.top_k(biased, 2)[0], axis=-1)
    g_sel = jnp.argmax(grp_score, axis=-1).astype(jnp.int32)
    in_grp = jnp.take_along_axis(biased, g_sel[:, None, None], axis=1)[:, 0]
    _, local = lax.top_k(in_grp, TOP_K)
    idx = g_sel[:, None] * EXPERTS_PER_GROUP + local.astype(jnp.int32)
    w = jnp.take_along_axis(scores, idx, axis=1)
    return idx, w / jnp.sum(w, axis=-1, keepdims=True)


def moe_ffn(h, router_w, router_b, w1, w3, w2):
    T, D = h.shape
    idx, gate = route(h, router_w, router_b)
    A = T * TOP_K
    flat_e = idx.reshape(-1)
    flat_tok = jnp.repeat(jnp.arange(T, dtype=jnp.int32), TOP_K)
    flat_gate = gate.reshape(-1)
    order = jnp.argsort(flat_e)
    e_sorted = flat_e[order]
    counts = jnp.bincount(flat_e, length=N_EXPERTS)
    padded = (counts + MOE_BLOCK - 1) // MOE_BLOCK * MOE_BLOCK
    pad_end = jnp.cumsum(padded)
    pad_start = pad_end - padded
    start = jnp.cumsum(counts) - counts
    dest = pad_start[e_sorted] + (jnp.arange(A, dtype=jnp.int32) - start[e_sorted])
    n_blocks = -(-A // MOE_BLOCK) + N_EXPERTS
    n_slots = n_blocks * MOE_BLOCK
    slot_tok = jnp.full((n_slots,), T, jnp.int32).at[dest].set(flat_tok[order])
    slot_gate = jnp.zeros((n_slots,), jnp.float32).at[dest].set(flat_gate[order])
    block_e = jnp.minimum(jnp.searchsorted(pad_end, jnp.arange(n_blocks) * MOE_BLOCK, side='right'),
                          N_EXPERTS - 1)
    h_pad = jnp.concatenate([h, jnp.zeros((1, D), h.dtype)], axis=0)
    xb = h_pad[slot_tok].reshape(n_blocks, MOE_BLOCK, D)

    def expert_block(args):
        xblk, e = args
        return (jax.nn.silu(xblk @ w1[e]) * (xblk @ w3[e])) @ w2[e]

    yb = lax.map(expert_block, (xb, block_e))
    y = yb.reshape(n_slots, D) * slot_gate[:, None].astype(h.dtype)
    return jnp.zeros((T + 1, D), h.dtype).at[slot_tok].add(y)[:T]


def setup_inputs(seed: int = 0) -> dict:
    key = jax.random.key(seed)
    ks = iter(jax.random.split(key, 32))

    def nrm(shape, scale):
        return jax.random.normal(next(ks), shape, jnp.float32) * scale

    D = D_MODEL
    H = HY_FILTER_HIDDEN
    return {
        'x': nrm((BATCH, SEQ, D), 1.0),
        'c': nrm((BATCH, D), 1.0),
        'ctx': nrm((BATCH, CTX_LEN, D), 1.0),
        'c_ctx': nrm((D,), 1.0),
        'w_ada': nrm((DEPTH, D, 6 * D), D ** -0.5),
        'b_ada': nrm((DEPTH, 6 * D), 0.02),
        'norm1_w': 1.0 + nrm((DEPTH, D), 0.05),
        'norm2_w': 1.0 + nrm((DEPTH, D), 0.05),
        'w_in': nrm((DEPTH, D, IN_WIDTH), D ** -0.5),
        'q_norm_w': 1.0 + nrm((DEPTH, HEAD_DIM), 0.05),
        'k_norm_w': 1.0 + nrm((DEPTH, HEAD_DIM), 0.05),
        'hy_conv_w': nrm((DEPTH, HY_SHORT, 3 * HY_WIDTH), HY_SHORT ** -0.5),
        'hy_conv_b': nrm((DEPTH, 3 * HY_WIDTH), 0.02),
        'hy_pe_w1': nrm((DEPTH, HY_PE_DIM, H), HY_PE_DIM ** -0.5),
        'hy_pe_b1': nrm((DEPTH, H), 0.1),
        'hy_freq1': 1.0 + nrm((DEPTH, H), 0.1),
        'hy_pe_w2': nrm((DEPTH, H, H), H ** -0.5),
        'hy_pe_b2': nrm((DEPTH, H), 0.1),
        'hy_freq2': 1.0 + nrm((DEPTH, H), 0.1),
        'hy_pe_w3': nrm((DEPTH, H, HY_ORDER * 2 * HY_WIDTH), 0.01),
        'hy_skip': nrm((DEPTH, HY_ORDER, HY_WIDTH), 0.5),
        'w_att_proj': nrm((DEPTH, ATT_WIDTH, D), ATT_WIDTH ** -0.5),
        'w_hy_proj': nrm((DEPTH, HY_WIDTH, D), HY_WIDTH ** -0.5),
        'w_out': nrm((DEPTH, D, D), D ** -0.5),
        'router_w': nrm((D, N_EXPERTS), D ** -0.5),
        'router_b': nrm((N_EXPERTS,), 0.01),
        'exp_w1': nrm((DEPTH, N_EXPERTS, D, EXPERT_DIM), D ** -0.5),
        'exp_w3': nrm((DEPTH, N_EXPERTS, D, EXPERT_DIM), D ** -0.5),
        'exp_w2': nrm((DEPTH, N_EXPERTS, EXPERT_DIM, D), EXPERT_DIM ** -0.5),
        'final_norm_w': 1.0 + nrm((D,), 0.05),
    }


def reference(x, c, ctx, c_ctx, w_ada, b_ada, norm1_w, norm2_w, w_in, q_norm_w, k_norm_w,
              hy_conv_w, hy_conv_b, hy_pe_w1, hy_pe_b1, hy_freq1, hy_pe_w2, hy_pe_b2, hy_freq2,
              hy_pe_w3, hy_skip, w_att_proj, w_hy_proj, w_out, router_w, router_b,
              exp_w1, exp_w3, exp_w2, final_norm_w):
    B, S, D = x.shape
    C = ctx.shape[1]
    cos, sin = axial_rope(S)
    for i in range(DEPTH):
        last = i == DEPTH - 1
        mods = jnp.split(jax.nn.silu(c) @ w_ada[i] + b_ada[i], 6, axis=-1)
        sh1, sc1, g1, sh2, sc2, g2 = [m[:, None, :] for m in mods]
        csh1, csc1, cg1, csh2, csc2, cg2 = jnp.split(jax.nn.silu(c_ctx) @ w_ada[i] + b_ada[i], 6, axis=-1)

        hx = modulate(rms_norm(x, norm1_w[i]), sh1, sc1)
        hc = modulate(rms_norm(ctx, norm1_w[i]), csh1, csc1)
        qx, kx, vx, ux, gx = jnp.split(hx @ w_in[i], SPLITS, axis=-1)
        qx = apply_rope(head_norm(qx, q_norm_w[i], N_HEADS), cos, sin)
        kx = apply_rope(head_norm(kx, k_norm_w[i], N_KV_HEADS), cos, sin)
        vx = vx.reshape(B, S, N_KV_HEADS, HEAD_DIM)
        if last:
            kc, vc = jnp.split(hc @ w_in[i][:, ATT_WIDTH:ATT_WIDTH + 2 * KV_WIDTH], 2, axis=-1)
        else:
            qc, kc, vc, uc, gc = jnp.split(hc @ w_in[i], SPLITS, axis=-1)
        kc = head_norm(kc, k_norm_w[i], N_KV_HEADS)
        vc = vc.reshape(B, C, N_KV_HEADS, HEAD_DIM)

        k_all = jnp.concatenate([kx, kc], axis=1)
        v_all = jnp.concatenate([vx, vc], axis=1)
        ya_x = gqa_attention(qx, k_all, v_all, Q_BLOCK)
        filt_x = hyena_filters(S, hy_pe_w1[i], hy_pe_b1[i], hy_freq1[i], hy_pe_w2[i], hy_pe_b2[i],
                               hy_freq2[i], hy_pe_w3[i])
        yh_x = hyena_operator(ux, filt_x, hy_conv_w[i], hy_conv_b[i], hy_skip[i])
        x = x + g1 * merge_branches(ya_x, yh_x, gx, w_att_proj[i], w_hy_proj[i], w_out[i])
        hx2 = modulate(rms_norm(x, norm2_w[i]), sh2, sc2)

        if last:
            f = moe_ffn(hx2.reshape(B * S, D), router_w, router_b, exp_w1[i], exp_w3[i], exp_w2[i])
            x = x + g2 * f.reshape(B, S, D)
        else:
            qc = head_norm(qc, q_norm_w[i], N_HEADS)
            ya_c = gqa_attention(qc, kc, vc, C)
            filt_c = hyena_filters(C, hy_pe_w1[i], hy_pe_b1[i], hy_freq1[i], hy_pe_w2[i], hy_pe_b2[i],
                                   hy_freq2[i], hy_pe_w3[i])
            yh_c = hyena_operator(uc, filt_c, hy_conv_w[i], hy_conv_b[i], hy_skip[i])
            ctx = ctx + cg1 * merge_branches(ya_c, yh_c, gc, w_att_proj[i], w_hy_proj[i], w_out[i])
            hc2 = modulate(rms_norm(ctx, norm2_w[i]), csh2, csc2)
            tokens = jnp.concatenate([hx2.reshape(B * S, D), hc2.reshape(B * C, D)], axis=0)
            f = moe_ffn(tokens, router_w, router_b, exp_w1[i], exp_w3[i], exp_w2[i])
            x = x + g2 * f[:B * S].reshape(B, S, D)
            ctx = ctx + cg2 * f[B * S:].reshape(B, C, D)
    return rms_norm(x, final_norm_w)
```

```python
import math
import numpy as np
import concourse.bass as bass
import concourse.mybir as mybir
from concourse.bass_utils import run_bass_kernel_spmd
from contextlib import ExitStack

F32 = mybir.dt.float32
BF16 = mybir.dt.bfloat16
AF = mybir.ActivationFunctionType
ALU = mybir.AluOpType
AX = mybir.AxisListType

S = 4096
C = 256
T = S + C
NT = T // 128
D = 1024
NE = 64
EPS = 1e-6
NFFT = 8192
CW = 256
PI = float(np.pi)


class _Stop(Exception):
    pass


class Prog:
    EPOCH = 30000
    NDMA = 24

    def __init__(self, nc, es):
        self.nc = nc
        self.es = es
        self.eng = {"pe": nc.tensor, "act": nc.scalar, "dve": nc.vector, "pool": nc.gpsimd, "sp": nc.sync}
        self.sems = {e: [es.enter_context(nc.semaphore(f"s_{e}_0"))] for e in self.eng}
        self.cnt = {e: 0 for e in self.eng}
        self.ep = {e: 0 for e in self.eng}
        self.dsem = [es.enter_context(nc.semaphore(f"s_dma_{i}")) for i in range(self.NDMA)]
        self.dcnt = [0] * self.NDMA
        self.dnext = 0
        self.waited = {e: {} for e in self.eng}
        self.W = {}
        self.R = {}
        self.nops = 0
        self.dead = False

    def _wait(self, e, tok):
        sem, val, src = tok
        if src == e and e == "pe":
            return
        w = self.waited[e]
        k = id(sem)
        if w.get(k, 0) >= val:
            return
        self.eng[e].wait_ge(sem, val)
        w[k] = val

    def _deps(self, reads, writes):
        toks = []
        for k in reads:
            toks.extend(self.W.get(k, {}).values())
        for k in writes:
            toks.extend(self.W.get(k, {}).values())
            toks.extend(self.R.get(k, {}).values())
        return toks

    def _commit(self, tok, reads, writes):
        sid = id(tok[0])
        for k in reads:
            d = self.R.setdefault(k, {})
            if sid not in d or d[sid][1] < tok[1]:
                d[sid] = tok
        for k in writes:
            d = self.W.setdefault(k, {})
            if sid not in d or d[sid][1] < tok[1]:
                d[sid] = tok

    def op(self, e, fn, reads=(), writes=()):
        if self.dead:
            return None
        for t in self._deps(reads, writes):
            self._wait(e, t)
        if self.cnt[e] >= self.EPOCH:
            self.ep[e] += 1
            self.sems[e].append(self.es.enter_context(self.nc.semaphore(f"s_{e}_{self.ep[e]}")))
            self.cnt[e] = 0
        inst = fn(self.eng[e])
        self.cnt[e] += 1
        sem = self.sems[e][-1]
        inst.then_inc(sem, 1)
        tok = (sem, self.cnt[e], e)
        self._commit(tok, reads, writes)
        self.nops += 1
        return tok

    def dma(self, q, fn, reads=(), writes=()):
        if self.dead:
            return None
        for t in self._deps(reads, writes):
            self._wait(q, t)
        i = self.dnext
        self.dnext = (self.dnext + 1) % self.NDMA
        sem = self.dsem[i]
        if self.dcnt[i] > 0:
            self._wait(q, (sem, 16 * self.dcnt[i], None))
        inst = fn(self.eng[q])
        self.dcnt[i] += 1
        inst.then_inc(sem, 16)
        tok = (sem, 16 * self.dcnt[i], None)
        self._commit(tok, reads, writes)
        self.nops += 1
        return tok

    def barrier(self):
        if self.dead:
            return
        toks = []
        for e in self.eng:
            if self.cnt[e] > 0:
                toks.append((self.sems[e][-1], self.cnt[e], e))
        for i in range(self.NDMA):
            if self.dcnt[i] > 0:
                toks.append((self.dsem[i], 16 * self.dcnt[i], None))
        for e in self.eng:
            for t in toks:
                if t[2] != e:
                    self._wait(e, t)
        self.W = {}
        self.R = {}


def _consts():
    c = {}
    c["ident"] = np.eye(128, dtype=np.float32)
    t = np.arange(S)
    row = (t // 64).astype(np.float32)
    col = (t % 64).astype(np.float32)
    n = 32
    inv = (10000.0 ** (-np.arange(n, dtype=np.float32) / n)).astype(np.float32)
    ang = np.concatenate([row[:, None] * inv, col[:, None] * inv], -1).astype(np.float32)
    cs = np.ones((T, 64), np.float32)
    sn = np.zeros((T, 64), np.float32)
    cs[:S] = np.cos(ang)
    sn[:S] = np.sin(ang)
    c["ropec"] = np.ascontiguousarray(cs.reshape(NT, 128, 64).transpose(1, 0, 2))
    c["ropes"] = np.ascontiguousarray(sn.reshape(NT, 128, 64).transpose(1, 0, 2))
    p = np.arange(128, dtype=np.float64)[:, None, None]
    j = np.arange(64, dtype=np.float64)[None, :, None]
    f1 = np.arange(128, dtype=np.float64)[None, None, :]
    M = np.exp(-2j * np.pi * (p * f1 / 128.0 + j * f1 / NFFT))
    Mri = np.stack([M.real, M.imag], 2)
    c["mlo"] = np.ascontiguousarray(Mri[:64]).astype(np.float32)
    c["mhi"] = np.ascontiguousarray(Mri[64:]).astype(np.float32)
    Mi = np.stack([M.real[:64], M.imag[:64]], 0) / NFFT
    c["minvT"] = np.ascontiguousarray(Mi.transpose(3, 2, 0, 1)).astype(np.float32)
    jj = np.arange(64, dtype=np.float64)[:, None]
    f2 = np.arange(64, dtype=np.float64)[None, :]
    Wre = np.cos(2 * np.pi * jj * f2 / 64.0)
    Wim = -np.sin(2 * np.pi * jj * f2 / 64.0)
    cat = lambda a, b: np.concatenate([a, b], 1)
    st = [cat(Wre, Wim), cat(-Wim, Wre), cat(Wim, Wre), cat(Wre, -Wim),
          cat(Wre, Wre), cat(-Wim, -Wim), cat(-Wim, Wim), cat(-Wre, Wre)]
    c["w64"] = np.ascontiguousarray(np.stack(st, 1)).astype(np.float32)
    wi = np.zeros((128, 128))
    wi[:64, :64] = Wre.T
    wi[:64, 64:] = -Wim.T
    wi[64:, :64] = Wim.T
    wi[64:, 64:] = Wre.T
    c["wi1"] = wi.astype(np.float32)
    deltas = np.abs(np.linspace(math.log(1e-2) / 1.5, math.log(1e-2) / 0.3, 512, dtype=np.float32))
    c["delta"] = np.ascontiguousarray(np.broadcast_to(deltas[None, :], (64, 512))).astype(np.float32)

    def zfeat(pos, L):
        t01 = (np.linspace(0.0, 1.0, L, dtype=np.float32))[pos][:, None]
        posf = pos.astype(np.float32)[:, None]
        bands = np.linspace(1e-4, 15, 16, dtype=np.float32)[None, :]
        f = (2.0 * math.pi * posf * bands / L).astype(np.float32)
        return np.concatenate([t01, np.cos(f), -np.sin(f)], -1).astype(np.float32), t01[:, 0]

    for name, L in (("lat", S), ("ctx", C)):
        q = np.arange(4096)
        vf = q < L
        posf = np.where(vf, q, 0)
        zf, t01f = zfeat(posf, L)
        d = 4096 - q
        vb = (d >= 1) & (d < L)
        posb = np.where(vb, d, 0)
        zb, t01b = zfeat(posb, L)
        c[f"z_{name}"] = np.ascontiguousarray(np.stack([zf.T, zb.T], 0))
        c[f"nt_{name}"] = np.ascontiguousarray(np.stack([-t01f.reshape(64, 64), -t01b.reshape(64, 64)], 0))
        c[f"vm_{name}"] = np.ascontiguousarray(np.stack([vf.reshape(64, 64), vb.reshape(64, 64)], 0).astype(np.float32))
    return c


_CONST_SHAPES = None


def build(debug=(), nlayers=2, do_moe=True, do_hyena=True, do_attn=True, stop=99):
    nc = bass.Bass("TRN2", target_bir_lowering=False)
    consts = _consts()
    _u = [0]

    def uniq(n):
        _u[0] += 1
        return f"t{_u[0]}_{n}"

    def din(name, shape, dt=F32):
        return nc.dram_tensor(name, list(shape), dt, kind="ExternalInput").ap()

    def dscr(name, shape, dt):
        kind = "ExternalOutput" if name in debug else "Internal"
        return nc.dram_tensor(name, list(shape), dt, kind=kind).ap()

    I = {}
    I["x"] = din("x", [S, D]); I["ctx"] = din("ctx", [C, D]); I["cc"] = din("cc", [128, 8, 2])
    I["w_ada"] = din("w_ada", [2, D, 6 * D]); I["b_adaT"] = din("b_adaT", [2, 128, 48])
    I["n1T"] = din("n1T", [2, 128, 8]); I["n2T"] = din("n2T", [2, 128, 8])
    I["w_in"] = din("w_in", [2, D, 5120])
    I["qkw"] = din("qkw", [2, 128, 1280])
    I["convw"] = din("convw", [2, 128, 3 * 1536]); I["convb"] = din("convb", [2, 128, 1536])
    I["pe_w1"] = din("pe_w1", [2, 33, 64]); I["pe_w2"] = din("pe_w2", [2, 64, 64]); I["pe_w3"] = din("pe_w3", [2, 64, 2048])
    I["pe_v"] = din("pe_v", [2, 64, 4])
    I["skipb"] = din("skipb", [2, 64, 1024])
    I["w_ap"] = din("w_ap", [2, D, D]); I["w_hp"] = din("w_hp", [2, 512, D]); I["w_out"] = din("w_out", [2, D, D])
    I["router_w"] = din("router_w", [D, NE]); I["router_bb"] = din("router_bb", [128, NE])
    if do_moe:
        I["exp_w1"] = din("exp_w1", [2, NE, D, 512]); I["exp_w3"] = din("exp_w3", [2, NE, D, 512]); I["exp_w2"] = din("exp_w2", [2, NE, 512, D])
    I["fnw"] = din("fnw", [128, D])
    for k, v in consts.items():
        I[k] = din("c_" + k, v.shape)
    OUT = nc.dram_tensor("out", [S, D], F32, kind="ExternalOutput").ap()

    X0 = dscr("X0", [T, D], F32); X1 = dscr("X1", [T, D], F32)
    QT_d = dscr("QT_d", [8, 128, T], BF16); KT_d = dscr("KT_d", [2, 128, T], BF16)
    V_d = dscr("V_d", [T, 256], BF16); U_d = dscr("U_d", [T, 1536], F32)
    G_d = dscr("G_d", [2048, T], BF16); YA_d = dscr("YA_d", [D, T], BF16)
    XV_d = dscr("XV_d", [T, 1536], F32)
    A_d = dscr("A_d", [128, 64, 2, 512], BF16); C_d = dscr("C_d", [128, 128, CW], BF16)
    KH_d = dscr("KH_d", [2, 2, 128, 2, 128, 512], F32)
    YH_d = dscr("YH_d", [T, 512], BF16)
    H2T_d = dscr("H2T_d", [D, T], BF16); GATE_d = dscr("GATE_d", [T, NE], F32)
    HT_d = dscr("HT_d", [D, T], BF16)

    with ExitStack() as es0:
        P = Prog(nc, es0)
        V_ = lambda fn, r=(), w=(): P.op("dve", fn, r, w)
        A_ = lambda fn, r=(), w=(): P.op("act", fn, r, w)
        M_ = lambda fn, r=(), w=(): P.op("pe", fn, r, w)
        G_ = lambda fn, r=(), w=(): P.op("dve", fn, r, w)
        def LD(out, in_, r=(), w=()):
            q = "pool" if (str(out.space) == "DRAM" and str(in_.space) != "DRAM") else "sp"
            return P.dma(q, lambda e: e.dma_start(out=out, in_=in_), r, w)
        LDC = lambda out, in_, r=(), w=(): P.dma("pool", lambda e: e.dma_start(out=out, in_=in_), r, w)

        psf = [es0.enter_context(nc.psum_tensor(f"psf{i}", [128, 512], F32)) for i in range(6)]
        psb = [es0.enter_context(nc.psum_tensor(f"psb{i}", [128, 1024], BF16)) for i in range(2)]
        PF = [f"psf{i}" for i in range(6)]
        PB = [f"psb{i}" for i in range(2)]

        def sbp(name, shape, dt):
            return es0.enter_context(nc.sbuf_tensor(uniq(name), list(shape), dt))
        idf = sbp("idf", [128, 128], F32); idb = sbp("idb", [128, 128], BF16)
        onesf = sbp("onesf", [128, 128], F32); onesb = sbp("onesb", [128, 128], BF16)
        epsc = sbp("epsc", [128, 1], F32)
        mods = sbp("mods", [128, 48, 2], F32)
        scA = sbp("scA", [128, 8, 2], F32); scB = sbp("scB", [128, 8, 2], F32)
        gb = sbp("gb", [128, 4, D], F32)
        LD(idf[:], I["ident"][:, :], w=["idf"]); LDC(idb[:], I["ident"][:, :], w=["idb"])
        V_(lambda e: e.memset(onesf[:], 1.0), w=["onesf"]); V_(lambda e: e.memset(onesb[:], 1.0), w=["onesb"])
        V_(lambda e: e.memset(epsc[:], EPS), w=["epsc"])
        LD(X0[0:S, :], I["x"][:, :], w=["X0"]); LD(X0[S:T, :], I["ctx"][:, :], w=["X0"])
        P.barrier()

        def chk(x):
            if stop == x and not P.dead:
                P.barrier()
                P.dead = True

        for li in range(nlayers):
          try:
              last = li == 1
              if stop == 0:
                  break
              ntl = 32 if False else NT

              with ExitStack() as es:
                  sb = lambda n, s, d: es.enter_context(nc.sbuf_tensor(uniq(n), list(s), d))
                  ccs = sb("ccs", [128, 8, 2], F32)
                  wa = [sb(f"wa{i}", [128, 8, 512], F32) for i in range(2)]
                  bT = sb("bT", [128, 48], F32); n1 = sb("n1", [128, 8], F32); n2 = sb("n2", [128, 8], F32)
                  dg = sb("dg", [128, 128], F32); tmp = sb("tmpa", [128, 8, 2], F32)
                  LD(ccs[:], I["cc"][:, :, :], w=["ccs"])
                  A_(lambda e: e.activation(out=ccs[:], in_=ccs[:], func=AF.Silu), ["ccs"], ["ccs"])
                  LD(bT[:], I["b_adaT"][li], w=["bT"]); LD(n1[:], I["n1T"][li], w=["n1"]); LD(n2[:], I["n2T"][li], w=["n2"])
                  wsrc = I["w_ada"][li].rearrange("(kt k) n -> k kt n", k=128)
                  for g in range(12 if stop != 0.3 else 0):
                      w = wa[g % 2]
                      LD(w[:], wsrc[:, :, g * 512:(g + 1) * 512], w=[f"wa{g % 2}"])
                      for sub in range(4):
                          ch = g * 4 + sub
                          for kt in range(8):
                              M_(lambda e: e.matmul(psf[0][:, ch * 2:ch * 2 + 2], w[:, kt, sub * 128:(sub + 1) * 128], ccs[:, kt, :],
                                                    start=(kt == 0), stop=(kt == 7)), [f"wa{g % 2}", "ccs"], [PF[0]])
                  if stop in (0.3, 0.5):
                      break
                  V_(lambda e: e.tensor_tensor(mods[:], psf[0][:, 0:96].rearrange("p (c m) -> p c m", m=2),
                                               bT[:].unsqueeze(2).to_broadcast([128, 48, 2]), op=ALU.add), [PF[0], "bT"], ["mods"])
                  V_(lambda e: e.tensor_scalar(tmp[:], mods[:, 8:16, :], 1.0, None, op0=ALU.add), ["mods"], ["tmpa"])
                  V_(lambda e: e.tensor_tensor(scA[:], tmp[:], n1[:].unsqueeze(2).to_broadcast([128, 8, 2]), op=ALU.mult), ["tmpa", "n1"], ["scA"])
                  V_(lambda e: e.tensor_scalar(tmp[:], mods[:, 32:40, :], 1.0, None, op0=ALU.add), ["mods", "scA"], ["tmpa"])
                  V_(lambda e: e.tensor_tensor(scB[:], tmp[:], n2[:].unsqueeze(2).to_broadcast([128, 8, 2]), op=ALU.mult), ["tmpa", "n2"], ["scB"])
                  if stop == 0.7:
                      break
                  for gi, base in enumerate((16, 40)):
                      for m in range(2):
                          for dt in range(8):
                              V_(lambda e: e.tensor_scalar(dg[:], idf[:], mods[:, base + dt, m:m + 1], None, op0=ALU.mult), ["idf", "mods"], ["dg"])
                              M_(lambda e: e.matmul(psf[1][:, 0:128], onesf[:], dg[:], start=True, stop=True), ["onesf", "dg"], [PF[1]])
                              A_(lambda e: e.activation(out=gb[:, gi * 2 + m, dt * 128:(dt + 1) * 128], in_=psf[1][:, 0:128], func=AF.Identity), [PF[1]], ["gb"])
              P.barrier()

              if stop == 1:
                  break
              cur, mid = X0, X1
              with ExitStack() as es:
                  sb = lambda n, s, d: es.enter_context(nc.sbuf_tensor(uniq(n), list(s), d))
                  wq = sb("wq", [128, 8, 3072], BF16)
                  qkw = sb("qkw", [128, 10, 128], F32)
                  rc = sb("rc", [128, 2, 64], F32); rs = sb("rs", [128, 2, 64], F32)
                  xt = [sb(f"xt{i}", [128, D], F32) for i in range(2)]
                  xn = [sb(f"xn{i}", [128, D], BF16) for i in range(2)]
                  junk = sb("junk", [128, 1280], BF16)
                  st = [sb(f"st{i}", [128, 4], F32) for i in range(2)]
                  hT = [sb(f"hT{i}", [128, 8, 512], BF16) for i in range(2)]
                  qs = sb("qs", [128, 1280], F32); q2 = sb("q2", [128, 1280], F32)
                  hs = sb("hs", [128, 16], F32)
                  r1 = sb("r1", [128, 10, 64], F32); r2 = sb("r2", [128, 10, 64], F32)
                  qr = [sb(f"qr{i}", [128, 10, 128], BF16) for i in range(2)]
                  qT = [sb(f"qT{i}", [128, 10, 512], BF16) for i in range(2)]
                  vsb = [sb(f"vsb{i}", [128, 256], BF16) for i in range(2)]
                  usb = [sb(f"usb{i}", [128, 1536], F32) for i in range(2)]
                  wsrc = I["w_in"][li].rearrange("(kt k) n -> k kt n", k=128)
                  for cgi in range(3):
                      LDC(wq[:, :, cgi * 1024:(cgi + 1) * 1024], wsrc[:, :, cgi * 1024:(cgi + 1) * 1024], w=["wq"])
                  LD(qkw[:].rearrange("p a b -> p (a b)"), I["qkw"][li], w=["qkw"])
                  nblk = (T + 511) // 512
                  for blk in range(nblk):
                      tts = list(range(blk * 4, min(blk * 4 + 4, NT)))
                      ntok = len(tts) * 128
                      hb = blk % 2
                      for ti, tt in enumerate(tts):
                          m = 0 if tt < 32 else 1
                          b = tt % 2
                          LD(xt[b][:], cur[tt * 128:(tt + 1) * 128, :], ["X0", "X1"] if False else [], [f"xt{b}"])
                          A_(lambda e: e.activation(out=junk[:, 0:D], in_=xt[b][:], func=AF.Square, accum_out=st[b][:, 0:1]), [f"xt{b}"], ["junk", f"st{b}"])
                          A_(lambda e: e.activation(out=st[b][:, 1:2], in_=st[b][:, 0:1], func=AF.Sqrt, scale=1.0 / D, bias=epsc[:, 0:1]), [f"st{b}", "epsc"], [f"st{b}"])
                          V_(lambda e: e.reciprocal(st[b][:, 2:3], st[b][:, 1:2]), [f"st{b}"], [f"st{b}"])
                          V_(lambda e: e.tensor_scalar(xn[b][:], xt[b][:], st[b][:, 2:3], None, op0=ALU.mult), [f"xt{b}", f"st{b}"], [f"xn{b}"])
                          for dt in range(8):
                              M_(lambda e: e.transpose(psb[0][:, dt * 128:(dt + 1) * 128], xn[b][:, dt * 128:(dt + 1) * 128], idb[:]), [f"xn{b}", "idb"], [PB[0]])
                          for dt in range(8):
                              A_(lambda e: e.activation(out=hT[hb][:, dt, ti * 128:(ti + 1) * 128], in_=psb[0][:, dt * 128:(dt + 1) * 128], func=AF.Identity,
                                                        scale=scA[:, dt, m:m + 1], bias=mods[:, dt, m:m + 1]), [PB[0], "scA", "mods"], [f"hT{hb}"])
                          chk(1.2)
                      for ti, tt in enumerate(tts):
                          b = tt % 2
                          for cgi in range(6):
                              pb = PF[cgi % 3]; pt = psf[cgi % 3]
                              for kt in range(8):
                                  M_(lambda e: e.matmul(pt[:], hT[hb][:, kt, ti * 128:(ti + 1) * 128], wq[:, kt, cgi * 512:(cgi + 1) * 512],
                                                        start=(kt == 0), stop=(kt == 7)), [f"hT{hb}", "wq"], [pb])
                              chk(1.25)
                              if cgi == 1:
                                  chk(1.2515)
                              if cgi < 2:
                                  A_(lambda e: e.activation(out=qs[:, cgi * 512:(cgi + 1) * 512], in_=pt[:], func=AF.Identity), [pb], ["qs"])
                                  chk(1.251 + 0.001 * cgi)
                              elif cgi == 2:
                                  A_(lambda e: e.activation(out=qs[:, 1024:1280], in_=pt[:, 0:256], func=AF.Identity), [pb], ["qs"])
                                  chk(1.253)
                                  A_(lambda e: e.activation(out=vsb[b][:], in_=pt[:, 256:512], func=AF.Identity), [pb], [f"vsb{b}"])
                                  chk(1.26)
                                  LD(V_d[tt * 128:(tt + 1) * 128, :], vsb[b][:], [f"vsb{b}"], ["V_d"])
                                  chk(1.27)
                              else:
                                  if cgi % 2 == 0:
                                      A_(lambda e: e.activation(out=usb[b][:, (cgi - 3) * 512:(cgi - 2) * 512], in_=pt[:], func=AF.Identity), [pb], [f"usb{b}"])
                                  else:
                                      V_(lambda e: e.tensor_scalar(usb[b][:, (cgi - 3) * 512:(cgi - 2) * 512], pt[:], 1.0, None, op0=ALU.mult), [pb], [f"usb{b}"])
                          LD(U_d[tt * 128:(tt + 1) * 128, :], usb[b][:], [f"usb{b}"], ["U_d"])
                          chk(1.3)
                          q3 = qs[:].rearrange("p (h d) -> p h d", d=128)
                          A_(lambda e: e.activation(out=q2[:], in_=qs[:], func=AF.Square), ["qs"], ["q2"])
                          V_(lambda e: e.tensor_reduce(hs[:, 0:10], q2[:].rearrange("p (h d) -> p h d", d=128), axis=AX.X, op=ALU.add), ["q2"], ["hs"])
                          A_(lambda e: e.activation(out=hs[:, 0:10], in_=hs[:, 0:10], func=AF.Sqrt, scale=1.0 / 128, bias=epsc[:, 0:1]), ["hs", "epsc"], ["hs"])
                          V_(lambda e: e.reciprocal(hs[:, 0:10], hs[:, 0:10]), ["hs"], ["hs"])
                          V_(lambda e: e.tensor_tensor(q2[:].rearrange("p (h d) -> p h d", d=128), q3, hs[:, 0:10].unsqueeze(2).to_broadcast([128, 10, 128]), op=ALU.mult), ["qs", "hs"], ["q2"])
                          G_(lambda e: e.tensor_tensor(qs[:], q2[:], qkw[:].rearrange("p a b -> p (a b)"), op=ALU.mult), ["q2", "qkw"], ["qs"])
                          chk(1.4)
                          LD(rc[:, b, :], I["ropec"][:, tt, :], w=["rc"]); LD(rs[:, b, :], I["ropes"][:, tt, :], w=["rs"])
                          cb = rc[:, b, :].unsqueeze(1).to_broadcast([128, 10, 64]); sbb = rs[:, b, :].unsqueeze(1).to_broadcast([128, 10, 64])
                          x1v = q3[:, :, 0:64]; x2v = q3[:, :, 64:128]
                          V_(lambda e: e.tensor_tensor(r1[:], x1v, cb, op=ALU.mult), ["qs", "rc"], ["r1"])
                          G_(lambda e: e.tensor_tensor(r2[:], x2v, sbb, op=ALU.mult), ["qs", "rs"], ["r2"])
                          V_(lambda e: e.tensor_tensor(qr[b][:, :, 0:64], r1[:], r2[:], op=ALU.subtract), ["r1", "r2"], [f"qr{b}"])
                          V_(lambda e: e.tensor_tensor(r1[:], x1v, sbb, op=ALU.mult), ["qs", "rs", f"qr{b}"], ["r1"])
                          G_(lambda e: e.tensor_tensor(r2[:], x2v, cb, op=ALU.mult), ["qs", "rc", f"qr{b}"], ["r2"])
                          V_(lambda e: e.tensor_tensor(qr[b][:, :, 64:128], r1[:], r2[:], op=ALU.add), ["r1", "r2"], [f"qr{b}"])
                          chk(1.5)
                          for h in range(10):
                              pbi = 1 if h < 8 else 0
                              off = (h % 8) * 128
                              M_(lambda e: e.transpose(psb[1][:, off:off + 128] if h < 8 else psb[0][:, off:off + 128], qr[b][:, h, :], idb[:]),
                                 [f"qr{b}", "idb"], [PB[pbi]])
                          V_(lambda e: e.tensor_scalar(qT[hb][:, 0:8, ti * 128:(ti + 1) * 128], psb[1][:].rearrange("p (h t) -> p h t", t=128), 1.0, None, op0=ALU.mult), [PB[1]], [f"qT{hb}"])
                          A_(lambda e: e.activation(out=qT[hb][:, 8:10, ti * 128:(ti + 1) * 128], in_=psb[0][:, 0:256].rearrange("p (h t) -> p h t", t=128), func=AF.Identity), [PB[0]], [f"qT{hb}"])
                          chk(1.6)
                      t0 = blk * 512
                      for h in range(8):
                          LD(QT_d[h, :, t0:t0 + ntok], qT[hb][:, h, 0:ntok], [f"qT{hb}"], ["QT_d"])
                      for h in range(2):
                          LD(KT_d[h, :, t0:t0 + ntok], qT[hb][:, 8 + h, 0:ntok], [f"qT{hb}"], ["KT_d"])
                      for dt in range(8):
                          LD(HT_d[dt * 128:(dt + 1) * 128, t0:t0 + ntok], hT[hb][:, dt, 0:ntok], [f"hT{hb}"], ["HT_d"])
              P.barrier()
              if stop == 2:
                  break
              with ExitStack() as es:
                  sb = lambda n, s, d: es.enter_context(nc.sbuf_tensor(uniq(n), list(s), d))
                  wg = sb("wg", [128, 8, 2048], BF16)
                  hT = [sb(f"hT{i}", [128, 8, 512], BF16) for i in range(2)]
                  gsb = [sb(f"gsb{i}", [128, 512], BF16) for i in range(2)]
                  wsrc = I["w_in"][li].rearrange("(kt k) n -> k kt n", k=128)
                  for cgi in range(2):
                      LDC(wg[:, :, cgi * 1024:(cgi + 1) * 1024], wsrc[:, :, 3072 + cgi * 1024:3072 + (cgi + 1) * 1024], w=["wg"])
                  ntg = T if not last else S
                  for blk in range((ntg + 511) // 512):
                      t0 = blk * 512
                      ntok = min(512, ntg - t0)
                      hb = blk % 2
                      LD(hT[hb][:, :, 0:ntok], HT_d[:, t0:t0 + ntok].rearrange("(kt p) t -> p kt t", p=128), w=[f"hT{hb}"])
                      for ncn in range(16):
                          pb = PF[3 + ncn % 2]; pt = psf[3 + ncn % 2]
                          for kt in range(8):
                              M_(lambda e: e.matmul(pt[:, 0:ntok], wg[:, kt, ncn * 128:(ncn + 1) * 128], hT[hb][:, kt, 0:ntok],
                                                    start=(kt == 0), stop=(kt == 7)), [f"hT{hb}", "wg"], [pb])
                          gi = ncn % 2
                          A_(lambda e: e.activation(out=gsb[gi][:, 0:ntok], in_=pt[:, 0:ntok], func=AF.Sigmoid), [pb], [f"gsb{gi}"])
                          LD(G_d[ncn * 128:(ncn + 1) * 128, t0:t0 + ntok], gsb[gi][:, 0:ntok], [f"gsb{gi}"], ["G_d"])
              P.barrier()

              if stop == 3:
                  break
              if do_attn:
                  with ExitStack() as es:
                      sb = lambda n, s, d: es.enter_context(nc.sbuf_tensor(uniq(n), list(s), d))
                      kT = sb("kT", [128, 2, T], BF16); vv = sb("vv", [128, NT, 256], BF16)
                      qb = [sb(f"qb{i}", [128, 512], BF16) for i in range(2)]
                      pT = [sb(f"pT{i}", [128, 512], BF16) for i in range(3)]
                      rcp = sb("rcp", [128, 512], F32)
                      yo = [sb(f"yo{i}", [128, 512], BF16) for i in range(2)]
                      for h in range(2):
                          LD(kT[:, h, :], KT_d[h], w=["kT"])
                      LD(vv[:], V_d.rearrange("(t p) c -> p t c", p=128), w=["vv"])
                      scale = 128 ** -0.5
                      it = 0
                      blocks = [(h, q0, 512, 0, NT) for h in range(8) for q0 in range(0, S, 512)]
                      if not last:
                          blocks += [(h, S, 256, 32, NT) for h in range(8)]
                      for bi, (h, q0, nq, k0, k1) in enumerate(blocks):
                          kv = h // 4
                          b = bi % 2
                          LD(qb[b][:, 0:nq], QT_d[h, :, q0:q0 + nq], w=[f"qb{b}"])
                          po = psf[4 + b]; pok = PF[4 + b]
                          psm = psf[2 + b]; psmk = PF[2 + b]
                          def qk_(kt, g):
                              sbk = g % 2
                              M_(lambda e: e.matmul(psf[sbk][:, 0:nq], kT[:, kv, kt * 128:(kt + 1) * 128], qb[b][:, 0:nq], start=True, stop=True),
                                 ["kT", f"qb{b}"], [PF[sbk]])

                          def ex_(kt, g):
                              sbk = g % 2; pb3 = g % 3
                              A_(lambda e: e.activation(out=pT[pb3][:, 0:nq], in_=psf[sbk][:, 0:nq], func=AF.Exp, scale=scale), [PF[sbk]], [f"pT{pb3}"])

                          def pv_(kt, g):
                              pb3 = g % 3
                              M_(lambda e: e.matmul(po[:, 0:nq], vv[:, kt, kv * 128:(kv + 1) * 128], pT[pb3][:, 0:nq], start=(kt == k0), stop=(kt == k1 - 1)),
                                 ["vv", f"pT{pb3}"], [pok])
                              M_(lambda e: e.matmul(psm[:, 0:nq], onesb[:], pT[pb3][:, 0:nq], start=(kt == k0), stop=(kt == k1 - 1)),
                                 ["onesb", f"pT{pb3}"], [psmk])

                          g0 = it
                          it += (k1 - k0)
                          qk_(k0, g0)
                          for kt in range(k0, k1):
                              g = g0 + (kt - k0)
                              if kt + 1 < k1:
                                  qk_(kt + 1, g + 1)
                              ex_(kt, g)
                              pv_(kt, g)
                          V_(lambda e: e.reciprocal(rcp[:, 0:nq], psm[:, 0:nq]), [psmk], ["rcp"])
                          V_(lambda e: e.tensor_tensor(yo[b][:, 0:nq], po[:, 0:nq], rcp[:, 0:nq], op=ALU.mult), [pok, "rcp"], [f"yo{b}"])
                          LD(YA_d[h * 128:(h + 1) * 128, q0:q0 + nq], yo[b][:, 0:nq], [f"yo{b}"], ["YA_d"])
                  P.barrier()

              if stop == 4:
                  break
              if do_hyena:
                  with ExitStack() as es:
                      sb = lambda n, s, d: es.enter_context(nc.sbuf_tensor(uniq(n), list(s), d))
                      cw_ = sb("cw_", [128, 3, 1536], F32); cb_ = sb("cb_", [128, 1536], F32)
                      um = [sb(f"um{i}", [128, 1536], F32) for i in range(2)]
                      u0 = [sb(f"u0{i}", [128, 1536], F32) for i in range(2)]
                      up = [sb(f"up{i}", [128, 1536], F32) for i in range(2)]
                      oc = [sb(f"oc{i}", [128, 1536], F32) for i in range(2)]
                      t3 = sb("t3", [128, 1536], F32)
                      LD(cw_[:].rearrange("p a b -> p (a b)"), I["convw"][li], w=["cw_"]); LD(cb_[:], I["convb"][li], w=["cb_"])
                      tiles = list(range(NT if not last else 32))
                      for tt in tiles:
                          b = tt % 2
                          first = tt in (0, 32); lastt = tt in (31, 33)
                          r0 = tt * 128
                          if first:
                              V_(lambda e: e.memset(um[b][:], 0.0), w=[f"um{b}"])
                              LD(um[b][1:128, :], U_d[r0:r0 + 127, :], w=[f"um{b}"])
                          else:
                              LD(um[b][:], U_d[r0 - 1:r0 + 127, :], w=[f"um{b}"])
                          LD(u0[b][:], U_d[r0:r0 + 128, :], w=[f"u0{b}"])
                          if lastt:
                              V_(lambda e: e.memset(up[b][:], 0.0), w=[f"up{b}"])
                              LD(up[b][0:127, :], U_d[r0 + 1:r0 + 128, :], w=[f"up{b}"])
                          else:
                              LD(up[b][:], U_d[r0 + 1:r0 + 129, :], w=[f"up{b}"])
                          V_(lambda e: e.tensor_tensor(oc[b][:], u0[b][:], cw_[:, 1, :], op=ALU.mult), [f"u0{b}", "cw_"], [f"oc{b}"])
                          G_(lambda e: e.tensor_tensor(t3[:], um[b][:], cw_[:, 0, :], op=ALU.mult), [f"um{b}", "cw_"], ["t3"])
                          V_(lambda e: e.tensor_tensor(oc[b][:], oc[b][:], t3[:], op=ALU.add), [f"oc{b}", "t3"], [f"oc{b}"])
                          G_(lambda e: e.tensor_tensor(t3[:], up[b][:], cw_[:, 2, :], op=ALU.mult), [f"up{b}", "cw_", f"oc{b}"], ["t3"])
                          V_(lambda e: e.tensor_tensor(oc[b][:], oc[b][:], t3[:], op=ALU.add), [f"oc{b}", "t3"], [f"oc{b}"])
                          G_(lambda e: e.tensor_tensor(oc[b][:], oc[b][:], cb_[:], op=ALU.add), [f"oc{b}", "cb_"], [f"oc{b}"])
                          LD(XV_d[r0:r0 + 128, :], oc[b][:], [f"oc{b}"], ["XV_d"])
                  P.barrier()

                  seqs = [("lat", 0, 64)] + ([] if last else [("ctx", S, 4)])
                  with ExitStack() as es:
                      sb = lambda n, s, d: es.enter_context(nc.sbuf_tensor(uniq(n), list(s), d))
                      w3 = sb("w3", [64, 2048], F32)
                      h2 = sb("h2", [64, 2, 4096], F32)
                      nt_ = sb("nt_", [64, 2, 64], F32); vm_ = sb("vm_", [64, 2, 64], F32); dl = sb("dl", [64, 512], F32)
                      LD(w3[:], I["pe_w3"][li], w=["w3"]); LD(dl[:], I["delta"][:, :], w=["dl"])
                      for si, (sname, tok0, pmax) in enumerate(seqs):
                          LD(nt_[:], I[f"nt_{sname}"].rearrange("d p j -> p d j"), w=["nt_"]); LD(vm_[:], I[f"vm_{sname}"].rearrange("d p j -> p d j"), w=["vm_"])
                          with ExitStack() as es2:
                              sb2 = lambda n, s, d: es2.enter_context(nc.sbuf_tensor(uniq(n), list(s), d))
                              w1 = sb2("w1", [33, 64], F32); w2 = sb2("w2", [64, 64], F32)
                              pv = sb2("pv", [64, 4], F32); pvb = sb2("pvb", [64, 2], F32)
                              zT = sb2("zT", [33, 4096], F32)
                              h1 = sb2("h1", [64, 512], F32); harg = sb2("harg", [64, 512], F32); hw1 = sb2("hw1", [64, 512], F32); hw2 = sb2("hw2", [64, 512], F32)
                              LD(w1[:], I["pe_w1"][li], w=["w1"]); LD(w2[:], I["pe_w2"][li], w=["w2"]); LD(pv[:], I["pe_v"][li], w=["pv"])
                              V_(lambda e: e.tensor_tensor(pvb[:, 0:1], pv[:, 0:1], pv[:, 1:2], op=ALU.mult), ["pv"], ["pvb"])
                              V_(lambda e: e.tensor_tensor(pvb[:, 1:2], pv[:, 2:3], pv[:, 3:4], op=ALU.mult), ["pv"], ["pvb"])

                              def sin_layer(ps, fcol, bcol, out_ap, okey):
                                  A_(lambda e: e.activation(out=harg[:], in_=ps, func=AF.Identity, scale=pv[:, fcol:fcol + 1], bias=pvb[:, bcol:bcol + 1]), [PF[0], "pv", "pvb"], ["harg"])
                                  for _ in range(2):
                                      V_(lambda e: e.tensor_scalar(hw1[:], harg[:], PI, -2 * PI, op0=ALU.is_gt, op1=ALU.mult), ["harg"], ["hw1"])
                                      V_(lambda e: e.tensor_scalar(hw2[:], harg[:], -PI, 2 * PI, op0=ALU.is_lt, op1=ALU.mult), ["harg"], ["hw2"])
                                      V_(lambda e: e.tensor_tensor(hw1[:], hw1[:], hw2[:], op=ALU.add), ["hw1", "hw2"], ["hw1"])
                                      V_(lambda e: e.tensor_tensor(harg[:], harg[:], hw1[:], op=ALU.add), ["harg", "hw1"], ["harg"])
                                  A_(lambda e: e.activation(out=out_ap, in_=harg[:], func=AF.Sin), ["harg"], [okey])

                              for dr in range(2):
                                  LD(zT[:], I[f"z_{sname}"][dr], w=["zT"])
                                  for ck in range(8):
                                      M_(lambda e: e.matmul(psf[0][0:64, :], w1[:], zT[:, ck * 512:(ck + 1) * 512], start=True, stop=True), ["w1", "zT"], [PF[0]])
                                      sin_layer(psf[0][0:64, :], 0, 0, h1[:], "h1")
                                      M_(lambda e: e.matmul(psf[0][0:64, :], w2[:], h1[:], start=True, stop=True), ["w2", "h1"], [PF[0]])
                                      sin_layer(psf[0][0:64, :], 2, 1, h2[:, dr, ck * 512:(ck + 1) * 512], "h2")
                          P.barrier()
                          for o in range(2):
                              with ExitStack() as es2:
                                  sb2 = lambda n, s, d: es2.enter_context(nc.sbuf_tensor(uniq(n), list(s), d))
                                  mlo = sb2("mlo", [64, 64, 2, 128], BF16); mhi = sb2("mhi", [64, 64, 2, 128], BF16)
                                  win = [sb2(f"win{i}", [64, 512], F32) for i in range(2)]
                                  kfb = [sb2(f"kfb{i}", [64, 2, 512], BF16) for i in range(2)]
                                  asb = [sb2(f"asb{i}", [128, 2, 512], BF16) for i in range(2)]
                                  LDC(mlo[:], I["mlo"][:, :, :, :], w=["mlo"]); LDC(mhi[:], I["mhi"][:, :, :, :], w=["mhi"])
                                  for j in range(64):
                                      b = j % 2
                                      for dr in range(2):
                                          A_(lambda e: e.activation(out=win[dr][:], in_=dl[:], func=AF.Exp, scale=nt_[:, dr, j:j + 1]), ["dl", "nt_"], [f"win{dr}"])
                                          M_(lambda e: e.matmul(psf[1 + dr][0:64, :], h2[:, dr, :].rearrange("k (p j) -> k j p", j=64)[:, j, :],
                                                                w3[:, o * 1024 + dr * 512:o * 1024 + (dr + 1) * 512], start=True, stop=True), ["h2", "w3"], [PF[1 + dr]])
                                          V_(lambda e: e.scalar_tensor_tensor(out=kfb[b][:, dr, :], in0=win[dr][:], scalar=vm_[:, dr, j:j + 1], in1=psf[1 + dr][0:64, :], op0=ALU.mult, op1=ALU.mult),
                                             [f"win{dr}", "vm_", PF[1 + dr]], [f"kfb{b}"])
                                      for ri in range(2):
                                          M_(lambda e: e.matmul(psf[3 + ri][:], mlo[:, j, ri, :], kfb[b][:, 0, :], start=True, stop=False), ["mlo", f"kfb{b}"], [PF[3 + ri]])
                                          M_(lambda e: e.matmul(psf[3 + ri][:], mhi[:, j, ri, :], kfb[b][:, 1, :], start=False, stop=True), ["mhi", f"kfb{b}"], [PF[3 + ri]])
                                      A_(lambda e: e.activation(out=asb[b][:, 0, :], in_=psf[3][:], func=AF.Identity), [PF[3]], [f"asb{b}"])
                                      V_(lambda e: e.tensor_scalar(asb[b][:, 1, :], psf[4][:], 1.0, None, op0=ALU.mult), [PF[4]], [f"asb{b}"])
                                      LD(A_d[:, j, :, :], asb[b][:], [f"asb{b}"], ["A_d"])
                              P.barrier()
                              with ExitStack() as es2:
                                  sb2 = lambda n, s, d: es2.enter_context(nc.sbuf_tensor(uniq(n), list(s), d))
                                  w64 = sb2("w64", [64, 8, 128], BF16)
                                  bsb = [sb2(f"bsb{i}", [64, 4, 2, 512], BF16) for i in range(2)]
                                  ksb = [sb2(f"ksb{i}", [128, 2, 512], F32) for i in range(2)]
                                  LDC(w64[:], I["w64"][:, :, :], w=["w64"])
                                  for fc in range(32):
                                      b = fc % 2
                                      LD(bsb[b][:], A_d[fc * 4:(fc + 1) * 4].rearrange("f j r c -> j f r c"), w=[f"bsb{b}"])
                                      for fl in range(4):
                                          f1i = fc * 4 + fl
                                          kb2 = f1i % 2
                                          for ab in range(2):
                                              pt = psf[ab]; pk = PF[ab]
                                              M_(lambda e: e.matmul(pt[:], w64[:, 4 + 2 * ab, :], bsb[b][:, fl, 0, :], start=True, stop=False), ["w64", f"bsb{b}"], [pk])
                                              M_(lambda e: e.matmul(pt[:], w64[:, 5 + 2 * ab, :], bsb[b][:, fl, 1, :], start=False, stop=True), ["w64", f"bsb{b}"], [pk])
                                          A_(lambda e: e.activation(out=ksb[kb2][:, 0, :], in_=psf[0][:], func=AF.Identity), [PF[0]], [f"ksb{kb2}"])
                                          V_(lambda e: e.tensor_scalar(ksb[kb2][:, 1, :], psf[1][:], 1.0, None, op0=ALU.mult), [PF[1]], [f"ksb{kb2}"])
                                          LD(KH_d[si, o, f1i].rearrange("a r c -> r a c"), ksb[kb2][:], [f"ksb{kb2}"], ["KH_d"])
                              P.barrier()
                  P.barrier()

                  with ExitStack() as es:
                      sb = lambda n, s, d: es.enter_context(nc.sbuf_tensor(uniq(n), list(s), d))
                      mlo = sb("mlo", [64, 64, 2, 128], BF16); minvT = sb("minvT", [128, 64, 2, 64], BF16)
                      w64 = sb("w64", [64, 4, 128], BF16); wi1 = sb("wi1", [128, 128], BF16)
                      skb = sb("skb", [64, 1024], F32)
                      vz = sb("vz", [64, 64, CW], BF16); zz = sb("zz", [64, 64, CW], BF16)
                      asb = [sb(f"asb{i}", [128, 2, CW], BF16) for i in range(2)]
                      bsb = [sb(f"bsb{i}", [64, 8, 2, CW], BF16) for i in range(2)]
                      ksb = [sb(f"ksb{i}", [128, 2, CW], F32) for i in range(3)]
                      y1 = [sb(f"y1{i}", [128, CW], F32) for i in range(2)]
                      y2 = [sb(f"y2{i}", [128, CW], F32) for i in range(2)]
                      yb = [sb(f"yb{i}", [128, CW], BF16) for i in range(2)]
                      csb = [sb(f"csb{i}", [128, 8, CW], BF16) for i in range(2)]
                      cj = [sb(f"cj{i}", [128, 2, CW], BF16) for i in range(2)]
                      xg = [sb(f"xg{i}", [64, CW], F32) for i in range(2)]
                      tg = [sb(f"tg{i}", [64, CW], F32) for i in range(2)]
                      yh = [sb(f"yh{i}", [64, CW], BF16) for i in range(2)]
                      LDC(mlo[:], I["mlo"][:, :, :, :], w=["mlo"]); LDC(minvT[:], I["minvT"][:, :, :, :], w=["minvT"])
                      LDC(w64[:], I["w64"][:, 0:4, :], w=["w64"]); LDC(wi1[:], I["wi1"][:, :], w=["wi1"])
                      LD(skb[:], I["skipb"][li], w=["skb"])
                      skp = sb("skp", [64, CW], F32)
                      passes = [[(0, 0, 64, k * 256, 256, 0)] for k in range(2)]
                      if not last:
                          passes += [[(1, S, 4, k * 256, 256, 0)] for k in range(2)]
                      for segs in passes:
                          if True:
                              for b in range(2):
                                  V_(lambda e: e.memset(xg[b][:], 0.0), w=[f"xg{b}"])
                              if segs[0][2] < 64:
                                  V_(lambda e: e.memset(vz[:], 0.0), w=["vz"])
                              for (si, tok0, pmax, c0, w_, off) in segs:
                                  LDC(vz[0:pmax, :, off:off + w_], XV_d[tok0:tok0 + pmax * 64, 1024 + c0:1024 + c0 + w_].rearrange("(p j) c -> p j c", j=64), w=["vz"])
                              for o in range(2):
                                  src = vz if o == 0 else zz
                                  skey = "vz" if o == 0 else "zz"
                                  for j in range(64):
                                      b = j % 2
                                      for ri in range(2):
                                          M_(lambda e: e.matmul(psf[ri + 2 * b][:, 0:CW], mlo[:, j, ri, :], src[:, j, :], start=True, stop=True), ["mlo", skey], [PF[ri + 2 * b]])
                                      A_(lambda e: e.activation(out=asb[b][:, 0, :], in_=psf[2 * b][:, 0:CW], func=AF.Identity), [PF[2 * b]], [f"asbr{b}"])
                                      V_(lambda e: e.tensor_scalar(asb[b][:, 1, :], psf[1 + 2 * b][:, 0:CW], 1.0, None, op0=ALU.mult), [PF[1 + 2 * b]], [f"asbi{b}"])
                                      LD(A_d[:, j, :, 0:CW], asb[b][:], [f"asbr{b}", f"asbi{b}"], ["A_d"])
                                  def s3_mm(f1i):
                                      fc, fl = divmod(f1i, 8)
                                      b = fc % 2; par = f1i % 2; k3 = f1i % 3
                                      if fl == 0:
                                          for r_ in range(2):
                                              LD(bsb[b][:, :, r_, :], A_d[fc * 8:(fc + 1) * 8, :, r_, 0:CW].rearrange("f j c -> j f c"), w=[f"bsb{b}"])
                                      for (si, tok0, pmax, c0, w_, off) in segs:
                                          LD(ksb[k3][:, :, off:off + w_], KH_d[si, o, f1i, :, :, c0:c0 + w_].rearrange("a r c -> r a c"), w=[f"ksb{k3}"])
                                      for pq in range(2):
                                          pi = pq + 2 * par
                                          M_(lambda e: e.matmul(psf[pi][:, 0:CW], w64[:, 2 * pq, :], bsb[b][:, fl, 0, :], start=True, stop=False), ["w64", f"bsb{b}"], [PF[pi]])
                                          M_(lambda e: e.matmul(psf[pi][:, 0:CW], w64[:, 2 * pq + 1, :], bsb[b][:, fl, 1, :], start=False, stop=True), ["w64", f"bsb{b}"], [PF[pi]])

                                  def s3_post(f1i):
                                      fc, fl = divmod(f1i, 8)
                                      b = fc % 2; par = f1i % 2; k3 = f1i % 3; b2 = par
                                      V_(lambda e: e.tensor_tensor(y1[b2][:], psf[2 * par][:, 0:CW], ksb[k3][:, 0, :], op=ALU.mult), [PF[2 * par], f"ksb{k3}"], [f"y1{b2}"])
                                      V_(lambda e: e.tensor_tensor(y2[b2][:], psf[1 + 2 * par][:, 0:CW], ksb[k3][:, 1, :], op=ALU.mult), [PF[1 + 2 * par], f"ksb{k3}"], [f"y2{b2}"])
                                      V_(lambda e: e.tensor_tensor(yb[b2][:], y1[b2][:], y2[b2][:], op=ALU.add), [f"y1{b2}", f"y2{b2}"], [f"yb{b2}"])
                                      M_(lambda e: e.matmul(psf[4 + par][:, 0:CW], wi1[:], yb[b2][:], start=True, stop=True), ["wi1", f"yb{b2}"], [PF[4 + par]])
                                      A_(lambda e: e.activation(out=csb[b][:, fl, :], in_=psf[4 + par][:, 0:CW], func=AF.Identity), [PF[4 + par]], [f"csb{b}"])
                                      if fl == 7:
                                          LD(C_d[:, fc * 8:(fc + 1) * 8, :], csb[b][:], [f"csb{b}"], ["C_d"])

                                  P.barrier()
                                  s3_mm(0)
                                  for f1i in range(128):
                                      if f1i + 1 < 128:
                                          s3_mm(f1i + 1)
                                      s3_post(f1i)
                                  P.barrier()
                                  Cv = C_d.rearrange("(r j) f c -> j f r c", j=64)
                                  for (si, tok0, pmax, c0, w_, off) in segs:
                                      V_(lambda e: e.tensor_scalar(skp[:, off:off + w_], skb[:, o * 512 + c0:o * 512 + c0 + w_], 1.0, None, op0=ALU.mult), ["skb"], ["skp"])
                                  for j in range(64):
                                      b = j % 2
                                      LD(cj[b][:], Cv[j], w=[f"cj{b}"])
                                      M_(lambda e: e.matmul(psf[4 + b][0:64, 0:CW], minvT[:, j, 0, :], cj[b][:, 0, :], start=True, stop=False), ["minvT", f"cj{b}"], [PF[4 + b]])
                                      M_(lambda e: e.matmul(psf[4 + b][0:64, 0:CW], minvT[:, j, 1, :], cj[b][:, 1, :], start=False, stop=True), ["minvT", f"cj{b}"], [PF[4 + b]])
                                      for (si, tok0, pmax, c0, w_, off) in segs:
                                          xcol = (0 if o == 0 else 512) + c0
                                          LD(xg[b][0:pmax, off:off + w_], XV_d[tok0:tok0 + pmax * 64, xcol:xcol + w_].rearrange("(p j) c -> p j c", j=64)[:, j, :], w=[f"xg{b}"])
                                      G_(lambda e: e.tensor_tensor(tg[b][:], src[:, j, :], skp[:], op=ALU.mult), [skey, "skp"], [f"tg{b}"])
                                      V_(lambda e: e.tensor_tensor(tg[b][:], psf[4 + b][0:64, 0:CW], tg[b][:], op=ALU.add), [PF[4 + b], f"tg{b}"], [f"tg{b}"])
                                      if o == 0:
                                          V_(lambda e: e.tensor_tensor(zz[:, j, :], tg[b][:], xg[b][:], op=ALU.mult), [f"tg{b}", f"xg{b}"], ["zz"])
                                      else:
                                          V_(lambda e: e.tensor_tensor(yh[b][:], tg[b][:], xg[b][:], op=ALU.mult), [f"tg{b}", f"xg{b}"], [f"yh{b}"])
                                          for (si, tok0, pmax, c0, w_, off) in segs:
                                              LD(YH_d[tok0:tok0 + pmax * 64, c0:c0 + w_].rearrange("(p j) c -> p j c", j=64)[:, j, :], yh[b][0:pmax, off:off + w_], [f"yh{b}"], ["YH_d"])
                                  P.barrier()
                  P.barrier()

              if stop == 5:
                  break
              with ExitStack() as es:
                  sb = lambda n, s, d: es.enter_context(nc.sbuf_tensor(uniq(n), list(s), d))
                  wap = sb("wap", [128, 8, D], BF16); whp = sb("whp", [128, 4, D], BF16); wo = sb("wo", [128, 8, D], BF16)
                  rw = sb("rw", [128, 8, NE], F32); rbb = sb("rbb", [128, NE], F32)
                  ya = [sb(f"ya{i}", [128, 8, 512], BF16) for i in range(2)]
                  yht = [sb(f"yht{i}", [128, 512], BF16) for i in range(2)]
                  yhT = sb("yhT", [128, 4, 512], BF16)
                  gt = [sb("gt0", [128, 16, 512], BF16)] * 2
                  mT = sb("mT", [128, 8, 512], BF16)
                  m1 = [sb(f"m1{i}", [128, 512], F32) for i in range(2)]
                  m2 = [sb(f"m2{i}", [128, 512], F32) for i in range(2)]
                  xt = [sb(f"xt{i}", [128, D], F32) for i in range(2)]
                  xo = [sb(f"xo{i}", [128, D], F32) for i in range(2)]
                  junk = sb("junk", [128, D], BF16)
                  st = [sb(f"st{i}", [128, 4], F32) for i in range(2)]
                  xn = [sb(f"xn{i}", [128, D], F32) for i in range(2)]
                  h32 = sb("h32", [128, 8, 128], F32)
                  h2b = sb("h2b", [128, 8, 512], BF16)
                  lg = sb("lg", [128, NE], F32); sc_ = sb("sc_", [128, NE], F32); bi_ = sb("bi_", [128, NE], F32)
                  t8 = sb("t8", [128, 8, 8], F32); gs = sb("gs", [128, 8], F32); gm = sb("gm", [128, 8], F32)
                  sm = sb("sm", [128, 4], F32); em = sb("em", [128, NE], F32); gate = [sb(f"gate{i}", [128, NE], F32) for i in range(2)]
                  LDC(wap[:], I["w_ap"][li].rearrange("(kt k) n -> k kt n", k=128), w=["wap"])
                  LDC(whp[:], I["w_hp"][li].rearrange("(kt k) n -> k kt n", k=128), w=["whp"])
                  LDC(wo[:], I["w_out"][li].rearrange("(kt k) n -> k kt n", k=128), w=["wo"])
                  LD(rw[:], I["router_w"].rearrange("(kt k) n -> k kt n", k=128), w=["rw"]); LD(rbb[:], I["router_bb"][:, :], w=["rbb"])
                  ntiles = NT if not last else 32
                  nblk = (ntiles * 128 + 511) // 512
                  for blk in range(nblk):
                      tts = list(range(blk * 4, min(blk * 4 + 4, ntiles)))
                      ntok = len(tts) * 128
                      t0 = blk * 512
                      b = blk % 2
                      LD(ya[b][:, :, 0:ntok], YA_d[:, t0:t0 + ntok].rearrange("(h p) t -> p h t", p=128), w=[f"ya{b}"])
                      LD(gt[b][:, :, 0:ntok], G_d[:, t0:t0 + ntok].rearrange("(h p) t -> p h t", p=128), w=["gt"])
                      for ti, tt in enumerate(tts):
                          yb_ = tt % 2
                          LD(yht[yb_][:], YH_d[tt * 128:(tt + 1) * 128, :], w=[f"yht{yb_}"])
                          for ct in range(4):
                              M_(lambda e: e.transpose(psb[0][:, ct * 128:(ct + 1) * 128], yht[yb_][:, ct * 128:(ct + 1) * 128], idb[:]), [f"yht{yb_}", "idb"], [PB[0]])
                          A_(lambda e: e.activation(out=yhT[:, :, ti * 128:(ti + 1) * 128], in_=psb[0][:, 0:512].rearrange("p (c t) -> p c t", t=128), func=AF.Identity), [PB[0]], ["yhT"])
                      for ncn in range(8):
                          pa = psf[ncn % 2]; pak = PF[ncn % 2]; ph = psf[2 + ncn % 2]; phk = PF[2 + ncn % 2]
                          mb = ncn % 2
                          for kt in range(8):
                              M_(lambda e: e.matmul(pa[:, 0:ntok], wap[:, kt, ncn * 128:(ncn + 1) * 128], ya[b][:, kt, 0:ntok], start=(kt == 0), stop=(kt == 7)), ["wap", f"ya{b}"], [pak])
                          for ct in range(4):
                              M_(lambda e: e.matmul(ph[:, 0:ntok], whp[:, ct, ncn * 128:(ncn + 1) * 128], yhT[:, ct, 0:ntok], start=(ct == 0), stop=(ct == 3)), ["whp", "yhT"], [phk])
                          V_(lambda e: e.tensor_tensor(m1[mb][:, 0:ntok], pa[:, 0:ntok], gt[b][:, ncn, 0:ntok], op=ALU.mult), [pak, "gt"], [f"m1{mb}"])
                          V_(lambda e: e.tensor_tensor(m2[mb][:, 0:ntok], ph[:, 0:ntok], gt[b][:, 8 + ncn, 0:ntok], op=ALU.mult), [phk, "gt"], [f"m2{mb}"])
                          G_(lambda e: e.tensor_tensor(mT[:, ncn, 0:ntok], m1[mb][:, 0:ntok], m2[mb][:, 0:ntok], op=ALU.add), [f"m1{mb}", f"m2{mb}"], ["mT"])
                      for ti, tt in enumerate(tts):
                          m = 0 if tt < 32 else 1
                          xb_ = tt % 2
                          LD(xt[xb_][:], cur[tt * 128:(tt + 1) * 128, :], w=[f"xt{xb_}"])
                          for hf in range(2):
                              po = psf[4 + hf]; pok = PF[4 + hf]
                              for kt in range(8):
                                  M_(lambda e: e.matmul(po[:], mT[:, kt, ti * 128:(ti + 1) * 128], wo[:, kt, hf * 512:(hf + 1) * 512], start=(kt == 0), stop=(kt == 7)), ["mT", "wo"], [pok])
                              V_(lambda e: e.tensor_tensor(xo[xb_][:, hf * 512:(hf + 1) * 512], po[:], gb[:, m, hf * 512:(hf + 1) * 512], op=ALU.mult), [pok, "gb"], [f"xo{xb_}"])
                          G_(lambda e: e.tensor_tensor(xo[xb_][:], xo[xb_][:], xt[xb_][:], op=ALU.add), [f"xo{xb_}", f"xt{xb_}"], [f"xo{xb_}"])
                          LD(mid[tt * 128:(tt + 1) * 128, :], xo[xb_][:], [f"xo{xb_}"], ["mid"])
                          A_(lambda e: e.activation(out=junk[:], in_=xo[xb_][:], func=AF.Square, accum_out=st[xb_][:, 0:1]), [f"xo{xb_}"], ["junk", f"st{xb_}"])
                          A_(lambda e: e.activation(out=st[xb_][:, 1:2], in_=st[xb_][:, 0:1], func=AF.Sqrt, scale=1.0 / D, bias=epsc[:, 0:1]), [f"st{xb_}", "epsc"], [f"st{xb_}"])
                          V_(lambda e: e.reciprocal(st[xb_][:, 2:3], st[xb_][:, 1:2]), [f"st{xb_}"], [f"st{xb_}"])
                          V_(lambda e: e.tensor_scalar(xn[xb_][:], xo[xb_][:], st[xb_][:, 2:3], None, op0=ALU.mult), [f"xo{xb_}", f"st{xb_}"], [f"xn{xb_}"])
                          for dt in range(8):
                              M_(lambda e: e.transpose(psf[dt // 4][:, (dt % 4) * 128:(dt % 4 + 1) * 128], xn[xb_][:, dt * 128:(dt + 1) * 128], idf[:]), [f"xn{xb_}", "idf"], [PF[dt // 4]])
                          for dt in range(8):
                              A_(lambda e: e.activation(out=h32[:, dt, :], in_=psf[dt // 4][:, (dt % 4) * 128:(dt % 4 + 1) * 128], func=AF.Identity,
                                                        scale=scB[:, dt, m:m + 1], bias=mods[:, 24 + dt, m:m + 1]), [PF[dt // 4], "scB", "mods"], ["h32"])
                          G_(lambda e: e.tensor_scalar(h2b[:, :, ti * 128:(ti + 1) * 128], h32[:], 1.0, None, op0=ALU.mult), ["h32"], ["h2b"])
                          for dt in range(8):
                              M_(lambda e: e.matmul(psf[2][:, 0:NE], h32[:, dt, :], rw[:, dt, :], start=(dt == 0), stop=(dt == 7)), ["h32", "rw"], [PF[2]])
                          gb_ = tt % 2
                          A_(lambda e: e.activation(out=sc_[:], in_=psf[2][:, 0:NE], func=AF.Sigmoid), [PF[2]], ["sc_"])
                          V_(lambda e: e.tensor_tensor(bi_[:], sc_[:], rbb[:], op=ALU.add), ["sc_", "rbb"], ["bi_"])
                          for g in range(8):
                              V_(lambda e: e.max(out=t8[:, g, :], in_=bi_[:, g * 8:(g + 1) * 8]), ["bi_"], ["t8"])
                          V_(lambda e: e.tensor_tensor(gs[:], t8[:, :, 0], t8[:, :, 1], op=ALU.add), ["t8"], ["gs"])
                          V_(lambda e: e.tensor_reduce(sm[:, 0:1], gs[:], axis=AX.X, op=ALU.max), ["gs"], ["sm"])
                          V_(lambda e: e.tensor_scalar(gm[:], gs[:], sm[:, 0:1], None, op0=ALU.is_equal), ["gs", "sm"], ["gm"])
                          V_(lambda e: e.tensor_tensor(gs[:], gm[:], t8[:, :, 1], op=ALU.mult), ["gm", "t8"], ["gs"])
                          V_(lambda e: e.tensor_reduce(sm[:, 1:2], gs[:], axis=AX.X, op=ALU.add), ["gs"], ["sm"])
                          V_(lambda e: e.tensor_scalar(em[:], bi_[:], sm[:, 1:2], None, op0=ALU.is_ge), ["bi_", "sm"], ["em"])
                          V_(lambda e: e.tensor_tensor(em[:].rearrange("p (g k) -> p g k", k=8), em[:].rearrange("p (g k) -> p g k", k=8),
                                                       gm[:].unsqueeze(2).to_broadcast([128, 8, 8]), op=ALU.mult), ["em", "gm"], ["em"])
                          V_(lambda e: e.tensor_tensor(em[:], em[:], sc_[:], op=ALU.mult), ["em", "sc_"], ["em"])
                          V_(lambda e: e.tensor_reduce(sm[:, 2:3], em[:], axis=AX.X, op=ALU.add), ["em"], ["sm"])
                          V_(lambda e: e.reciprocal(sm[:, 3:4], sm[:, 2:3]), ["sm"], ["sm"])
                          V_(lambda e: e.tensor_scalar(gate[gb_][:], em[:], sm[:, 3:4], None, op0=ALU.mult), ["em", "sm"], [f"gate{gb_}"])
                          LD(GATE_d[tt * 128:(tt + 1) * 128, :], gate[gb_][:], [f"gate{gb_}"], ["GATE_d"])
                      for dt in range(8):
                          LD(H2T_d[dt * 128:(dt + 1) * 128, t0:t0 + ntok], h2b[:, dt, 0:ntok], ["h2b"], ["H2T_d"])
              P.barrier()

              if stop == 6:
                  break
              ntiles = NT if not last else 32
              with ExitStack() as es:
                  sb = lambda n, s, d: es.enter_context(nc.sbuf_tensor(uniq(n), list(s), d))
                  SBK = 1024
                  hT2 = sb("hT2", [128, 8, SBK], BF16)
                  acc = sb("acc", [128, SBK // 128, D], F32)
                  gat = sb("gat", [128, SBK // 128, NE], F32)
                  w1 = [sb(f"ew1{i}", [128, 8, 512], BF16) for i in range(2)]
                  w3 = [sb(f"ew3{i}", [128, 8, 512], BF16) for i in range(2)]
                  w2 = [sb(f"ew2{i}", [128, 4, D], BF16) for i in range(2)]
                  s1 = [sb(f"s1{i}", [128, 512], F32) for i in range(2)]
                  gT = [sb(f"gT{i}", [128, 4, 512], BF16) for i in range(2)]
                  xt = [sb(f"xt{i}", [128, D], F32) for i in range(2)]
                  xo = [sb(f"xo{i}", [128, D], F32) for i in range(2)]
                  junk = sb("junk", [128, D], BF16); st = [sb(f"st{i}", [128, 4], F32) for i in range(2)]
                  fnw = sb("fnw", [128, D], F32)
                  LD(fnw[:], I["fnw"][:, :], w=["fnw"])
                  ntok_all = ntiles * 128
                  for s0 in range(0, ntok_all, SBK):
                      sn = min(SBK, ntok_all - s0)
                      stl = sn // 128
                      LD(hT2[:, :, 0:sn], H2T_d[:, s0:s0 + sn].rearrange("(kt p) t -> p kt t", p=128), w=["hT2"])
                      LD(gat[:, 0:stl, :], GATE_d[s0:s0 + sn, :].rearrange("(t p) e -> p t e", p=128), w=["gat"])
                      V_(lambda e: e.memset(acc[:], 0.0), w=["acc"])
                      if do_moe:
                          for ex in range(NE):
                              wb = ex % 2
                              LDC(w1[wb][:], I["exp_w1"][li, ex].rearrange("(kt k) n -> k kt n", k=128), w=[f"ew1{wb}"])
                              LDC(w3[wb][:], I["exp_w3"][li, ex].rearrange("(kt k) n -> k kt n", k=128), w=[f"ew3{wb}"])
                              LDC(w2[wb][:], I["exp_w2"][li, ex].rearrange("(kt k) n -> k kt n", k=128), w=[f"ew2{wb}"])
                              for b0 in range(0, sn, 512):
                                  bn = min(512, sn - b0)
                                  gbi = (b0 // 512) % 2
                                  for fcn in range(4):
                                      p1 = psf[fcn % 2]; p1k = PF[fcn % 2]; p3 = psf[2 + fcn % 2]; p3k = PF[2 + fcn % 2]
                                      sbi = fcn % 2
                                      for kt in range(8):
                                          M_(lambda e: e.matmul(p1[:, 0:bn], w1[wb][:, kt, fcn * 128:(fcn + 1) * 128], hT2[:, kt, b0:b0 + bn], start=(kt == 0), stop=(kt == 7)), [f"ew1{wb}", "hT2"], [p1k])
                                      for kt in range(8):
                                          M_(lambda e: e.matmul(p3[:, 0:bn], w3[wb][:, kt, fcn * 128:(fcn + 1) * 128], hT2[:, kt, b0:b0 + bn], start=(kt == 0), stop=(kt == 7)), [f"ew3{wb}", "hT2"], [p3k])
                                      A_(lambda e: e.activation(out=s1[sbi][:, 0:bn], in_=p1[:, 0:bn], func=AF.Silu), [p1k], [f"s1{sbi}"])
                                      V_(lambda e: e.tensor_tensor(gT[gbi][:, fcn, 0:bn], p3[:, 0:bn], s1[sbi][:, 0:bn], op=ALU.mult), [p3k, f"s1{sbi}"], [f"gT{gbi}"])
                                  for ti in range(bn // 128):
                                      tl = (b0 // 128) + ti
                                      for hf in range(2):
                                          po = psf[4 + hf]; pok = PF[4 + hf]
                                          for ft in range(4):
                                              M_(lambda e: e.matmul(po[:], gT[gbi][:, ft, ti * 128:(ti + 1) * 128], w2[wb][:, ft, hf * 512:(hf + 1) * 512], start=(ft == 0), stop=(ft == 3)), [f"gT{gbi}", f"ew2{wb}"], [pok])
                                          V_(lambda e: e.scalar_tensor_tensor(out=acc[:, tl, hf * 512:(hf + 1) * 512], in0=po[:], scalar=gat[:, tl, ex:ex + 1],
                                                                              in1=acc[:, tl, hf * 512:(hf + 1) * 512], op0=ALU.mult, op1=ALU.add), [pok, "gat", "acc"], ["acc"])
                      for tl in range(stl):
                          tt = s0 // 128 + tl
                          m = 0 if tt < 32 else 1
                          xb_ = tt % 2
                          LD(xt[xb_][:], mid[tt * 128:(tt + 1) * 128, :], w=[f"xt{xb_}"])
                          V_(lambda e: e.tensor_tensor(xo[xb_][:], acc[:, tl, :], gb[:, 2 + m, :], op=ALU.mult), ["acc", "gb"], [f"xo{xb_}"])
                          G_(lambda e: e.tensor_tensor(xo[xb_][:], xo[xb_][:], xt[xb_][:], op=ALU.add), [f"xo{xb_}", f"xt{xb_}"], [f"xo{xb_}"])
                          if not last:
                              LD(cur[tt * 128:(tt + 1) * 128, :], xo[xb_][:], [f"xo{xb_}"], ["cur"])
                          else:
                              A_(lambda e: e.activation(out=junk[:], in_=xo[xb_][:], func=AF.Square, accum_out=st[xb_][:, 0:1]), [f"xo{xb_}"], ["junk", f"st{xb_}"])
                              A_(lambda e: e.activation(out=st[xb_][:, 1:2], in_=st[xb_][:, 0:1], func=AF.Sqrt, scale=1.0 / D, bias=epsc[:, 0:1]), [f"st{xb_}", "epsc"], [f"st{xb_}"])
                              V_(lambda e: e.reciprocal(st[xb_][:, 2:3], st[xb_][:, 1:2]), [f"st{xb_}"], [f"st{xb_}"])
                              V_(lambda e: e.scalar_tensor_tensor(out=xt[xb_][:], in0=xo[xb_][:], scalar=st[xb_][:, 2:3], in1=fnw[:], op0=ALU.mult, op1=ALU.mult),
                                 [f"xo{xb_}", f"st{xb_}", "fnw"], [f"xt{xb_}"])
                              LD(OUT[tt * 128:(tt + 1) * 128, :], xt[xb_][:], [f"xt{xb_}"], ["OUT"])
              P.barrier()
          except _Stop:
              break
        P.dead = False
        P.barrier()
        nops = P.nops
    return nc, nops


def make_in_maps(inputs, cores=range(8)):
    f = lambda a: np.ascontiguousarray(np.asarray(a, dtype=np.float32))
    consts = _consts()
    shared = {}
    shared["w_ada"] = f(inputs["w_ada"])
    shared["b_adaT"] = f(np.asarray(inputs["b_ada"]).reshape(2, 48, 128).transpose(0, 2, 1))
    shared["n1T"] = f(np.asarray(inputs["norm1_w"]).reshape(2, 8, 128).transpose(0, 2, 1))
    shared["n2T"] = f(np.asarray(inputs["norm2_w"]).reshape(2, 8, 128).transpose(0, 2, 1))
    shared["w_in"] = f(inputs["w_in"])
    qw = np.asarray(inputs["q_norm_w"]); kw = np.asarray(inputs["k_norm_w"])
    qkw = np.concatenate([np.tile(qw, (1, 8)), np.tile(kw, (1, 2))], 1)
    shared["qkw"] = f(np.broadcast_to(qkw[:, None, :], (2, 128, 1280)))
    cw = np.asarray(inputs["hy_conv_w"]).reshape(2, 3 * 1536)
    shared["convw"] = f(np.broadcast_to(cw[:, None, :], (2, 128, 3 * 1536)))
    shared["convb"] = f(np.broadcast_to(np.asarray(inputs["hy_conv_b"])[:, None, :], (2, 128, 1536)))
    shared["pe_w1"] = f(inputs["hy_pe_w1"]); shared["pe_w2"] = f(inputs["hy_pe_w2"]); shared["pe_w3"] = f(inputs["hy_pe_w3"])
    shared["pe_v"] = f(np.stack([np.asarray(inputs["hy_freq1"]), np.asarray(inputs["hy_pe_b1"]),
                                 np.asarray(inputs["hy_freq2"]), np.asarray(inputs["hy_pe_b2"])], -1))
    sk = np.asarray(inputs["hy_skip"]).reshape(2, 1024)
    shared["skipb"] = f(np.broadcast_to(sk[:, None, :], (2, 64, 1024)))
    shared["w_ap"] = f(inputs["w_att_proj"]); shared["w_hp"] = f(inputs["w_hy_proj"]); shared["w_out"] = f(inputs["w_out"])
    shared["router_w"] = f(inputs["router_w"])
    shared["router_bb"] = f(np.broadcast_to(np.asarray(inputs["router_b"])[None, :], (128, NE)))
    shared["exp_w1"] = f(inputs["exp_w1"]); shared["exp_w3"] = f(inputs["exp_w3"]); shared["exp_w2"] = f(inputs["exp_w2"])
    shared["fnw"] = f(np.broadcast_to(np.asarray(inputs["final_norm_w"])[None, :], (128, D)))
    for k, v in consts.items():
        shared["c_" + k] = f(v)
    x = np.asarray(inputs["x"]); c = np.asarray(inputs["c"]); ctx = np.asarray(inputs["ctx"]); c_ctx = np.asarray(inputs["c_ctx"])
    maps = []
    for b in cores:
        m = dict(shared)
        m["x"] = f(x[b]); m["ctx"] = f(ctx[b])
        cc = np.stack([c[b].reshape(8, 128).T, c_ctx.reshape(8, 128).T], -1)
        m["cc"] = f(cc)
        maps.append(m)
    return maps


_NC_CACHE = {}


def kernel(**inputs):
    if "nc" not in _NC_CACHE:
        _NC_CACHE["nc"] = build()[0]
    nc = _NC_CACHE["nc"]
    maps = make_in_maps(inputs)
    res = run_bass_kernel_spmd(nc, maps, core_ids=list(range(8)))
    out = np.stack([np.asarray(r["out"]) for r in res.results], 0).astype(np.float32)
    return out
```

```python
import math
import numpy as np
import concourse.bass as bass
import concourse.mybir as mybir
from concourse.bass_utils import run_bass_kernel_spmd
from contextlib import ExitStack

F32 = mybir.dt.float32
BF16 = mybir.dt.bfloat16
AF = mybir.ActivationFunctionType
ALU = mybir.AluOpType
AX = mybir.AxisListType

S = 4096
C = 256
T = S + C
NT = T // 128
D = 1024
NE = 64
EPS = 1e-6
NFFT = 8192
CW = 256
PI = float(np.pi)


class _Stop(Exception):
    pass


class Prog:
    EPOCH = 30000
    NDMA = 24

    def __init__(self, nc, es):
        self.nc = nc
        self.es = es
        self.eng = {"pe": nc.tensor, "act": nc.scalar, "dve": nc.vector, "pool": nc.gpsimd, "sp": nc.sync}
        self.sems = {e: [es.enter_context(nc.semaphore(f"s_{e}_0"))] for e in self.eng}
        self.cnt = {e: 0 for e in self.eng}
        self.ep = {e: 0 for e in self.eng}
        self.dsem = [es.enter_context(nc.semaphore(f"s_dma_{i}")) for i in range(self.NDMA)]
        self.dcnt = [0] * self.NDMA
        self.dnext = 0
        self.waited = {e: {} for e in self.eng}
        self.W = {}
        self.R = {}
        self.nops = 0
        self.dead = False

    def _wait(self, e, tok):
        sem, val, src = tok
        if src == e and e == "pe":
            return
        w = self.waited[e]
        k = id(sem)
        if w.get(k, 0) >= val:
            return
        self.eng[e].wait_ge(sem, val)
        w[k] = val

    def _deps(self, reads, writes):
        toks = []
        for k in reads:
            toks.extend(self.W.get(k, {}).values())
        for k in writes:
            toks.extend(self.W.get(k, {}).values())
            toks.extend(self.R.get(k, {}).values())
        return toks

    def _commit(self, tok, reads, writes):
        sid = id(tok[0])
        for k in reads:
            d = self.R.setdefault(k, {})
            if sid not in d or d[sid][1] < tok[1]:
                d[sid] = tok
        for k in writes:
            d = self.W.setdefault(k, {})
            if sid not in d or d[sid][1] < tok[1]:
                d[sid] = tok

    def op(self, e, fn, reads=(), writes=()):
        if self.dead:
            return None
        for t in self._deps(reads, writes):
            self._wait(e, t)
        if self.cnt[e] >= self.EPOCH:
            self.ep[e] += 1
            self.sems[e].append(self.es.enter_context(self.nc.semaphore(f"s_{e}_{self.ep[e]}")))
            self.cnt[e] = 0
        inst = fn(self.eng[e])
        self.cnt[e] += 1
        sem = self.sems[e][-1]
        inst.then_inc(sem, 1)
        tok = (sem, self.cnt[e], e)
        self._commit(tok, reads, writes)
        self.nops += 1
        return tok

    def dma(self, q, fn, reads=(), writes=()):
        if self.dead:
            return None
        for t in self._deps(reads, writes):
            self._wait(q, t)
        i = self.dnext
        self.dnext = (self.dnext + 1) % self.NDMA
        sem = self.dsem[i]
        if self.dcnt[i] > 0:
            self._wait(q, (sem, 16 * self.dcnt[i], None))
        inst = fn(self.eng[q])
        self.dcnt[i] += 1
        inst.then_inc(sem, 16)
        tok = (sem, 16 * self.dcnt[i], None)
        self._commit(tok, reads, writes)
        self.nops += 1
        return tok

    def barrier(self):
        if self.dead:
            return
        toks = []
        for e in self.eng:
            if self.cnt[e] > 0:
                toks.append((self.sems[e][-1], self.cnt[e], e))
        for i in range(self.NDMA):
            if self.dcnt[i] > 0:
                toks.append((self.dsem[i], 16 * self.dcnt[i], None))
        for e in self.eng:
            for t in toks:
                if t[2] != e:
                    self._wait(e, t)
        self.W = {}
        self.R = {}


def _consts():
    c = {}
    c["ident"] = np.eye(128, dtype=np.float32)
    t = np.arange(S)
    row = (t // 64).astype(np.float32)
    col = (t % 64).astype(np.float32)
    n = 32
    inv = (10000.0 ** (-np.arange(n, dtype=np.float32) / n)).astype(np.float32)
    ang = np.concatenate([row[:, None] * inv, col[:, None] * inv], -1).astype(np.float32)
    cs = np.ones((T, 64), np.float32)
    sn = np.zeros((T, 64), np.float32)
    cs[:S] = np.cos(ang)
    sn[:S] = np.sin(ang)
    c["ropec"] = np.ascontiguousarray(cs.reshape(NT, 128, 64).transpose(1, 0, 2))
    c["ropes"] = np.ascontiguousarray(sn.reshape(NT, 128, 64).transpose(1, 0, 2))
    p = np.arange(128, dtype=np.float64)[:, None, None]
    j = np.arange(64, dtype=np.float64)[None, :, None]
    f1 = np.arange(128, dtype=np.float64)[None, None, :]
    M = np.exp(-2j * np.pi * (p * f1 / 128.0 + j * f1 / NFFT))
    Mri = np.stack([M.real, M.imag], 2)
    c["mlo"] = np.ascontiguousarray(Mri[:64]).astype(np.float32)
    c["mhi"] = np.ascontiguousarray(Mri[64:]).astype(np.float32)
    Mi = np.stack([M.real[:64], M.imag[:64]], 0) / NFFT
    c["minvT"] = np.ascontiguousarray(Mi.transpose(3, 2, 0, 1)).astype(np.float32)
    jj = np.arange(64, dtype=np.float64)[:, None]
    f2 = np.arange(64, dtype=np.float64)[None, :]
    Wre = np.cos(2 * np.pi * jj * f2 / 64.0)
    Wim = -np.sin(2 * np.pi * jj * f2 / 64.0)
    cat = lambda a, b: np.concatenate([a, b], 1)
    st = [cat(Wre, Wim), cat(-Wim, Wre), cat(Wim, Wre), cat(Wre, -Wim),
          cat(Wre, Wre), cat(-Wim, -Wim), cat(-Wim, Wim), cat(-Wre, Wre)]
    c["w64"] = np.ascontiguousarray(np.stack(st, 1)).astype(np.float32)
    wi = np.zeros((128, 128))
    wi[:64, :64] = Wre.T
    wi[:64, 64:] = -Wim.T
    wi[64:, :64] = Wim.T
    wi[64:, 64:] = Wre.T
    c["wi1"] = wi.astype(np.float32)
    deltas = np.abs(np.linspace(math.log(1e-2) / 1.5, math.log(1e-2) / 0.3, 512, dtype=np.float32))
    c["delta"] = np.ascontiguousarray(np.broadcast_to(deltas[None, :], (64, 512))).astype(np.float32)

    def zfeat(pos, L):
        t01 = (np.linspace(0.0, 1.0, L, dtype=np.float32))[pos][:, None]
        posf = pos.astype(np.float32)[:, None]
        bands = np.linspace(1e-4, 15, 16, dtype=np.float32)[None, :]
        f = (2.0 * math.pi * posf * bands / L).astype(np.float32)
        return np.concatenate([t01, np.cos(f), -np.sin(f)], -1).astype(np.float32), t01[:, 0]

    for name, L in (("lat", S), ("ctx", C)):
        q = np.arange(4096)
        vf = q < L
        posf = np.where(vf, q, 0)
        zf, t01f = zfeat(posf, L)
        d = 4096 - q
        vb = (d >= 1) & (d < L)
        posb = np.where(vb, d, 0)
        zb, t01b = zfeat(posb, L)
        c[f"z_{name}"] = np.ascontiguousarray(np.stack([zf.T, zb.T], 0))
        c[f"nt_{name}"] = np.ascontiguousarray(np.stack([-t01f.reshape(64, 64), -t01b.reshape(64, 64)], 0))
        c[f"vm_{name}"] = np.ascontiguousarray(np.stack([vf.reshape(64, 64), vb.reshape(64, 64)], 0).astype(np.float32))
    return c


_CONST_SHAPES = None


def build(debug=(), nlayers=2, do_moe=True, do_hyena=True, do_attn=True, stop=99):
    nc = bass.Bass("TRN2", target_bir_lowering=False)
    consts = _consts()
    _u = [0]

    def uniq(n):
        _u[0] += 1
        return f"t{_u[0]}_{n}"

    def din(name, shape, dt=F32):
        return nc.dram_tensor(name, list(shape), dt, kind="ExternalInput").ap()

    def dscr(name, shape, dt):
        kind = "ExternalOutput" if name in debug else "Internal"
        return nc.dram_tensor(name, list(shape), dt, kind=kind).ap()

    I = {}
    I["x"] = din("x", [S, D]); I["ctx"] = din("ctx", [C, D]); I["cc"] = din("cc", [128, 8, 2])
    I["w_ada"] = din("w_ada", [2, D, 6 * D]); I["b_adaT"] = din("b_adaT", [2, 128, 48])
    I["n1T"] = din("n1T", [2, 128, 8]); I["n2T"] = din("n2T", [2, 128, 8])
    I["w_in"] = din("w_in", [2, D, 5120])
    I["qkw"] = din("qkw", [2, 128, 1280])
    I["convw"] = din("convw", [2, 128, 3 * 1536]); I["convb"] = din("convb", [2, 128, 1536])
    I["pe_w1"] = din("pe_w1", [2, 33, 64]); I["pe_w2"] = din("pe_w2", [2, 64, 64]); I["pe_w3"] = din("pe_w3", [2, 64, 2048])
    I["pe_v"] = din("pe_v", [2, 64, 4])
    I["skipb"] = din("skipb", [2, 64, 1024])
    I["w_ap"] = din("w_ap", [2, D, D]); I["w_hp"] = din("w_hp", [2, 512, D]); I["w_out"] = din("w_out", [2, D, D])
    I["router_w"] = din("router_w", [D, NE]); I["router_bb"] = din("router_bb", [128, NE])
    if do_moe:
        I["exp_w1"] = din("exp_w1", [2, NE, D, 512]); I["exp_w3"] = din("exp_w3", [2, NE, D, 512]); I["exp_w2"] = din("exp_w2", [2, NE, 512, D])
    I["fnw"] = din("fnw", [128, D])
    for k, v in consts.items():
        I[k] = din("c_" + k, v.shape)
    OUT = nc.dram_tensor("out", [S, D], F32, kind="ExternalOutput").ap()

    X0 = dscr("X0", [T, D], F32); X1 = dscr("X1", [T, D], F32)
    QT_d = dscr("QT_d", [8, 128, T], BF16); KT_d = dscr("KT_d", [2, 128, T], BF16)
    V_d = dscr("V_d", [T, 256], BF16); U_d = dscr("U_d", [T, 1536], F32)
    G_d = dscr("G_d", [2048, T], BF16); YA_d = dscr("YA_d", [D, T], BF16)
    XV_d = dscr("XV_d", [T, 1536], F32)
    A_d = dscr("A_d", [128, 64, 2, 512], BF16); C_d = dscr("C_d", [128, 128, CW], BF16)
    KH_d = dscr("KH_d", [2, 2, 128, 2, 128, 512], BF16)
    YH_d = dscr("YH_d", [T, 512], BF16)
    A2_d = dscr("A2_d", [128, 64, 2, CW], BF16)
    H2T_d = dscr("H2T_d", [D, T], BF16); GATE_d = dscr("GATE_d", [T, NE], F32)
    HT_d = dscr("HT_d", [D, T], BF16)

    with ExitStack() as es0:
        P = Prog(nc, es0)
        V_ = lambda fn, r=(), w=(): P.op("dve", fn, r, w)
        A_ = lambda fn, r=(), w=(): P.op("act", fn, r, w)
        M_ = lambda fn, r=(), w=(): P.op("pe", fn, r, w)
        G_ = lambda fn, r=(), w=(): P.op("dve", fn, r, w)
        def LD(out, in_, r=(), w=()):
            q = "pool" if (str(out.space) == "DRAM" and str(in_.space) != "DRAM") else "sp"
            return P.dma(q, lambda e: e.dma_start(out=out, in_=in_), r, w)
        LDC = lambda out, in_, r=(), w=(): P.dma("pool", lambda e: e.dma_start(out=out, in_=in_), r, w)

        psf = [es0.enter_context(nc.psum_tensor(f"psf{i}", [128, 512], F32)) for i in range(6)]
        psb = [es0.enter_context(nc.psum_tensor(f"psb{i}", [128, 1024], BF16)) for i in range(2)]
        PF = [f"psf{i}" for i in range(6)]
        PB = [f"psb{i}" for i in range(2)]

        def sbp(name, shape, dt):
            return es0.enter_context(nc.sbuf_tensor(uniq(name), list(shape), dt))
        idf = sbp("idf", [128, 128], F32); idb = sbp("idb", [128, 128], BF16)
        onesf = sbp("onesf", [128, 128], F32); onesb = sbp("onesb", [128, 128], BF16)
        epsc = sbp("epsc", [128, 1], F32)
        mods = sbp("mods", [128, 48, 2], F32)
        scA = sbp("scA", [128, 8, 2], F32); scB = sbp("scB", [128, 8, 2], F32)
        gb = sbp("gb", [128, 4, D], F32)
        LD(idf[:], I["ident"][:, :], w=["idf"]); LDC(idb[:], I["ident"][:, :], w=["idb"])
        V_(lambda e: e.memset(onesf[:], 1.0), w=["onesf"]); V_(lambda e: e.memset(onesb[:], 1.0), w=["onesb"])
        V_(lambda e: e.memset(epsc[:], EPS), w=["epsc"])
        LD(X0[0:S, :], I["x"][:, :], w=["X0"]); LD(X0[S:T, :], I["ctx"][:, :], w=["X0"])
        P.barrier()

        def chk(x):
            if stop == x and not P.dead:
                P.barrier()
                P.dead = True

        for li in range(nlayers):
          try:
              last = li == 1
              if stop == 0:
                  break
              ntl = 32 if False else NT

              with ExitStack() as es:
                  sb = lambda n, s, d: es.enter_context(nc.sbuf_tensor(uniq(n), list(s), d))
                  ccs = sb("ccs", [128, 8, 2], F32)
                  wa = [sb(f"wa{i}", [128, 8, 512], F32) for i in range(2)]
                  bT = sb("bT", [128, 48], F32); n1 = sb("n1", [128, 8], F32); n2 = sb("n2", [128, 8], F32)
                  dg = sb("dg", [128, 128], F32); tmp = sb("tmpa", [128, 8, 2], F32)
                  LD(ccs[:], I["cc"][:, :, :], w=["ccs"])
                  A_(lambda e: e.activation(out=ccs[:], in_=ccs[:], func=AF.Silu), ["ccs"], ["ccs"])
                  LD(bT[:], I["b_adaT"][li], w=["bT"]); LD(n1[:], I["n1T"][li], w=["n1"]); LD(n2[:], I["n2T"][li], w=["n2"])
                  wsrc = I["w_ada"][li].rearrange("(kt k) n -> k kt n", k=128)
                  for g in range(12 if stop != 0.3 else 0):
                      w = wa[g % 2]
                      LD(w[:], wsrc[:, :, g * 512:(g + 1) * 512], w=[f"wa{g % 2}"])
                      for sub in range(4):
                          ch = g * 4 + sub
                          for kt in range(8):
                              M_(lambda e: e.matmul(psf[0][:, ch * 2:ch * 2 + 2], w[:, kt, sub * 128:(sub + 1) * 128], ccs[:, kt, :],
                                                    start=(kt == 0), stop=(kt == 7)), [f"wa{g % 2}", "ccs"], [PF[0]])
                  if stop in (0.3, 0.5):
                      break
                  V_(lambda e: e.tensor_tensor(mods[:], psf[0][:, 0:96].rearrange("p (c m) -> p c m", m=2),
                                               bT[:].unsqueeze(2).to_broadcast([128, 48, 2]), op=ALU.add), [PF[0], "bT"], ["mods"])
                  V_(lambda e: e.tensor_scalar(tmp[:], mods[:, 8:16, :], 1.0, None, op0=ALU.add), ["mods"], ["tmpa"])
                  V_(lambda e: e.tensor_tensor(scA[:], tmp[:], n1[:].unsqueeze(2).to_broadcast([128, 8, 2]), op=ALU.mult), ["tmpa", "n1"], ["scA"])
                  V_(lambda e: e.tensor_scalar(tmp[:], mods[:, 32:40, :], 1.0, None, op0=ALU.add), ["mods", "scA"], ["tmpa"])
                  V_(lambda e: e.tensor_tensor(scB[:], tmp[:], n2[:].unsqueeze(2).to_broadcast([128, 8, 2]), op=ALU.mult), ["tmpa", "n2"], ["scB"])
                  if stop == 0.7:
                      break
                  for gi, base in enumerate((16, 40)):
                      for m in range(2):
                          for dt in range(8):
                              V_(lambda e: e.tensor_scalar(dg[:], idf[:], mods[:, base + dt, m:m + 1], None, op0=ALU.mult), ["idf", "mods"], ["dg"])
                              M_(lambda e: e.matmul(psf[1][:, 0:128], onesf[:], dg[:], start=True, stop=True), ["onesf", "dg"], [PF[1]])
                              A_(lambda e: e.activation(out=gb[:, gi * 2 + m, dt * 128:(dt + 1) * 128], in_=psf[1][:, 0:128], func=AF.Identity), [PF[1]], ["gb"])
              P.barrier()

              if stop == 1:
                  break
              cur, mid = X0, X1
              with ExitStack() as es:
                  sb = lambda n, s, d: es.enter_context(nc.sbuf_tensor(uniq(n), list(s), d))
                  wq = sb("wq", [128, 8, 3072], BF16)
                  qkw = sb("qkw", [128, 10, 128], F32)
                  rc = sb("rc", [128, 2, 64], F32); rs = sb("rs", [128, 2, 64], F32)
                  xt = [sb(f"xt{i}", [128, D], F32) for i in range(2)]
                  xn = [sb(f"xn{i}", [128, D], BF16) for i in range(2)]
                  junk = sb("junk", [128, 1280], BF16)
                  st = [sb(f"st{i}", [128, 4], F32) for i in range(2)]
                  hT = [sb(f"hT{i}", [128, 8, 512], BF16) for i in range(2)]
                  qs = sb("qs", [128, 1280], F32); q2 = sb("q2", [128, 1280], F32)
                  hs = sb("hs", [128, 16], F32)
                  r1 = sb("r1", [128, 10, 64], F32); r2 = sb("r2", [128, 10, 64], F32)
                  qr = [sb(f"qr{i}", [128, 10, 128], BF16) for i in range(2)]
                  qT = [sb(f"qT{i}", [128, 10, 512], BF16) for i in range(2)]
                  vsb = [sb(f"vsb{i}", [128, 256], BF16) for i in range(2)]
                  usb = [sb(f"usb{i}", [128, 1536], F32) for i in range(2)]
                  wsrc = I["w_in"][li].rearrange("(kt k) n -> k kt n", k=128)
                  for cgi in range(3):
                      LDC(wq[:, :, cgi * 1024:(cgi + 1) * 1024], wsrc[:, :, cgi * 1024:(cgi + 1) * 1024], w=["wq"])
                  LD(qkw[:].rearrange("p a b -> p (a b)"), I["qkw"][li], w=["qkw"])
                  nblk = (T + 511) // 512
                  for blk in range(nblk):
                      tts = list(range(blk * 4, min(blk * 4 + 4, NT)))
                      ntok = len(tts) * 128
                      hb = blk % 2
                      for ti, tt in enumerate(tts):
                          m = 0 if tt < 32 else 1
                          b = tt % 2
                          LD(xt[b][:], cur[tt * 128:(tt + 1) * 128, :], ["X0", "X1"] if False else [], [f"xt{b}"])
                          A_(lambda e: e.activation(out=junk[:, 0:D], in_=xt[b][:], func=AF.Square, accum_out=st[b][:, 0:1]), [f"xt{b}"], ["junk", f"st{b}"])
                          A_(lambda e: e.activation(out=st[b][:, 1:2], in_=st[b][:, 0:1], func=AF.Sqrt, scale=1.0 / D, bias=epsc[:, 0:1]), [f"st{b}", "epsc"], [f"st{b}"])
                          V_(lambda e: e.reciprocal(st[b][:, 2:3], st[b][:, 1:2]), [f"st{b}"], [f"st{b}"])
                          V_(lambda e: e.tensor_scalar(xn[b][:], xt[b][:], st[b][:, 2:3], None, op0=ALU.mult), [f"xt{b}", f"st{b}"], [f"xn{b}"])
                          for dt in range(8):
                              M_(lambda e: e.transpose(psb[0][:, dt * 128:(dt + 1) * 128], xn[b][:, dt * 128:(dt + 1) * 128], idb[:]), [f"xn{b}", "idb"], [PB[0]])
                          for dt in range(8):
                              A_(lambda e: e.activation(out=hT[hb][:, dt, ti * 128:(ti + 1) * 128], in_=psb[0][:, dt * 128:(dt + 1) * 128], func=AF.Identity,
                                                        scale=scA[:, dt, m:m + 1], bias=mods[:, dt, m:m + 1]), [PB[0], "scA", "mods"], [f"hT{hb}"])
                          chk(1.2)
                      for ti, tt in enumerate(tts):
                          b = tt % 2
                          for cgi in range(6):
                              pb = PF[cgi % 3]; pt = psf[cgi % 3]
                              for kt in range(8):
                                  M_(lambda e: e.matmul(pt[:], hT[hb][:, kt, ti * 128:(ti + 1) * 128], wq[:, kt, cgi * 512:(cgi + 1) * 512],
                                                        start=(kt == 0), stop=(kt == 7)), [f"hT{hb}", "wq"], [pb])
                              chk(1.25)
                              if cgi == 1:
                                  chk(1.2515)
                              if cgi < 2:
                                  A_(lambda e: e.activation(out=qs[:, cgi * 512:(cgi + 1) * 512], in_=pt[:], func=AF.Identity), [pb], ["qs"])
                                  chk(1.251 + 0.001 * cgi)
                              elif cgi == 2:
                                  A_(lambda e: e.activation(out=qs[:, 1024:1280], in_=pt[:, 0:256], func=AF.Identity), [pb], ["qs"])
                                  chk(1.253)
                                  A_(lambda e: e.activation(out=vsb[b][:], in_=pt[:, 256:512], func=AF.Identity), [pb], [f"vsb{b}"])
                                  chk(1.26)
                                  LD(V_d[tt * 128:(tt + 1) * 128, :], vsb[b][:], [f"vsb{b}"], ["V_d"])
                                  chk(1.27)
                              else:
                                  if cgi % 2 == 0:
                                      A_(lambda e: e.activation(out=usb[b][:, (cgi - 3) * 512:(cgi - 2) * 512], in_=pt[:], func=AF.Identity), [pb], [f"usb{b}"])
                                  else:
                                      V_(lambda e: e.tensor_scalar(usb[b][:, (cgi - 3) * 512:(cgi - 2) * 512], pt[:], 1.0, None, op0=ALU.mult), [pb], [f"usb{b}"])
                          LD(U_d[tt * 128:(tt + 1) * 128, :], usb[b][:], [f"usb{b}"], ["U_d"])
                          chk(1.3)
                          q3 = qs[:].rearrange("p (h d) -> p h d", d=128)
                          A_(lambda e: e.activation(out=q2[:], in_=qs[:], func=AF.Square), ["qs"], ["q2"])
                          V_(lambda e: e.tensor_reduce(hs[:, 0:10], q2[:].rearrange("p (h d) -> p h d", d=128), axis=AX.X, op=ALU.add), ["q2"], ["hs"])
                          A_(lambda e: e.activation(out=hs[:, 0:10], in_=hs[:, 0:10], func=AF.Sqrt, scale=1.0 / 128, bias=epsc[:, 0:1]), ["hs", "epsc"], ["hs"])
                          V_(lambda e: e.reciprocal(hs[:, 0:10], hs[:, 0:10]), ["hs"], ["hs"])
                          V_(lambda e: e.tensor_tensor(q2[:].rearrange("p (h d) -> p h d", d=128), q3, hs[:, 0:10].unsqueeze(2).to_broadcast([128, 10, 128]), op=ALU.mult), ["qs", "hs"], ["q2"])
                          G_(lambda e: e.tensor_tensor(qs[:], q2[:], qkw[:].rearrange("p a b -> p (a b)"), op=ALU.mult), ["q2", "qkw"], ["qs"])
                          chk(1.4)
                          LD(rc[:, b, :], I["ropec"][:, tt, :], w=["rc"]); LD(rs[:, b, :], I["ropes"][:, tt, :], w=["rs"])
                          cb = rc[:, b, :].unsqueeze(1).to_broadcast([128, 10, 64]); sbb = rs[:, b, :].unsqueeze(1).to_broadcast([128, 10, 64])
                          x1v = q3[:, :, 0:64]; x2v = q3[:, :, 64:128]
                          V_(lambda e: e.tensor_tensor(r1[:], x1v, cb, op=ALU.mult), ["qs", "rc"], ["r1"])
                          G_(lambda e: e.tensor_tensor(r2[:], x2v, sbb, op=ALU.mult), ["qs", "rs"], ["r2"])
                          V_(lambda e: e.tensor_tensor(qr[b][:, :, 0:64], r1[:], r2[:], op=ALU.subtract), ["r1", "r2"], [f"qr{b}"])
                          V_(lambda e: e.tensor_tensor(r1[:], x1v, sbb, op=ALU.mult), ["qs", "rs", f"qr{b}"], ["r1"])
                          G_(lambda e: e.tensor_tensor(r2[:], x2v, cb, op=ALU.mult), ["qs", "rc", f"qr{b}"], ["r2"])
                          V_(lambda e: e.tensor_tensor(qr[b][:, :, 64:128], r1[:], r2[:], op=ALU.add), ["r1", "r2"], [f"qr{b}"])
                          chk(1.5)
                          for h in range(10):
                              pbi = 1 if h < 8 else 0
                              off = (h % 8) * 128
                              M_(lambda e: e.transpose(psb[1][:, off:off + 128] if h < 8 else psb[0][:, off:off + 128], qr[b][:, h, :], idb[:]),
                                 [f"qr{b}", "idb"], [PB[pbi]])
                          V_(lambda e: e.tensor_scalar(qT[hb][:, 0:8, ti * 128:(ti + 1) * 128], psb[1][:].rearrange("p (h t) -> p h t", t=128), 1.0, None, op0=ALU.mult), [PB[1]], [f"qT{hb}"])
                          A_(lambda e: e.activation(out=qT[hb][:, 8:10, ti * 128:(ti + 1) * 128], in_=psb[0][:, 0:256].rearrange("p (h t) -> p h t", t=128), func=AF.Identity), [PB[0]], [f"qT{hb}"])
                          chk(1.6)
                      t0 = blk * 512
                      for h in range(8):
                          LD(QT_d[h, :, t0:t0 + ntok], qT[hb][:, h, 0:ntok], [f"qT{hb}"], ["QT_d"])
                      for h in range(2):
                          LD(KT_d[h, :, t0:t0 + ntok], qT[hb][:, 8 + h, 0:ntok], [f"qT{hb}"], ["KT_d"])
                      for dt in range(8):
                          LD(HT_d[dt * 128:(dt + 1) * 128, t0:t0 + ntok], hT[hb][:, dt, 0:ntok], [f"hT{hb}"], ["HT_d"])
              P.barrier()
              if stop == 2:
                  break
              with ExitStack() as es:
                  sb = lambda n, s, d: es.enter_context(nc.sbuf_tensor(uniq(n), list(s), d))
                  wg = sb("wg", [128, 8, 2048], BF16)
                  hT = [sb(f"hT{i}", [128, 8, 512], BF16) for i in range(2)]
                  gsb = [sb(f"gsb{i}", [128, 512], BF16) for i in range(2)]
                  wsrc = I["w_in"][li].rearrange("(kt k) n -> k kt n", k=128)
                  for cgi in range(2):
                      LDC(wg[:, :, cgi * 1024:(cgi + 1) * 1024], wsrc[:, :, 3072 + cgi * 1024:3072 + (cgi + 1) * 1024], w=["wg"])
                  ntg = T if not last else S
                  for blk in range((ntg + 511) // 512):
                      t0 = blk * 512
                      ntok = min(512, ntg - t0)
                      hb = blk % 2
                      LD(hT[hb][:, :, 0:ntok], HT_d[:, t0:t0 + ntok].rearrange("(kt p) t -> p kt t", p=128), w=[f"hT{hb}"])
                      for ncn in range(16):
                          pb = PF[3 + ncn % 2]; pt = psf[3 + ncn % 2]
                          for kt in range(8):
                              M_(lambda e: e.matmul(pt[:, 0:ntok], wg[:, kt, ncn * 128:(ncn + 1) * 128], hT[hb][:, kt, 0:ntok],
                                                    start=(kt == 0), stop=(kt == 7)), [f"hT{hb}", "wg"], [pb])
                          gi = ncn % 2
                          A_(lambda e: e.activation(out=gsb[gi][:, 0:ntok], in_=pt[:, 0:ntok], func=AF.Sigmoid), [pb], [f"gsb{gi}"])
                          LD(G_d[ncn * 128:(ncn + 1) * 128, t0:t0 + ntok], gsb[gi][:, 0:ntok], [f"gsb{gi}"], ["G_d"])
              P.barrier()

              if stop == 3:
                  break
              if do_attn:
                  with ExitStack() as es:
                      sb = lambda n, s, d: es.enter_context(nc.sbuf_tensor(uniq(n), list(s), d))
                      kT = sb("kT", [128, 2, T], BF16); vv = sb("vv", [128, NT, 256], BF16)
                      qb = [sb(f"qb{i}", [128, 512], BF16) for i in range(2)]
                      pT = [sb(f"pT{i}", [128, 512], BF16) for i in range(4)]
                      stb = [psf[0][:], psf[1][:], psb[0][:].bitcast(F32), psb[1][:].bitcast(F32)]
                      stk = [PF[0], PF[1], PB[0], PB[1]]
                      rcp = sb("rcp", [128, 512], F32)
                      yo = [sb(f"yo{i}", [128, 512], BF16) for i in range(2)]
                      for h in range(2):
                          LD(kT[:, h, :], KT_d[h], w=["kT"])
                      LD(vv[:], V_d.rearrange("(t p) c -> p t c", p=128), w=["vv"])
                      scale = 128 ** -0.5
                      it = 0
                      blocks = [(h, q0, 512, 0, NT) for h in range(8) for q0 in range(0, S, 512)]
                      if not last:
                          blocks += [(h, S, 256, 32, NT) for h in range(8)]
                      for bi, (h, q0, nq, k0, k1) in enumerate(blocks):
                          kv = h // 4
                          b = bi % 2
                          LD(qb[b][:, 0:nq], QT_d[h, :, q0:q0 + nq], w=[f"qb{b}"])
                          po = psf[4 + b]; pok = PF[4 + b]
                          psm = psf[2 + b]; psmk = PF[2 + b]
                          def qk_(kt, g):
                              sbk = g % 4
                              M_(lambda e: e.matmul(stb[sbk][:, 0:nq], kT[:, kv, kt * 128:(kt + 1) * 128], qb[b][:, 0:nq], start=True, stop=True),
                                 ["kT", f"qb{b}"], [stk[sbk]])

                          def ex_(kt, g):
                              sbk = g % 4; pb3 = g % 4
                              A_(lambda e: e.activation(out=pT[pb3][:, 0:nq], in_=stb[sbk][:, 0:nq], func=AF.Exp, scale=scale), [stk[sbk]], [f"pT{pb3}"])

                          def pv_(kt, g):
                              pb3 = g % 4
                              M_(lambda e: e.matmul(po[:, 0:nq], vv[:, kt, kv * 128:(kv + 1) * 128], pT[pb3][:, 0:nq], start=(kt == k0), stop=(kt == k1 - 1)),
                                 ["vv", f"pT{pb3}"], [pok])
                              M_(lambda e: e.matmul(psm[:, 0:nq], onesb[:], pT[pb3][:, 0:nq], start=(kt == k0), stop=(kt == k1 - 1)),
                                 ["onesb", f"pT{pb3}"], [psmk])

                          g0 = it
                          it += (k1 - k0)
                          qk_(k0, g0)
                          if k0 + 1 < k1:
                              qk_(k0 + 1, g0 + 1)
                          for kt in range(k0, k1):
                              g = g0 + (kt - k0)
                              if kt + 2 < k1:
                                  qk_(kt + 2, g + 2)
                              ex_(kt, g)
                              pv_(kt, g)
                          V_(lambda e: e.reciprocal(rcp[:, 0:nq], psm[:, 0:nq]), [psmk], ["rcp"])
                          V_(lambda e: e.tensor_tensor(yo[b][:, 0:nq], po[:, 0:nq], rcp[:, 0:nq], op=ALU.mult), [pok, "rcp"], [f"yo{b}"])
                          LD(YA_d[h * 128:(h + 1) * 128, q0:q0 + nq], yo[b][:, 0:nq], [f"yo{b}"], ["YA_d"])
                  P.barrier()

              if stop == 4:
                  break
              if do_hyena:
                  with ExitStack() as es:
                      sb = lambda n, s, d: es.enter_context(nc.sbuf_tensor(uniq(n), list(s), d))
                      cw_ = sb("cw_", [128, 3, 1536], F32); cb_ = sb("cb_", [128, 1536], F32)
                      um = [sb(f"um{i}", [128, 1536], F32) for i in range(2)]
                      u0 = [sb(f"u0{i}", [128, 1536], F32) for i in range(2)]
                      up = [sb(f"up{i}", [128, 1536], F32) for i in range(2)]
                      oc = [sb(f"oc{i}", [128, 1536], F32) for i in range(2)]
                      t3 = sb("t3", [128, 1536], F32)
                      LD(cw_[:].rearrange("p a b -> p (a b)"), I["convw"][li], w=["cw_"]); LD(cb_[:], I["convb"][li], w=["cb_"])
                      tiles = list(range(NT if not last else 32))
                      for tt in tiles:
                          b = tt % 2
                          first = tt in (0, 32); lastt = tt in (31, 33)
                          r0 = tt * 128
                          if first:
                              V_(lambda e: e.memset(um[b][:], 0.0), w=[f"um{b}"])
                              LD(um[b][1:128, :], U_d[r0:r0 + 127, :], w=[f"um{b}"])
                          else:
                              LD(um[b][:], U_d[r0 - 1:r0 + 127, :], w=[f"um{b}"])
                          LD(u0[b][:], U_d[r0:r0 + 128, :], w=[f"u0{b}"])
                          if lastt:
                              V_(lambda e: e.memset(up[b][:], 0.0), w=[f"up{b}"])
                              LD(up[b][0:127, :], U_d[r0 + 1:r0 + 128, :], w=[f"up{b}"])
                          else:
                              LD(up[b][:], U_d[r0 + 1:r0 + 129, :], w=[f"up{b}"])
                          V_(lambda e: e.tensor_tensor(oc[b][:], u0[b][:], cw_[:, 1, :], op=ALU.mult), [f"u0{b}", "cw_"], [f"oc{b}"])
                          G_(lambda e: e.tensor_tensor(t3[:], um[b][:], cw_[:, 0, :], op=ALU.mult), [f"um{b}", "cw_"], ["t3"])
                          V_(lambda e: e.tensor_tensor(oc[b][:], oc[b][:], t3[:], op=ALU.add), [f"oc{b}", "t3"], [f"oc{b}"])
                          G_(lambda e: e.tensor_tensor(t3[:], up[b][:], cw_[:, 2, :], op=ALU.mult), [f"up{b}", "cw_", f"oc{b}"], ["t3"])
                          V_(lambda e: e.tensor_tensor(oc[b][:], oc[b][:], t3[:], op=ALU.add), [f"oc{b}", "t3"], [f"oc{b}"])
                          G_(lambda e: e.tensor_tensor(oc[b][:], oc[b][:], cb_[:], op=ALU.add), [f"oc{b}", "cb_"], [f"oc{b}"])
                          LD(XV_d[r0:r0 + 128, :], oc[b][:], [f"oc{b}"], ["XV_d"])
                  P.barrier()

                  chk(4.1)
                  seqs = [("lat", 0, 64)] + ([] if last else [("ctx", S, 4)])
                  with ExitStack() as es:
                      sb = lambda n, s, d: es.enter_context(nc.sbuf_tensor(uniq(n), list(s), d))
                      w3 = sb("w3", [64, 2048], F32)
                      h2 = sb("h2", [64, 2, 4096], F32)
                      nt_ = sb("nt_", [64, 2, 64], F32); vm_ = sb("vm_", [64, 2, 64], F32); dl = sb("dl", [64, 512], F32)
                      LD(w3[:], I["pe_w3"][li], w=["w3"]); LD(dl[:], I["delta"][:, :], w=["dl"])
                      for si, (sname, tok0, pmax) in enumerate(seqs):
                          LD(nt_[:], I[f"nt_{sname}"].rearrange("d p j -> p d j"), w=["nt_"]); LD(vm_[:], I[f"vm_{sname}"].rearrange("d p j -> p d j"), w=["vm_"])
                          with ExitStack() as es2:
                              sb2 = lambda n, s, d: es2.enter_context(nc.sbuf_tensor(uniq(n), list(s), d))
                              w1 = sb2("w1", [33, 64], F32); w2 = sb2("w2", [64, 64], F32)
                              pv = sb2("pv", [64, 4], F32); pvb = sb2("pvb", [64, 2], F32)
                              zT = sb2("zT", [33, 4096], F32)
                              h1 = sb2("h1", [64, 512], F32); harg = sb2("harg", [64, 512], F32); hw1 = sb2("hw1", [64, 512], F32); hw2 = sb2("hw2", [64, 512], F32)
                              LD(w1[:], I["pe_w1"][li], w=["w1"]); LD(w2[:], I["pe_w2"][li], w=["w2"]); LD(pv[:], I["pe_v"][li], w=["pv"])
                              V_(lambda e: e.tensor_tensor(pvb[:, 0:1], pv[:, 0:1], pv[:, 1:2], op=ALU.mult), ["pv"], ["pvb"])
                              V_(lambda e: e.tensor_tensor(pvb[:, 1:2], pv[:, 2:3], pv[:, 3:4], op=ALU.mult), ["pv"], ["pvb"])

                              def sin_layer(ps, fcol, bcol, out_ap, okey):
                                  A_(lambda e: e.activation(out=harg[:], in_=ps, func=AF.Identity, scale=pv[:, fcol:fcol + 1], bias=pvb[:, bcol:bcol + 1]), [PF[0], "pv", "pvb"], ["harg"])
                                  for _ in range(2):
                                      V_(lambda e: e.tensor_scalar(hw1[:], harg[:], PI, -2 * PI, op0=ALU.is_gt, op1=ALU.mult), ["harg"], ["hw1"])
                                      V_(lambda e: e.tensor_scalar(hw2[:], harg[:], -PI, 2 * PI, op0=ALU.is_lt, op1=ALU.mult), ["harg"], ["hw2"])
                                      V_(lambda e: e.tensor_tensor(hw1[:], hw1[:], hw2[:], op=ALU.add), ["hw1", "hw2"], ["hw1"])
                                      V_(lambda e: e.tensor_tensor(harg[:], harg[:], hw1[:], op=ALU.add), ["harg", "hw1"], ["harg"])
                                  A_(lambda e: e.activation(out=out_ap, in_=harg[:], func=AF.Sin), ["harg"], [okey])

                              for dr in range(2):
                                  LD(zT[:], I[f"z_{sname}"][dr], w=["zT"])
                                  for ck in range(8):
                                      M_(lambda e: e.matmul(psf[0][0:64, :], w1[:], zT[:, ck * 512:(ck + 1) * 512], start=True, stop=True), ["w1", "zT"], [PF[0]])
                                      sin_layer(psf[0][0:64, :], 0, 0, h1[:], "h1")
                                      M_(lambda e: e.matmul(psf[0][0:64, :], w2[:], h1[:], start=True, stop=True), ["w2", "h1"], [PF[0]])
                                      sin_layer(psf[0][0:64, :], 2, 1, h2[:, dr, ck * 512:(ck + 1) * 512], "h2")
                          P.barrier()
                          for o in range(2):
                              with ExitStack() as es2:
                                  sb2 = lambda n, s, d: es2.enter_context(nc.sbuf_tensor(uniq(n), list(s), d))
                                  mlo = sb2("mlo", [64, 64, 2, 128], BF16); mhi = sb2("mhi", [64, 64, 2, 128], BF16)
                                  win = [sb2(f"win{i}", [64, 512], F32) for i in range(2)]
                                  kfb = [sb2(f"kfb{i}", [64, 2, 512], BF16) for i in range(2)]
                                  asb = [sb2(f"asb{i}", [128, 2, 512], BF16) for i in range(2)]
                                  LDC(mlo[:], I["mlo"][:, :, :, :], w=["mlo"]); LDC(mhi[:], I["mhi"][:, :, :, :], w=["mhi"])
                                  for j in range(64):
                                      b = j % 2
                                      for dr in range(2):
                                          A_(lambda e: e.activation(out=win[dr][:], in_=dl[:], func=AF.Exp, scale=nt_[:, dr, j:j + 1]), ["dl", "nt_"], [f"win{dr}"])
                                          M_(lambda e: e.matmul(psf[1 + dr][0:64, :], h2[:, dr, :].rearrange("k (p j) -> k j p", j=64)[:, j, :],
                                                                w3[:, o * 1024 + dr * 512:o * 1024 + (dr + 1) * 512], start=True, stop=True), ["h2", "w3"], [PF[1 + dr]])
                                          V_(lambda e: e.scalar_tensor_tensor(out=kfb[b][:, dr, :], in0=win[dr][:], scalar=vm_[:, dr, j:j + 1], in1=psf[1 + dr][0:64, :], op0=ALU.mult, op1=ALU.mult),
                                             [f"win{dr}", "vm_", PF[1 + dr]], [f"kfb{b}"])
                                      for ri in range(2):
                                          M_(lambda e: e.matmul(psf[3 + ri][:], mlo[:, j, ri, :], kfb[b][:, 0, :], start=True, stop=False), ["mlo", f"kfb{b}"], [PF[3 + ri]])
                                          M_(lambda e: e.matmul(psf[3 + ri][:], mhi[:, j, ri, :], kfb[b][:, 1, :], start=False, stop=True), ["mhi", f"kfb{b}"], [PF[3 + ri]])
                                      A_(lambda e: e.activation(out=asb[b][:, 0, :], in_=psf[3][:], func=AF.Identity), [PF[3]], [f"asb{b}"])
                                      V_(lambda e: e.tensor_scalar(asb[b][:, 1, :], psf[4][:], 1.0, None, op0=ALU.mult), [PF[4]], [f"asb{b}"])
                                      LD(A_d[:, j, :, :], asb[b][:], [f"asb{b}"], ["A_d"])
                              P.barrier()
                              with ExitStack() as es2:
                                  sb2 = lambda n, s, d: es2.enter_context(nc.sbuf_tensor(uniq(n), list(s), d))
                                  w64 = sb2("w64", [64, 8, 128], BF16)
                                  bsb = [sb2(f"bsb{i}", [64, 4, 2, 512], BF16) for i in range(2)]
                                  ksb = [sb2(f"ksb{i}", [128, 2, 512], BF16) for i in range(2)]
                                  LDC(w64[:], I["w64"][:, :, :], w=["w64"])
                                  for fc in range(32):
                                      b = fc % 2
                                      LD(bsb[b][:], A_d[fc * 4:(fc + 1) * 4].rearrange("f j r c -> j f r c"), w=[f"bsb{b}"])
                                      for fl in range(4):
                                          f1i = fc * 4 + fl
                                          kb2 = f1i % 2
                                          for ab in range(2):
                                              pt = psf[ab]; pk = PF[ab]
                                              M_(lambda e: e.matmul(pt[:], w64[:, 4 + 2 * ab, :], bsb[b][:, fl, 0, :], start=True, stop=False), ["w64", f"bsb{b}"], [pk])
                                              M_(lambda e: e.matmul(pt[:], w64[:, 5 + 2 * ab, :], bsb[b][:, fl, 1, :], start=False, stop=True), ["w64", f"bsb{b}"], [pk])
                                          A_(lambda e: e.activation(out=ksb[kb2][:, 0, :], in_=psf[0][:], func=AF.Identity), [PF[0]], [f"ksb{kb2}"])
                                          V_(lambda e: e.tensor_scalar(ksb[kb2][:, 1, :], psf[1][:], 1.0, None, op0=ALU.mult), [PF[1]], [f"ksb{kb2}"])
                                          LD(KH_d[si, o, f1i].rearrange("a r c -> r a c"), ksb[kb2][:], [f"ksb{kb2}"], ["KH_d"])
                              P.barrier()
                  P.barrier()

                  chk(4.2)
                  with ExitStack() as es:
                      sb = lambda n, s, d: es.enter_context(nc.sbuf_tensor(uniq(n), list(s), d))
                      mlo = sb("mlo", [64, 64, 2, 128], BF16); minvT = sb("minvT", [128, 64, 2, 64], BF16)
                      w64 = sb("w64", [64, 4, 128], BF16); wi1 = sb("wi1", [128, 128], BF16)
                      skb = sb("skb", [64, 1024], F32)
                      vz = sb("vz", [64, 64, CW], BF16); zz = sb("zz", [64, 64, CW], BF16)
                      asb = [sb(f"asb{i}", [128, 2, CW], BF16) for i in range(2)]
                      bsb = [sb(f"bsb{i}", [64, 8, 2, CW], BF16) for i in range(2)]
                      ksb = [sb(f"ksb{i}", [128, 2, CW], BF16) for i in range(4)]
                      y1 = [sb(f"y1{i}", [128, CW], F32) for i in range(2)]
                      y2 = [sb(f"y2{i}", [128, CW], F32) for i in range(2)]
                      yb = [sb(f"yb{i}", [128, CW], BF16) for i in range(2)]
                      csb = [sb(f"csb{i}", [128, 8, CW], BF16) for i in range(2)]
                      cj = [sb(f"cj{i}", [128, 2, CW], BF16) for i in range(2)]
                      xg = [sb(f"xg{i}", [64, CW], F32) for i in range(2)]
                      tg = [sb(f"tg{i}", [64, CW], F32) for i in range(2)]
                      yh = [sb(f"yh{i}", [64, CW], BF16) for i in range(2)]
                      LDC(mlo[:], I["mlo"][:, :, :, :], w=["mlo"]); LDC(minvT[:], I["minvT"][:, :, :, :], w=["minvT"])
                      LDC(w64[:], I["w64"][:, 0:4, :], w=["w64"]); LDC(wi1[:], I["wi1"][:, :], w=["wi1"])
                      LD(skb[:], I["skipb"][li], w=["skb"])
                      skp = sb("skp", [64, CW], F32)
                      passes = [[(0, 0, 64, k * 256, 256, 0)] for k in range(2)]
                      if not last:
                          passes += [[(1, S, 4, k * 256, 256, 0)] for k in range(2)]
                      for segs in passes:
                          if True:
                              for b in range(2):
                                  V_(lambda e: e.memset(xg[b][:], 0.0), w=[f"xg{b}"])
                              if segs[0][2] < 64:
                                  V_(lambda e: e.memset(vz[:], 0.0), w=["vz"])
                              for (si, tok0, pmax, c0, w_, off) in segs:
                                  LDC(vz[0:pmax, :, off:off + w_], XV_d[tok0:tok0 + pmax * 64, 1024 + c0:1024 + c0 + w_].rearrange("(p j) c -> p j c", j=64), w=["vz"])
                              for o in range(2):
                                  src = vz if o == 0 else zz
                                  skey = "vz" if o == 0 else "zz"
                                  for j in range(64):
                                      b = j % 2
                                      for ri in range(2):
                                          M_(lambda e: e.matmul(psf[ri + 2 * b][:, 0:CW], mlo[:, j, ri, :], src[:, j, :], start=True, stop=True), ["mlo", skey], [PF[ri + 2 * b]])
                                      A_(lambda e: e.activation(out=asb[b][:, 0, :], in_=psf[2 * b][:, 0:CW], func=AF.Identity), [PF[2 * b]], [f"asbr{b}"])
                                      V_(lambda e: e.tensor_scalar(asb[b][:, 1, :], psf[1 + 2 * b][:, 0:CW], 1.0, None, op0=ALU.mult), [PF[1 + 2 * b]], [f"asbi{b}"])
                                      LD(A2_d[:, j, :, :], asb[b][:], [f"asbr{b}", f"asbi{b}"], ["A_d"])
                                  def s3_mm(f1i):
                                      fc, fl = divmod(f1i, 8)
                                      b = fc % 2; par = f1i % 2; k3 = f1i % 4
                                      if fl == 0:
                                          LD(bsb[b][:].rearrange("j f r c -> j f (r c)"), A2_d[fc * 8:(fc + 1) * 8].rearrange("f j r c -> j f (r c)"), w=[f"bsb{b}"])
                                      for (si, tok0, pmax, c0, w_, off) in segs:
                                          LD(ksb[k3][:, :, off:off + w_], KH_d[si, o, f1i, :, :, c0:c0 + w_].rearrange("a r c -> r a c"), w=[f"ksb{k3}"])
                                      for pq in range(2):
                                          pi = pq + 2 * par
                                          M_(lambda e: e.matmul(psf[pi][:, 0:CW], w64[:, 2 * pq, :], bsb[b][:, fl, 0, :], start=True, stop=False), ["w64", f"bsb{b}"], [PF[pi]])
                                          M_(lambda e: e.matmul(psf[pi][:, 0:CW], w64[:, 2 * pq + 1, :], bsb[b][:, fl, 1, :], start=False, stop=True), ["w64", f"bsb{b}"], [PF[pi]])

                                  def s3_post(f1i):
                                      fc, fl = divmod(f1i, 8)
                                      b = fc % 2; par = f1i % 2; k3 = f1i % 4; b2 = par
                                      V_(lambda e: e.tensor_tensor(y1[b2][:], psf[2 * par][:, 0:CW], ksb[k3][:, 0, :], op=ALU.mult), [PF[2 * par], f"ksb{k3}"], [f"y1{b2}"])
                                      V_(lambda e: e.tensor_tensor(y2[b2][:], psf[1 + 2 * par][:, 0:CW], ksb[k3][:, 1, :], op=ALU.mult), [PF[1 + 2 * par], f"ksb{k3}"], [f"y2{b2}"])
                                      V_(lambda e: e.tensor_tensor(yb[b2][:], y1[b2][:], y2[b2][:], op=ALU.add), [f"y1{b2}", f"y2{b2}"], [f"yb{b2}"])
                                      M_(lambda e: e.matmul(psf[4 + par][:, 0:CW], wi1[:], yb[b2][:], start=True, stop=True), ["wi1", f"yb{b2}"], [PF[4 + par]])
                                      A_(lambda e: e.activation(out=csb[b][:, fl, :], in_=psf[4 + par][:, 0:CW], func=AF.Identity), [PF[4 + par]], [f"csb{b}"])
                                      if fl == 7:
                                          LD(C_d[:, fc * 8:(fc + 1) * 8, :], csb[b][:], [f"csb{b}"], ["C_d"])

                                  P.barrier()
                                  s3_mm(0)
                                  for f1i in range(128):
                                      if f1i + 1 < 128:
                                          s3_mm(f1i + 1)
                                      s3_post(f1i)
                                  P.barrier()
                                  Cv = C_d.rearrange("(r j) f c -> j f r c", j=64)
                                  for (si, tok0, pmax, c0, w_, off) in segs:
                                      V_(lambda e: e.tensor_scalar(skp[:, off:off + w_], skb[:, o * 512 + c0:o * 512 + c0 + w_], 1.0, None, op0=ALU.mult), ["skb"], ["skp"])
                                  for j in range(64):
                                      b = j % 2
                                      LD(cj[b][:], Cv[j], w=[f"cj{b}"])
                                      M_(lambda e: e.matmul(psf[4 + b][0:64, 0:CW], minvT[:, j, 0, :], cj[b][:, 0, :], start=True, stop=False), ["minvT", f"cj{b}"], [PF[4 + b]])
                                      M_(lambda e: e.matmul(psf[4 + b][0:64, 0:CW], minvT[:, j, 1, :], cj[b][:, 1, :], start=False, stop=True), ["minvT", f"cj{b}"], [PF[4 + b]])
                                      for (si, tok0, pmax, c0, w_, off) in segs:
                                          xcol = (0 if o == 0 else 512) + c0
                                          LD(xg[b][0:pmax, off:off + w_], XV_d[tok0:tok0 + pmax * 64, xcol:xcol + w_].rearrange("(p j) c -> p j c", j=64)[:, j, :], w=[f"xg{b}"])
                                      G_(lambda e: e.tensor_tensor(tg[b][:], src[:, j, :], skp[:], op=ALU.mult), [skey, "skp"], [f"tg{b}"])
                                      V_(lambda e: e.tensor_tensor(tg[b][:], psf[4 + b][0:64, 0:CW], tg[b][:], op=ALU.add), [PF[4 + b], f"tg{b}"], [f"tg{b}"])
                                      if o == 0:
                                          V_(lambda e: e.tensor_tensor(zz[:, j, :], tg[b][:], xg[b][:], op=ALU.mult), [f"tg{b}", f"xg{b}"], ["zz"])
                                      else:
                                          V_(lambda e: e.tensor_tensor(yh[b][:], tg[b][:], xg[b][:], op=ALU.mult), [f"tg{b}", f"xg{b}"], [f"yh{b}"])
                                          for (si, tok0, pmax, c0, w_, off) in segs:
                                              LD(YH_d[tok0:tok0 + pmax * 64, c0:c0 + w_].rearrange("(p j) c -> p j c", j=64)[:, j, :], yh[b][0:pmax, off:off + w_], [f"yh{b}"], ["YH_d"])
                                  P.barrier()
                  P.barrier()

              if stop == 5:
                  break
              with ExitStack() as es:
                  sb = lambda n, s, d: es.enter_context(nc.sbuf_tensor(uniq(n), list(s), d))
                  wap = sb("wap", [128, 8, D], BF16); whp = sb("whp", [128, 4, D], BF16); wo = sb("wo", [128, 8, D], BF16)
                  rw = sb("rw", [128, 8, NE], F32); rbb = sb("rbb", [128, NE], F32)
                  ya = [sb(f"ya{i}", [128, 8, 512], BF16) for i in range(2)]
                  yht = [sb(f"yht{i}", [128, 512], BF16) for i in range(2)]
                  yhT = sb("yhT", [128, 4, 512], BF16)
                  gt = [sb("gt0", [128, 16, 512], BF16)] * 2
                  mT = sb("mT", [128, 8, 512], BF16)
                  m1 = [sb(f"m1{i}", [128, 512], F32) for i in range(2)]
                  m2 = [sb(f"m2{i}", [128, 512], F32) for i in range(2)]
                  xt = [sb(f"xt{i}", [128, D], F32) for i in range(2)]
                  xo = [sb(f"xo{i}", [128, D], F32) for i in range(2)]
                  junk = sb("junk", [128, D], BF16)
                  st = [sb(f"st{i}", [128, 4], F32) for i in range(2)]
                  xn = [sb(f"xn{i}", [128, D], F32) for i in range(2)]
                  h32 = sb("h32", [128, 8, 128], F32)
                  h2b = sb("h2b", [128, 8, 512], BF16)
                  lg = sb("lg", [128, NE], F32); sc_ = sb("sc_", [128, NE], F32); bi_ = sb("bi_", [128, NE], F32)
                  t8 = sb("t8", [128, 8, 8], F32); gs = sb("gs", [128, 8], F32); gm = sb("gm", [128, 8], F32)
                  sm = sb("sm", [128, 4], F32); em = sb("em", [128, NE], F32); gate = [sb(f"gate{i}", [128, NE], F32) for i in range(2)]
                  LDC(wap[:], I["w_ap"][li].rearrange("(kt k) n -> k kt n", k=128), w=["wap"])
                  LDC(whp[:], I["w_hp"][li].rearrange("(kt k) n -> k kt n", k=128), w=["whp"])
                  LDC(wo[:], I["w_out"][li].rearrange("(kt k) n -> k kt n", k=128), w=["wo"])
                  LD(rw[:], I["router_w"].rearrange("(kt k) n -> k kt n", k=128), w=["rw"]); LD(rbb[:], I["router_bb"][:, :], w=["rbb"])
                  ntiles = NT if not last else 32
                  nblk = (ntiles * 128 + 511) // 512
                  for blk in range(nblk):
                      tts = list(range(blk * 4, min(blk * 4 + 4, ntiles)))
                      ntok = len(tts) * 128
                      t0 = blk * 512
                      b = blk % 2
                      LD(ya[b][:, :, 0:ntok], YA_d[:, t0:t0 + ntok].rearrange("(h p) t -> p h t", p=128), w=[f"ya{b}"])
                      LD(gt[b][:, :, 0:ntok], G_d[:, t0:t0 + ntok].rearrange("(h p) t -> p h t", p=128), w=["gt"])
                      for ti, tt in enumerate(tts):
                          yb_ = tt % 2
                          LD(yht[yb_][:], YH_d[tt * 128:(tt + 1) * 128, :], w=[f"yht{yb_}"])
                          for ct in range(4):
                              M_(lambda e: e.transpose(psb[0][:, ct * 128:(ct + 1) * 128], yht[yb_][:, ct * 128:(ct + 1) * 128], idb[:]), [f"yht{yb_}", "idb"], [PB[0]])
                          A_(lambda e: e.activation(out=yhT[:, :, ti * 128:(ti + 1) * 128], in_=psb[0][:, 0:512].rearrange("p (c t) -> p c t", t=128), func=AF.Identity), [PB[0]], ["yhT"])
                      for ncn in range(8):
                          pa = psf[ncn % 2]; pak = PF[ncn % 2]; ph = psf[2 + ncn % 2]; phk = PF[2 + ncn % 2]
                          mb = ncn % 2
                          for kt in range(8):
                              M_(lambda e: e.matmul(pa[:, 0:ntok], wap[:, kt, ncn * 128:(ncn + 1) * 128], ya[b][:, kt, 0:ntok], start=(kt == 0), stop=(kt == 7)), ["wap", f"ya{b}"], [pak])
                          for ct in range(4):
                              M_(lambda e: e.matmul(ph[:, 0:ntok], whp[:, ct, ncn * 128:(ncn + 1) * 128], yhT[:, ct, 0:ntok], start=(ct == 0), stop=(ct == 3)), ["whp", "yhT"], [phk])
                          V_(lambda e: e.tensor_tensor(m1[mb][:, 0:ntok], pa[:, 0:ntok], gt[b][:, ncn, 0:ntok], op=ALU.mult), [pak, "gt"], [f"m1{mb}"])
                          V_(lambda e: e.tensor_tensor(m2[mb][:, 0:ntok], ph[:, 0:ntok], gt[b][:, 8 + ncn, 0:ntok], op=ALU.mult), [phk, "gt"], [f"m2{mb}"])
                          G_(lambda e: e.tensor_tensor(mT[:, ncn, 0:ntok], m1[mb][:, 0:ntok], m2[mb][:, 0:ntok], op=ALU.add), [f"m1{mb}", f"m2{mb}"], ["mT"])
                      for ti, tt in enumerate(tts):
                          m = 0 if tt < 32 else 1
                          xb_ = tt % 2
                          LD(xt[xb_][:], cur[tt * 128:(tt + 1) * 128, :], w=[f"xt{xb_}"])
                          for hf in range(2):
                              po = psf[4 + hf]; pok = PF[4 + hf]
                              for kt in range(8):
                                  M_(lambda e: e.matmul(po[:], mT[:, kt, ti * 128:(ti + 1) * 128], wo[:, kt, hf * 512:(hf + 1) * 512], start=(kt == 0), stop=(kt == 7)), ["mT", "wo"], [pok])
                              V_(lambda e: e.tensor_tensor(xo[xb_][:, hf * 512:(hf + 1) * 512], po[:], gb[:, m, hf * 512:(hf + 1) * 512], op=ALU.mult), [pok, "gb"], [f"xo{xb_}"])
                          G_(lambda e: e.tensor_tensor(xo[xb_][:], xo[xb_][:], xt[xb_][:], op=ALU.add), [f"xo{xb_}", f"xt{xb_}"], [f"xo{xb_}"])
                          LD(mid[tt * 128:(tt + 1) * 128, :], xo[xb_][:], [f"xo{xb_}"], ["mid"])
                          A_(lambda e: e.activation(out=junk[:], in_=xo[xb_][:], func=AF.Square, accum_out=st[xb_][:, 0:1]), [f"xo{xb_}"], ["junk", f"st{xb_}"])
                          A_(lambda e: e.activation(out=st[xb_][:, 1:2], in_=st[xb_][:, 0:1], func=AF.Sqrt, scale=1.0 / D, bias=epsc[:, 0:1]), [f"st{xb_}", "epsc"], [f"st{xb_}"])
                          V_(lambda e: e.reciprocal(st[xb_][:, 2:3], st[xb_][:, 1:2]), [f"st{xb_}"], [f"st{xb_}"])
                          V_(lambda e: e.tensor_scalar(xn[xb_][:], xo[xb_][:], st[xb_][:, 2:3], None, op0=ALU.mult), [f"xo{xb_}", f"st{xb_}"], [f"xn{xb_}"])
                          for dt in range(8):
                              M_(lambda e: e.transpose(psf[dt // 4][:, (dt % 4) * 128:(dt % 4 + 1) * 128], xn[xb_][:, dt * 128:(dt + 1) * 128], idf[:]), [f"xn{xb_}", "idf"], [PF[dt // 4]])
                          for dt in range(8):
                              A_(lambda e: e.activation(out=h32[:, dt, :], in_=psf[dt // 4][:, (dt % 4) * 128:(dt % 4 + 1) * 128], func=AF.Identity,
                                                        scale=scB[:, dt, m:m + 1], bias=mods[:, 24 + dt, m:m + 1]), [PF[dt // 4], "scB", "mods"], ["h32"])
                          G_(lambda e: e.tensor_scalar(h2b[:, :, ti * 128:(ti + 1) * 128], h32[:], 1.0, None, op0=ALU.mult), ["h32"], ["h2b"])
                          for dt in range(8):
                              M_(lambda e: e.matmul(psf[2][:, 0:NE], h32[:, dt, :], rw[:, dt, :], start=(dt == 0), stop=(dt == 7)), ["h32", "rw"], [PF[2]])
                          gb_ = tt % 2
                          A_(lambda e: e.activation(out=sc_[:], in_=psf[2][:, 0:NE], func=AF.Sigmoid), [PF[2]], ["sc_"])
                          V_(lambda e: e.tensor_tensor(bi_[:], sc_[:], rbb[:], op=ALU.add), ["sc_", "rbb"], ["bi_"])
                          for g in range(8):
                              V_(lambda e: e.max(out=t8[:, g, :], in_=bi_[:, g * 8:(g + 1) * 8]), ["bi_"], ["t8"])
                          V_(lambda e: e.tensor_tensor(gs[:], t8[:, :, 0], t8[:, :, 1], op=ALU.add), ["t8"], ["gs"])
                          V_(lambda e: e.tensor_reduce(sm[:, 0:1], gs[:], axis=AX.X, op=ALU.max), ["gs"], ["sm"])
                          V_(lambda e: e.tensor_scalar(gm[:], gs[:], sm[:, 0:1], None, op0=ALU.is_equal), ["gs", "sm"], ["gm"])
                          V_(lambda e: e.tensor_tensor(gs[:], gm[:], t8[:, :, 1], op=ALU.mult), ["gm", "t8"], ["gs"])
                          V_(lambda e: e.tensor_reduce(sm[:, 1:2], gs[:], axis=AX.X, op=ALU.add), ["gs"], ["sm"])
                          V_(lambda e: e.tensor_scalar(em[:], bi_[:], sm[:, 1:2], None, op0=ALU.is_ge), ["bi_", "sm"], ["em"])
                          V_(lambda e: e.tensor_tensor(em[:].rearrange("p (g k) -> p g k", k=8), em[:].rearrange("p (g k) -> p g k", k=8),
                                                       gm[:].unsqueeze(2).to_broadcast([128, 8, 8]), op=ALU.mult), ["em", "gm"], ["em"])
                          V_(lambda e: e.tensor_tensor(em[:], em[:], sc_[:], op=ALU.mult), ["em", "sc_"], ["em"])
                          V_(lambda e: e.tensor_reduce(sm[:, 2:3], em[:], axis=AX.X, op=ALU.add), ["em"], ["sm"])
                          V_(lambda e: e.reciprocal(sm[:, 3:4], sm[:, 2:3]), ["sm"], ["sm"])
                          V_(lambda e: e.tensor_scalar(gate[gb_][:], em[:], sm[:, 3:4], None, op0=ALU.mult), ["em", "sm"], [f"gate{gb_}"])
                          LD(GATE_d[tt * 128:(tt + 1) * 128, :], gate[gb_][:], [f"gate{gb_}"], ["GATE_d"])
                      for dt in range(8):
                          LD(H2T_d[dt * 128:(dt + 1) * 128, t0:t0 + ntok], h2b[:, dt, 0:ntok], ["h2b"], ["H2T_d"])
              P.barrier()

              if stop == 6:
                  break
              ntiles = NT if not last else 32
              with ExitStack() as es:
                  sb = lambda n, s, d: es.enter_context(nc.sbuf_tensor(uniq(n), list(s), d))
                  SBK = 1024
                  hT2 = sb("hT2", [128, 8, SBK], BF16)
                  acc = sb("acc", [128, SBK // 128, D], F32)
                  gat = sb("gat", [128, SBK // 128, NE], F32)
                  w1 = [sb(f"ew1{i}", [128, 8, 512], BF16) for i in range(2)]
                  w3 = [sb(f"ew3{i}", [128, 8, 512], BF16) for i in range(2)]
                  w2 = [sb(f"ew2{i}", [128, 4, D], BF16) for i in range(2)]
                  s1 = [sb(f"s1{i}", [128, 512], F32) for i in range(2)]
                  gT = [sb(f"gT{i}", [128, 4, 512], BF16) for i in range(2)]
                  xt = [sb(f"xt{i}", [128, D], F32) for i in range(2)]
                  xo = [sb(f"xo{i}", [128, D], F32) for i in range(2)]
                  junk = sb("junk", [128, D], BF16); st = [sb(f"st{i}", [128, 4], F32) for i in range(2)]
                  fnw = sb("fnw", [128, D], F32)
                  LD(fnw[:], I["fnw"][:, :], w=["fnw"])
                  ntok_all = ntiles * 128
                  for s0 in range(0, ntok_all, SBK):
                      sn = min(SBK, ntok_all - s0)
                      stl = sn // 128
                      LD(hT2[:, :, 0:sn], H2T_d[:, s0:s0 + sn].rearrange("(kt p) t -> p kt t", p=128), w=["hT2"])
                      LD(gat[:, 0:stl, :], GATE_d[s0:s0 + sn, :].rearrange("(t p) e -> p t e", p=128), w=["gat"])
                      V_(lambda e: e.memset(acc[:], 0.0), w=["acc"])
                      if do_moe:
                          blist = [(ex, b0, min(512, sn - b0)) for ex in range(NE) for b0 in range(0, sn, 512)]

                          def moe_a(bi):
                              ex, b0, bn = blist[bi]
                              wb = ex % 2; gbi = bi % 2
                              if b0 == 0:
                                  LDC(w1[wb][:], I["exp_w1"][li, ex].rearrange("(kt k) n -> k kt n", k=128), w=[f"ew1{wb}"])
                                  LDC(w3[wb][:], I["exp_w3"][li, ex].rearrange("(kt k) n -> k kt n", k=128), w=[f"ew3{wb}"])
                                  LDC(w2[wb][:], I["exp_w2"][li, ex].rearrange("(kt k) n -> k kt n", k=128), w=[f"ew2{wb}"])
                              for fcn in range(4):
                                  p1 = psf[fcn % 2]; p1k = PF[fcn % 2]; p3 = psf[2 + fcn % 2]; p3k = PF[2 + fcn % 2]
                                  sbi = fcn % 2
                                  for kt in range(8):
                                      M_(lambda e: e.matmul(p1[:, 0:bn], w1[wb][:, kt, fcn * 128:(fcn + 1) * 128], hT2[:, kt, b0:b0 + bn], start=(kt == 0), stop=(kt == 7)), [f"ew1{wb}", "hT2"], [p1k])
                                  for kt in range(8):
                                      M_(lambda e: e.matmul(p3[:, 0:bn], w3[wb][:, kt, fcn * 128:(fcn + 1) * 128], hT2[:, kt, b0:b0 + bn], start=(kt == 0), stop=(kt == 7)), [f"ew3{wb}", "hT2"], [p3k])
                                  A_(lambda e: e.activation(out=s1[sbi][:, 0:bn], in_=p1[:, 0:bn], func=AF.Silu), [p1k], [f"s1{sbi}"])
                                  V_(lambda e: e.tensor_tensor(gT[gbi][:, fcn, 0:bn], p3[:, 0:bn], s1[sbi][:, 0:bn], op=ALU.mult), [p3k, f"s1{sbi}"], [f"gT{gbi}"])

                          def moe_b(bi):
                              ex, b0, bn = blist[bi]
                              wb = ex % 2; gbi = bi % 2
                              for ti in range(bn // 128):
                                  tl = (b0 // 128) + ti
                                  for hf in range(2):
                                      po = psf[4 + hf]; pok = PF[4 + hf]
                                      for ft in range(4):
                                          M_(lambda e: e.matmul(po[:], gT[gbi][:, ft, ti * 128:(ti + 1) * 128], w2[wb][:, ft, hf * 512:(hf + 1) * 512], start=(ft == 0), stop=(ft == 3)), [f"gT{gbi}", f"ew2{wb}"], [pok])
                                      V_(lambda e: e.scalar_tensor_tensor(out=acc[:, tl, hf * 512:(hf + 1) * 512], in0=po[:], scalar=gat[:, tl, ex:ex + 1],
                                                                          in1=acc[:, tl, hf * 512:(hf + 1) * 512], op0=ALU.mult, op1=ALU.add), [pok, "gat", "acc"], ["acc"])

                          moe_a(0)
                          for bi in range(len(blist)):
                              if bi + 1 < len(blist):
                                  moe_a(bi + 1)
                              moe_b(bi)
                      for tl in range(stl):
                          tt = s0 // 128 + tl
                          m = 0 if tt < 32 else 1
                          xb_ = tt % 2
                          LD(xt[xb_][:], mid[tt * 128:(tt + 1) * 128, :], w=[f"xt{xb_}"])
                          V_(lambda e: e.tensor_tensor(xo[xb_][:], acc[:, tl, :], gb[:, 2 + m, :], op=ALU.mult), ["acc", "gb"], [f"xo{xb_}"])
                          G_(lambda e: e.tensor_tensor(xo[xb_][:], xo[xb_][:], xt[xb_][:], op=ALU.add), [f"xo{xb_}", f"xt{xb_}"], [f"xo{xb_}"])
                          if not last:
                              LD(cur[tt * 128:(tt + 1) * 128, :], xo[xb_][:], [f"xo{xb_}"], ["cur"])
                          else:
                              A_(lambda e: e.activation(out=junk[:], in_=xo[xb_][:], func=AF.Square, accum_out=st[xb_][:, 0:1]), [f"xo{xb_}"], ["junk", f"st{xb_}"])
                              A_(lambda e: e.activation(out=st[xb_][:, 1:2], in_=st[xb_][:, 0:1], func=AF.Sqrt, scale=1.0 / D, bias=epsc[:, 0:1]), [f"st{xb_}", "epsc"], [f"st{xb_}"])
                              V_(lambda e: e.reciprocal(st[xb_][:, 2:3], st[xb_][:, 1:2]), [f"st{xb_}"], [f"st{xb_}"])
                              V_(lambda e: e.scalar_tensor_tensor(out=xt[xb_][:], in0=xo[xb_][:], scalar=st[xb_][:, 2:3], in1=fnw[:], op0=ALU.mult, op1=ALU.mult),
                                 [f"xo{xb_}", f"st{xb_}", "fnw"], [f"xt{xb_}"])
                              LD(OUT[tt * 128:(tt + 1) * 128, :], xt[xb_][:], [f"xt{xb_}"], ["OUT"])
              P.barrier()
          except _Stop:
              break
        P.dead = False
        P.barrier()
        nops = P.nops
    return nc, nops


def make_in_maps(inputs, cores=range(8)):
    f = lambda a: np.ascontiguousarray(np.asarray(a, dtype=np.float32))
    consts = _consts()
    shared = {}
    shared["w_ada"] = f(inputs["w_ada"])
    shared["b_adaT"] = f(np.asarray(inputs["b_ada"]).reshape(2, 48, 128).transpose(0, 2, 1))
    shared["n1T"] = f(np.asarray(inputs["norm1_w"]).reshape(2, 8, 128).transpose(0, 2, 1))
    shared["n2T"] = f(np.asarray(inputs["norm2_w"]).reshape(2, 8, 128).transpose(0, 2, 1))
    shared["w_in"] = f(inputs["w_in"])
    qw = np.asarray(inputs["q_norm_w"]); kw = np.asarray(inputs["k_norm_w"])
    qkw = np.concatenate([np.tile(qw, (1, 8)), np.tile(kw, (1, 2))], 1)
    shared["qkw"] = f(np.broadcast_to(qkw[:, None, :], (2, 128, 1280)))
    cw = np.asarray(inputs["hy_conv_w"]).reshape(2, 3 * 1536)
    shared["convw"] = f(np.broadcast_to(cw[:, None, :], (2, 128, 3 * 1536)))
    shared["convb"] = f(np.broadcast_to(np.asarray(inputs["hy_conv_b"])[:, None, :], (2, 128, 1536)))
    shared["pe_w1"] = f(inputs["hy_pe_w1"]); shared["pe_w2"] = f(inputs["hy_pe_w2"]); shared["pe_w3"] = f(inputs["hy_pe_w3"])
    shared["pe_v"] = f(np.stack([np.asarray(inputs["hy_freq1"]), np.asarray(inputs["hy_pe_b1"]),
                                 np.asarray(inputs["hy_freq2"]), np.asarray(inputs["hy_pe_b2"])], -1))
    sk = np.asarray(inputs["hy_skip"]).reshape(2, 1024)
    shared["skipb"] = f(np.broadcast_to(sk[:, None, :], (2, 64, 1024)))
    shared["w_ap"] = f(inputs["w_att_proj"]); shared["w_hp"] = f(inputs["w_hy_proj"]); shared["w_out"] = f(inputs["w_out"])
    shared["router_w"] = f(inputs["router_w"])
    shared["router_bb"] = f(np.broadcast_to(np.asarray(inputs["router_b"])[None, :], (128, NE)))
    shared["exp_w1"] = f(inputs["exp_w1"]); shared["exp_w3"] = f(inputs["exp_w3"]); shared["exp_w2"] = f(inputs["exp_w2"])
    shared["fnw"] = f(np.broadcast_to(np.asarray(inputs["final_norm_w"])[None, :], (128, D)))
    for k, v in consts.items():
        shared["c_" + k] = f(v)
    x = np.asarray(inputs["x"]); c = np.asarray(inputs["c"]); ctx = np.asarray(inputs["ctx"]); c_ctx = np.asarray(inputs["c_ctx"])
    maps = []
    for b in cores:
        m = dict(shared)
        m["x"] = f(x[b]); m["ctx"] = f(ctx[b])
        cc = np.stack([c[b].reshape(8, 128).T, c_ctx.reshape(8, 128).T], -1)
        m["cc"] = f(cc)
        maps.append(m)
    return maps


_NC_CACHE = {}


def kernel(**inputs):
    if "nc" not in _NC_CACHE:
        _NC_CACHE["nc"] = build()[0]
    nc = _NC_CACHE["nc"]
    maps = make_in_maps(inputs)
    res = run_bass_kernel_spmd(nc, maps, core_ids=list(range(8)))
    out = np.stack([np.asarray(r["out"]) for r in res.results], 0).astype(np.float32)
    return out
```

```python
import math
import numpy as np
import concourse.bass as bass
import concourse.mybir as mybir
from concourse.bass_utils import run_bass_kernel_spmd
from contextlib import ExitStack

F32 = mybir.dt.float32
BF16 = mybir.dt.bfloat16
AF = mybir.ActivationFunctionType
ALU = mybir.AluOpType
AX = mybir.AxisListType

S = 4096
C = 256
T = S + C
NT = T // 128
D = 1024
NE = 64
EPS = 1e-6
NFFT = 8192
CW = 256
PI = float(np.pi)


class _Stop(Exception):
    pass


class Prog:
    EPOCH = 30000
    NDMA = 24

    def __init__(self, nc, es):
        self.nc = nc
        self.es = es
        self.eng = {"pe": nc.tensor, "act": nc.scalar, "dve": nc.vector, "pool": nc.gpsimd, "sp": nc.sync}
        self.sems = {e: [es.enter_context(nc.semaphore(f"s_{e}_0"))] for e in self.eng}
        self.cnt = {e: 0 for e in self.eng}
        self.ep = {e: 0 for e in self.eng}
        self.dsem = [es.enter_context(nc.semaphore(f"s_dma_{i}")) for i in range(self.NDMA)]
        self.dcnt = [0] * self.NDMA
        self.dnext = 0
        self.waited = {e: {} for e in self.eng}
        self.W = {}
        self.R = {}
        self.nops = 0
        self.dead = False

    def _wait(self, e, tok):
        sem, val, src = tok
        if src == e and e == "pe":
            return
        w = self.waited[e]
        k = id(sem)
        if w.get(k, 0) >= val:
            return
        self.eng[e].wait_ge(sem, val)
        w[k] = val

    def _deps(self, reads, writes):
        toks = []
        for k in reads:
            toks.extend(self.W.get(k, {}).values())
        for k in writes:
            toks.extend(self.W.get(k, {}).values())
            toks.extend(self.R.get(k, {}).values())
        return toks

    def _commit(self, tok, reads, writes):
        sid = id(tok[0])
        for k in reads:
            d = self.R.setdefault(k, {})
            if sid not in d or d[sid][1] < tok[1]:
                d[sid] = tok
        for k in writes:
            d = self.W.setdefault(k, {})
            if sid not in d or d[sid][1] < tok[1]:
                d[sid] = tok

    def op(self, e, fn, reads=(), writes=()):
        if self.dead:
            return None
        for t in self._deps(reads, writes):
            self._wait(e, t)
        if self.cnt[e] >= self.EPOCH:
            self.ep[e] += 1
            self.sems[e].append(self.es.enter_context(self.nc.semaphore(f"s_{e}_{self.ep[e]}")))
            self.cnt[e] = 0
        inst = fn(self.eng[e])
        self.cnt[e] += 1
        sem = self.sems[e][-1]
        inst.then_inc(sem, 1)
        tok = (sem, self.cnt[e], e)
        self._commit(tok, reads, writes)
        self.nops += 1
        return tok

    def dma(self, q, fn, reads=(), writes=()):
        if self.dead:
            return None
        for t in self._deps(reads, writes):
            self._wait(q, t)
        i = self.dnext
        self.dnext = (self.dnext + 1) % self.NDMA
        sem = self.dsem[i]
        if self.dcnt[i] > 0:
            self._wait(q, (sem, 16 * self.dcnt[i], None))
        inst = fn(self.eng[q])
        self.dcnt[i] += 1
        inst.then_inc(sem, 16)
        tok = (sem, 16 * self.dcnt[i], None)
        self._commit(tok, reads, writes)
        self.nops += 1
        return tok

    def barrier(self):
        if self.dead:
            return
        toks = []
        for e in self.eng:
            if self.cnt[e] > 0:
                toks.append((self.sems[e][-1], self.cnt[e], e))
        for i in range(self.NDMA):
            if self.dcnt[i] > 0:
                toks.append((self.dsem[i], 16 * self.dcnt[i], None))
        for e in self.eng:
            for t in toks:
                if t[2] != e:
                    self._wait(e, t)
        self.W = {}
        self.R = {}


def _consts():
    c = {}
    c["ident"] = np.eye(128, dtype=np.float32)
    t = np.arange(S)
    row = (t // 64).astype(np.float32)
    col = (t % 64).astype(np.float32)
    n = 32
    inv = (10000.0 ** (-np.arange(n, dtype=np.float32) / n)).astype(np.float32)
    ang = np.concatenate([row[:, None] * inv, col[:, None] * inv], -1).astype(np.float32)
    cs = np.ones((T, 64), np.float32)
    sn = np.zeros((T, 64), np.float32)
    cs[:S] = np.cos(ang)
    sn[:S] = np.sin(ang)
    c["ropec"] = np.ascontiguousarray(cs.reshape(NT, 128, 64).transpose(1, 0, 2))
    c["ropes"] = np.ascontiguousarray(sn.reshape(NT, 128, 64).transpose(1, 0, 2))
    p = np.arange(128, dtype=np.float64)[:, None, None]
    j = np.arange(64, dtype=np.float64)[None, :, None]
    f1 = np.arange(128, dtype=np.float64)[None, None, :]
    M = np.exp(-2j * np.pi * (p * f1 / 128.0 + j * f1 / NFFT))
    Mri = np.stack([M.real, M.imag], 2)
    c["mlo"] = np.ascontiguousarray(Mri[:64]).astype(np.float32)
    c["mhi"] = np.ascontiguousarray(Mri[64:]).astype(np.float32)
    Mi = np.stack([M.real[:64], M.imag[:64]], 0) / NFFT
    c["minvT"] = np.ascontiguousarray(Mi.transpose(3, 2, 0, 1)).astype(np.float32)
    jj = np.arange(64, dtype=np.float64)[:, None]
    f2 = np.arange(64, dtype=np.float64)[None, :]
    Wre = np.cos(2 * np.pi * jj * f2 / 64.0)
    Wim = -np.sin(2 * np.pi * jj * f2 / 64.0)
    cat = lambda a, b: np.concatenate([a, b], 1)
    st = [cat(Wre, Wim), cat(-Wim, Wre), cat(Wim, Wre), cat(Wre, -Wim),
          cat(Wre, Wre), cat(-Wim, -Wim), cat(-Wim, Wim), cat(-Wre, Wre)]
    c["w64"] = np.ascontiguousarray(np.stack(st, 1)).astype(np.float32)
    wi = np.zeros((128, 128))
    wi[:64, :64] = Wre.T
    wi[:64, 64:] = -Wim.T
    wi[64:, :64] = Wim.T
    wi[64:, 64:] = Wre.T
    c["wi1"] = wi.astype(np.float32)
    deltas = np.abs(np.linspace(math.log(1e-2) / 1.5, math.log(1e-2) / 0.3, 512, dtype=np.float32))
    c["delta"] = np.ascontiguousarray(np.broadcast_to(deltas[None, :], (64, 512))).astype(np.float32)

    def zfeat(pos, L):
        t01 = (np.linspace(0.0, 1.0, L, dtype=np.float32))[pos][:, None]
        posf = pos.astype(np.float32)[:, None]
        bands = np.linspace(1e-4, 15, 16, dtype=np.float32)[None, :]
        f = (2.0 * math.pi * posf * bands / L).astype(np.float32)
        return np.concatenate([t01, np.cos(f), -np.sin(f)], -1).astype(np.float32), t01[:, 0]

    for name, L in (("lat", S), ("ctx", C)):
        q = np.arange(4096)
        vf = q < L
        posf = np.where(vf, q, 0)
        zf, t01f = zfeat(posf, L)
        d = 4096 - q
        vb = (d >= 1) & (d < L)
        posb = np.where(vb, d, 0)
        zb, t01b = zfeat(posb, L)
        c[f"z_{name}"] = np.ascontiguousarray(np.stack([zf.T, zb.T], 0))
        c[f"nt_{name}"] = np.ascontiguousarray(np.stack([-t01f.reshape(64, 64), -t01b.reshape(64, 64)], 0))
        c[f"vm_{name}"] = np.ascontiguousarray(np.stack([vf.reshape(64, 64), vb.reshape(64, 64)], 0).astype(np.float32))
    return c


_CONST_SHAPES = None


def build(debug=(), nlayers=2, do_moe=True, do_hyena=True, do_attn=True, stop=99, do_scopes=False):
    nc = bass.Bass("TRN2", target_bir_lowering=False)
    consts = _consts()
    _u = [0]

    def uniq(n):
        _u[0] += 1
        return f"t{_u[0]}_{n}"

    def din(name, shape, dt=F32):
        return nc.dram_tensor(name, list(shape), dt, kind="ExternalInput").ap()

    def dscr(name, shape, dt):
        kind = "ExternalOutput" if name in debug else "Internal"
        return nc.dram_tensor(name, list(shape), dt, kind=kind).ap()

    I = {}
    I["x"] = din("x", [S, D]); I["ctx"] = din("ctx", [C, D]); I["cc"] = din("cc", [128, 8, 2])
    I["w_ada"] = din("w_ada", [2, D, 6 * D]); I["b_adaT"] = din("b_adaT", [2, 128, 48])
    I["n1T"] = din("n1T", [2, 128, 8]); I["n2T"] = din("n2T", [2, 128, 8])
    I["w_in"] = din("w_in", [2, D, 5120])
    I["qkw"] = din("qkw", [2, 128, 1280])
    I["convw"] = din("convw", [2, 128, 3 * 1536]); I["convb"] = din("convb", [2, 128, 1536])
    I["pe_w1"] = din("pe_w1", [2, 33, 64]); I["pe_w2"] = din("pe_w2", [2, 64, 64]); I["pe_w3"] = din("pe_w3", [2, 64, 2048])
    I["pe_v"] = din("pe_v", [2, 64, 4])
    I["skipb"] = din("skipb", [2, 64, 1024])
    I["w_ap"] = din("w_ap", [2, D, D]); I["w_hp"] = din("w_hp", [2, 512, D]); I["w_out"] = din("w_out", [2, D, D])
    I["router_w"] = din("router_w", [D, NE]); I["router_bb"] = din("router_bb", [128, NE])
    if do_moe:
        I["exp_w1"] = din("exp_w1", [2, NE, D, 512]); I["exp_w3"] = din("exp_w3", [2, NE, D, 512]); I["exp_w2"] = din("exp_w2", [2, NE, 512, D])
    I["fnw"] = din("fnw", [128, D])
    for k, v in consts.items():
        I[k] = din("c_" + k, v.shape)
    OUT = nc.dram_tensor("out", [S, D], F32, kind="ExternalOutput").ap()

    X0 = dscr("X0", [T, D], F32); X1 = dscr("X1", [T, D], F32)
    QT_d = dscr("QT_d", [8, 128, T], BF16); KT_d = dscr("KT_d", [2, 128, T], BF16)
    V_d = dscr("V_d", [T, 256], BF16); U_d = dscr("U_d", [T, 1536], F32)
    G_d = dscr("G_d", [2048, T], BF16); YA_d = dscr("YA_d", [D, T], BF16)
    XV_d = dscr("XV_d", [T, 1536], F32)
    A_d = dscr("A_d", [128, 64, 2, 512], BF16); C_d = dscr("C_d", [128, 128, CW], BF16)
    KH_d = dscr("KH_d", [2, 2, 128, 2, 128, 512], BF16)
    YH_d = dscr("YH_d", [T, 512], BF16)
    A2_d = dscr("A2_d", [128, 64, 2, CW], BF16)
    H2T_d = dscr("H2T_d", [D, T], BF16); GATE_d = dscr("GATE_d", [T, NE], F32)
    HT_d = dscr("HT_d", [D, T], BF16)

    with ExitStack() as es0:
        P = Prog(nc, es0)
        V_ = lambda fn, r=(), w=(): P.op("dve", fn, r, w)
        A_ = lambda fn, r=(), w=(): P.op("act", fn, r, w)
        M_ = lambda fn, r=(), w=(): P.op("pe", fn, r, w)
        G_ = lambda fn, r=(), w=(): P.op("dve", fn, r, w)
        GP = lambda fn, r=(), w=(): P.op("dve", fn, r, w)
        def LD(out, in_, r=(), w=()):
            q = "pool" if (str(out.space) == "DRAM" and str(in_.space) != "DRAM") else "sp"
            return P.dma(q, lambda e: e.dma_start(out=out, in_=in_), r, w)
        LDC = lambda out, in_, r=(), w=(): P.dma("pool", lambda e: e.dma_start(out=out, in_=in_), r, w)

        psf = [es0.enter_context(nc.psum_tensor(f"psf{i}", [128, 512], F32)) for i in range(6)]
        psb = [es0.enter_context(nc.psum_tensor(f"psb{i}", [128, 1024], BF16)) for i in range(2)]
        PF = [f"psf{i}" for i in range(6)]
        PB = [f"psb{i}" for i in range(2)]

        def sbp(name, shape, dt):
            return es0.enter_context(nc.sbuf_tensor(uniq(name), list(shape), dt))
        idf = sbp("idf", [128, 128], F32); idb = sbp("idb", [128, 128], BF16)
        onesf = sbp("onesf", [128, 128], F32); onesb = sbp("onesb", [128, 128], BF16)
        epsc = sbp("epsc", [128, 1], F32)
        mods = sbp("mods", [128, 48, 2], F32)
        scA = sbp("scA", [128, 8, 2], F32); scB = sbp("scB", [128, 8, 2], F32)
        gb = sbp("gb", [128, 4, D], F32)
        LD(idf[:], I["ident"][:, :], w=["idf"]); LDC(idb[:], I["ident"][:, :], w=["idb"])
        V_(lambda e: e.memset(onesf[:], 1.0), w=["onesf"]); V_(lambda e: e.memset(onesb[:], 1.0), w=["onesb"])
        V_(lambda e: e.memset(epsc[:], EPS), w=["epsc"])
        LD(X0[0:S, :], I["x"][:, :], w=["X0"]); LD(X0[S:T, :], I["ctx"][:, :], w=["X0"])
        P.barrier()

        _sc = [None]

        def scope(name):
            if not do_scopes:
                return
            if _sc[0] is not None:
                nc.leave_named_scope(_sc[0]) if False else _sc[0].__exit__(None, None, None)
                _sc[0] = None
            if name is not None:
                cm = nc.named_scope(name)
                cm.__enter__()
                _sc[0] = cm

        def chk(x):
            if stop == x and not P.dead:
                P.barrier()
                P.dead = True

        for li in range(nlayers):
          try:
              last = li == 1
              if stop == 0:
                  break
              ntl = 32 if False else NT

              scope(f"L{li}_adaln")
              with ExitStack() as es:
                  sb = lambda n, s, d: es.enter_context(nc.sbuf_tensor(uniq(n), list(s), d))
                  ccs = sb("ccs", [128, 8, 2], F32)
                  wa = [sb(f"wa{i}", [128, 8, 512], F32) for i in range(2)]
                  bT = sb("bT", [128, 48], F32); n1 = sb("n1", [128, 8], F32); n2 = sb("n2", [128, 8], F32)
                  dg = sb("dg", [128, 128], F32); tmp = sb("tmpa", [128, 8, 2], F32)
                  LD(ccs[:], I["cc"][:, :, :], w=["ccs"])
                  A_(lambda e: e.activation(out=ccs[:], in_=ccs[:], func=AF.Silu), ["ccs"], ["ccs"])
                  LD(bT[:], I["b_adaT"][li], w=["bT"]); LD(n1[:], I["n1T"][li], w=["n1"]); LD(n2[:], I["n2T"][li], w=["n2"])
                  wsrc = I["w_ada"][li].rearrange("(kt k) n -> k kt n", k=128)
                  for g in range(12 if stop != 0.3 else 0):
                      w = wa[g % 2]
                      LD(w[:], wsrc[:, :, g * 512:(g + 1) * 512], w=[f"wa{g % 2}"])
                      for sub in range(4):
                          ch = g * 4 + sub
                          for kt in range(8):
                              M_(lambda e: e.matmul(psf[0][:, ch * 2:ch * 2 + 2], w[:, kt, sub * 128:(sub + 1) * 128], ccs[:, kt, :],
                                                    start=(kt == 0), stop=(kt == 7)), [f"wa{g % 2}", "ccs"], [PF[0]])
                  if stop in (0.3, 0.5):
                      break
                  V_(lambda e: e.tensor_tensor(mods[:], psf[0][:, 0:96].rearrange("p (c m) -> p c m", m=2),
                                               bT[:].unsqueeze(2).to_broadcast([128, 48, 2]), op=ALU.add), [PF[0], "bT"], ["mods"])
                  V_(lambda e: e.tensor_scalar(tmp[:], mods[:, 8:16, :], 1.0, None, op0=ALU.add), ["mods"], ["tmpa"])
                  V_(lambda e: e.tensor_tensor(scA[:], tmp[:], n1[:].unsqueeze(2).to_broadcast([128, 8, 2]), op=ALU.mult), ["tmpa", "n1"], ["scA"])
                  V_(lambda e: e.tensor_scalar(tmp[:], mods[:, 32:40, :], 1.0, None, op0=ALU.add), ["mods", "scA"], ["tmpa"])
                  V_(lambda e: e.tensor_tensor(scB[:], tmp[:], n2[:].unsqueeze(2).to_broadcast([128, 8, 2]), op=ALU.mult), ["tmpa", "n2"], ["scB"])
                  if stop == 0.7:
                      break
                  for gi, base in enumerate((16, 40)):
                      for m in range(2):
                          for dt in range(8):
                              V_(lambda e: e.tensor_scalar(dg[:], idf[:], mods[:, base + dt, m:m + 1], None, op0=ALU.mult), ["idf", "mods"], ["dg"])
                              M_(lambda e: e.matmul(psf[1][:, 0:128], onesf[:], dg[:], start=True, stop=True), ["onesf", "dg"], [PF[1]])
                              A_(lambda e: e.activation(out=gb[:, gi * 2 + m, dt * 128:(dt + 1) * 128], in_=psf[1][:, 0:128], func=AF.Identity), [PF[1]], ["gb"])
              P.barrier()

              if stop == 1:
                  break
              scope(f"L{li}_inproj")
              cur, mid = X0, X1
              with ExitStack() as es:
                  sb = lambda n, s, d: es.enter_context(nc.sbuf_tensor(uniq(n), list(s), d))
                  wq = sb("wq", [128, 8, 3072], BF16)
                  qkw = sb("qkw", [128, 10, 128], F32)
                  rc = sb("rc", [128, 2, 64], F32); rs = sb("rs", [128, 2, 64], F32)
                  xt = [sb(f"xt{i}", [128, D], F32) for i in range(2)]
                  xn = [sb(f"xn{i}", [128, D], BF16) for i in range(2)]
                  junk = sb("junk", [128, 1280], BF16)
                  st = [sb(f"st{i}", [128, 4], F32) for i in range(2)]
                  hT = [sb(f"hT{i}", [128, 8, 512], BF16) for i in range(2)]
                  qs = sb("qs", [128, 1280], F32); q2 = sb("q2", [128, 1280], F32)
                  hs = sb("hs", [128, 16], F32)
                  r1 = sb("r1", [128, 10, 64], F32); r2 = sb("r2", [128, 10, 64], F32)
                  qr = [sb(f"qr{i}", [128, 10, 128], BF16) for i in range(2)]
                  qT = [sb(f"qT{i}", [128, 10, 512], BF16) for i in range(2)]
                  vsb = [sb(f"vsb{i}", [128, 256], BF16) for i in range(2)]
                  usb = [sb(f"usb{i}", [128, 1536], F32) for i in range(2)]
                  wsrc = I["w_in"][li].rearrange("(kt k) n -> k kt n", k=128)
                  for cgi in range(3):
                      LDC(wq[:, :, cgi * 1024:(cgi + 1) * 1024], wsrc[:, :, cgi * 1024:(cgi + 1) * 1024], w=["wq"])
                  LD(qkw[:].rearrange("p a b -> p (a b)"), I["qkw"][li], w=["qkw"])
                  nblk = (T + 511) // 512
                  for blk in range(nblk):
                      tts = list(range(blk * 4, min(blk * 4 + 4, NT)))
                      ntok = len(tts) * 128
                      hb = blk % 2
                      for ti, tt in enumerate(tts):
                          m = 0 if tt < 32 else 1
                          b = tt % 2
                          LD(xt[b][:], cur[tt * 128:(tt + 1) * 128, :], ["X0", "X1"] if False else [], [f"xt{b}"])
                          A_(lambda e: e.activation(out=junk[:, 0:D], in_=xt[b][:], func=AF.Square, accum_out=st[b][:, 0:1]), [f"xt{b}"], ["junk", f"st{b}"])
                          A_(lambda e: e.activation(out=st[b][:, 1:2], in_=st[b][:, 0:1], func=AF.Sqrt, scale=1.0 / D, bias=epsc[:, 0:1]), [f"st{b}", "epsc"], [f"st{b}"])
                          V_(lambda e: e.reciprocal(st[b][:, 2:3], st[b][:, 1:2]), [f"st{b}"], [f"st{b}"])
                          V_(lambda e: e.tensor_scalar(xn[b][:], xt[b][:], st[b][:, 2:3], None, op0=ALU.mult), [f"xt{b}", f"st{b}"], [f"xn{b}"])
                          for dt in range(8):
                              M_(lambda e: e.transpose(psb[0][:, dt * 128:(dt + 1) * 128], xn[b][:, dt * 128:(dt + 1) * 128], idb[:]), [f"xn{b}", "idb"], [PB[0]])
                          for dt in range(8):
                              A_(lambda e: e.activation(out=hT[hb][:, dt, ti * 128:(ti + 1) * 128], in_=psb[0][:, dt * 128:(dt + 1) * 128], func=AF.Identity,
                                                        scale=scA[:, dt, m:m + 1], bias=mods[:, dt, m:m + 1]), [PB[0], "scA", "mods"], [f"hT{hb}"])
                          chk(1.2)
                      for ti, tt in enumerate(tts):
                          b = tt % 2
                          for cgi in range(6):
                              pb = PF[cgi % 3]; pt = psf[cgi % 3]
                              for kt in range(8):
                                  M_(lambda e: e.matmul(pt[:], hT[hb][:, kt, ti * 128:(ti + 1) * 128], wq[:, kt, cgi * 512:(cgi + 1) * 512],
                                                        start=(kt == 0), stop=(kt == 7)), [f"hT{hb}", "wq"], [pb])
                              chk(1.25)
                              if cgi == 1:
                                  chk(1.2515)
                              if cgi < 2:
                                  A_(lambda e: e.activation(out=qs[:, cgi * 512:(cgi + 1) * 512], in_=pt[:], func=AF.Identity), [pb], ["qs"])
                                  chk(1.251 + 0.001 * cgi)
                              elif cgi == 2:
                                  A_(lambda e: e.activation(out=qs[:, 1024:1280], in_=pt[:, 0:256], func=AF.Identity), [pb], ["qs"])
                                  chk(1.253)
                                  A_(lambda e: e.activation(out=vsb[b][:], in_=pt[:, 256:512], func=AF.Identity), [pb], [f"vsb{b}"])
                                  chk(1.26)
                                  LD(V_d[tt * 128:(tt + 1) * 128, :], vsb[b][:], [f"vsb{b}"], ["V_d"])
                                  chk(1.27)
                              else:
                                  if cgi % 2 == 0:
                                      A_(lambda e: e.activation(out=usb[b][:, (cgi - 3) * 512:(cgi - 2) * 512], in_=pt[:], func=AF.Identity), [pb], [f"usb{b}"])
                                  else:
                                      V_(lambda e: e.tensor_scalar(usb[b][:, (cgi - 3) * 512:(cgi - 2) * 512], pt[:], 1.0, None, op0=ALU.mult), [pb], [f"usb{b}"])
                          LD(U_d[tt * 128:(tt + 1) * 128, :], usb[b][:], [f"usb{b}"], ["U_d"])
                          chk(1.3)
                          q3 = qs[:].rearrange("p (h d) -> p h d", d=128)
                          A_(lambda e: e.activation(out=q2[:], in_=qs[:], func=AF.Square), ["qs"], ["q2"])
                          V_(lambda e: e.tensor_reduce(hs[:, 0:10], q2[:].rearrange("p (h d) -> p h d", d=128), axis=AX.X, op=ALU.add), ["q2"], ["hs"])
                          A_(lambda e: e.activation(out=hs[:, 0:10], in_=hs[:, 0:10], func=AF.Sqrt, scale=1.0 / 128, bias=epsc[:, 0:1]), ["hs", "epsc"], ["hs"])
                          V_(lambda e: e.reciprocal(hs[:, 0:10], hs[:, 0:10]), ["hs"], ["hs"])
                          V_(lambda e: e.tensor_tensor(q2[:].rearrange("p (h d) -> p h d", d=128), q3, hs[:, 0:10].unsqueeze(2).to_broadcast([128, 10, 128]), op=ALU.mult), ["qs", "hs"], ["q2"])
                          G_(lambda e: e.tensor_tensor(qs[:], q2[:], qkw[:].rearrange("p a b -> p (a b)"), op=ALU.mult), ["q2", "qkw"], ["qs"])
                          chk(1.4)
                          LD(rc[:, b, :], I["ropec"][:, tt, :], w=["rc"]); LD(rs[:, b, :], I["ropes"][:, tt, :], w=["rs"])
                          cb = rc[:, b, :].unsqueeze(1).to_broadcast([128, 10, 64]); sbb = rs[:, b, :].unsqueeze(1).to_broadcast([128, 10, 64])
                          x1v = q3[:, :, 0:64]; x2v = q3[:, :, 64:128]
                          V_(lambda e: e.tensor_tensor(r1[:], x1v, cb, op=ALU.mult), ["qs", "rc"], ["r1"])
                          G_(lambda e: e.tensor_tensor(r2[:], x2v, sbb, op=ALU.mult), ["qs", "rs"], ["r2"])
                          V_(lambda e: e.tensor_tensor(qr[b][:, :, 0:64], r1[:], r2[:], op=ALU.subtract), ["r1", "r2"], [f"qr{b}"])
                          V_(lambda e: e.tensor_tensor(r1[:], x1v, sbb, op=ALU.mult), ["qs", "rs", f"qr{b}"], ["r1"])
                          G_(lambda e: e.tensor_tensor(r2[:], x2v, cb, op=ALU.mult), ["qs", "rc", f"qr{b}"], ["r2"])
                          V_(lambda e: e.tensor_tensor(qr[b][:, :, 64:128], r1[:], r2[:], op=ALU.add), ["r1", "r2"], [f"qr{b}"])
                          chk(1.5)
                          for h in range(10):
                              pbi = 1 if h < 8 else 0
                              off = (h % 8) * 128
                              M_(lambda e: e.transpose(psb[1][:, off:off + 128] if h < 8 else psb[0][:, off:off + 128], qr[b][:, h, :], idb[:]),
                                 [f"qr{b}", "idb"], [PB[pbi]])
                          V_(lambda e: e.tensor_scalar(qT[hb][:, 0:8, ti * 128:(ti + 1) * 128], psb[1][:].rearrange("p (h t) -> p h t", t=128), 1.0, None, op0=ALU.mult), [PB[1]], [f"qT{hb}"])
                          A_(lambda e: e.activation(out=qT[hb][:, 8:10, ti * 128:(ti + 1) * 128], in_=psb[0][:, 0:256].rearrange("p (h t) -> p h t", t=128), func=AF.Identity), [PB[0]], [f"qT{hb}"])
                          chk(1.6)
                      t0 = blk * 512
                      LD(QT_d[:, :, t0:t0 + ntok].rearrange("h p t -> p h t"), qT[hb][:, 0:8, 0:ntok], [f"qT{hb}"], ["QT_d"])
                      LD(KT_d[:, :, t0:t0 + ntok].rearrange("h p t -> p h t"), qT[hb][:, 8:10, 0:ntok], [f"qT{hb}"], ["KT_d"])
                      LD(HT_d[:, t0:t0 + ntok].rearrange("(kt p) t -> p kt t", p=128), hT[hb][:, :, 0:ntok], [f"hT{hb}"], ["HT_d"])
              P.barrier()
              if stop == 2:
                  break
              scope(f"L{li}_gates")
              with ExitStack() as es:
                  sb = lambda n, s, d: es.enter_context(nc.sbuf_tensor(uniq(n), list(s), d))
                  wg = sb("wg", [128, 8, 2048], BF16)
                  hT = [sb(f"hT{i}", [128, 8, 512], BF16) for i in range(2)]
                  gsb = [sb(f"gsb{i}", [128, 16, 512], BF16) for i in range(2)]
                  wsrc = I["w_in"][li].rearrange("(kt k) n -> k kt n", k=128)
                  for cgi in range(2):
                      LDC(wg[:, :, cgi * 1024:(cgi + 1) * 1024], wsrc[:, :, 3072 + cgi * 1024:3072 + (cgi + 1) * 1024], w=["wg"])
                  ntg = T if not last else S
                  for blk in range((ntg + 511) // 512):
                      t0 = blk * 512
                      ntok = min(512, ntg - t0)
                      hb = blk % 2
                      LD(hT[hb][:, :, 0:ntok], HT_d[:, t0:t0 + ntok].rearrange("(kt p) t -> p kt t", p=128), w=[f"hT{hb}"])
                      for ncn in range(16):
                          pb = PF[3 + ncn % 2]; pt = psf[3 + ncn % 2]
                          for kt in range(8):
                              M_(lambda e: e.matmul(pt[:, 0:ntok], wg[:, kt, ncn * 128:(ncn + 1) * 128], hT[hb][:, kt, 0:ntok],
                                                    start=(kt == 0), stop=(kt == 7)), [f"hT{hb}", "wg"], [pb])
                          gi = blk % 2
                          A_(lambda e: e.activation(out=gsb[gi][:, ncn, 0:ntok], in_=pt[:, 0:ntok], func=AF.Sigmoid), [pb], [f"gsb{gi}"])
                      LD(G_d[:, t0:t0 + ntok].rearrange("(h p) t -> p h t", p=128), gsb[blk % 2][:, :, 0:ntok], [f"gsb{blk % 2}"], ["G_d"])
              P.barrier()

              if stop == 3:
                  break
              scope(f"L{li}_attn")
              if do_attn:
                  with ExitStack() as es:
                      sb = lambda n, s, d: es.enter_context(nc.sbuf_tensor(uniq(n), list(s), d))
                      kT = sb("kT", [128, 2, T], BF16); vv = sb("vv", [128, NT, 256], BF16)
                      qb = [sb(f"qb{i}", [128, 512], BF16) for i in range(2)]
                      pT = [sb(f"pT{i}", [128, 512], BF16) for i in range(4)]
                      stb = [psf[0][:], psf[1][:], psb[0][:].bitcast(F32), psb[1][:].bitcast(F32)]
                      stk = [PF[0], PF[1], PB[0], PB[1]]
                      rcp = sb("rcp", [128, 512], F32)
                      yo = [sb(f"yo{i}", [128, 512], BF16) for i in range(2)]
                      for h in range(2):
                          LD(kT[:, h, :], KT_d[h], w=["kT"])
                      LD(vv[:], V_d.rearrange("(t p) c -> p t c", p=128), w=["vv"])
                      scale = 128 ** -0.5
                      it = 0
                      blocks = [(h, q0, 512, 0, NT) for h in range(8) for q0 in range(0, S, 512)]
                      if not last:
                          blocks += [(h, S, 256, 32, NT) for h in range(8)]
                      for bi, (h, q0, nq, k0, k1) in enumerate(blocks):
                          kv = h // 4
                          b = bi % 2
                          LD(qb[b][:, 0:nq], QT_d[h, :, q0:q0 + nq], w=[f"qb{b}"])
                          po = psf[4 + b]; pok = PF[4 + b]
                          psm = psf[2 + b]; psmk = PF[2 + b]
                          def qk_(kt, g):
                              sbk = g % 4
                              M_(lambda e: e.matmul(stb[sbk][:, 0:nq], kT[:, kv, kt * 128:(kt + 1) * 128], qb[b][:, 0:nq], start=True, stop=True),
                                 ["kT", f"qb{b}"], [stk[sbk]])

                          def ex_(kt, g):
                              sbk = g % 4; pb3 = g % 4
                              A_(lambda e: e.activation(out=pT[pb3][:, 0:nq], in_=stb[sbk][:, 0:nq], func=AF.Exp, scale=scale), [stk[sbk]], [f"pT{pb3}"])

                          def pv_(kt, g):
                              pb3 = g % 4
                              M_(lambda e: e.matmul(po[:, 0:nq], vv[:, kt, kv * 128:(kv + 1) * 128], pT[pb3][:, 0:nq], start=(kt == k0), stop=(kt == k1 - 1)),
                                 ["vv", f"pT{pb3}"], [pok])
                              M_(lambda e: e.matmul(psm[:, 0:nq], onesb[:], pT[pb3][:, 0:nq], start=(kt == k0), stop=(kt == k1 - 1)),
                                 ["onesb", f"pT{pb3}"], [psmk])

                          g0 = it
                          it += (k1 - k0)
                          qk_(k0, g0)
                          if k0 + 1 < k1:
                              qk_(k0 + 1, g0 + 1)
                          for kt in range(k0, k1):
                              g = g0 + (kt - k0)
                              if kt + 2 < k1:
                                  qk_(kt + 2, g + 2)
                              ex_(kt, g)
                              pv_(kt, g)
                          V_(lambda e: e.reciprocal(rcp[:, 0:nq], psm[:, 0:nq]), [psmk], ["rcp"])
                          V_(lambda e: e.tensor_tensor(yo[b][:, 0:nq], po[:, 0:nq], rcp[:, 0:nq], op=ALU.mult), [pok, "rcp"], [f"yo{b}"])
                          LD(YA_d[h * 128:(h + 1) * 128, q0:q0 + nq], yo[b][:, 0:nq], [f"yo{b}"], ["YA_d"])
                  P.barrier()

              if stop == 4:
                  break
              if do_hyena:
                  scope(f"L{li}_shortconv")
                  with ExitStack() as es:
                      sb = lambda n, s, d: es.enter_context(nc.sbuf_tensor(uniq(n), list(s), d))
                      cw_ = sb("cw_", [128, 3, 1536], F32); cb_ = sb("cb_", [128, 1536], F32)
                      um = [sb(f"um{i}", [128, 1536], F32) for i in range(2)]
                      u0 = [sb(f"u0{i}", [128, 1536], F32) for i in range(2)]
                      up = [sb(f"up{i}", [128, 1536], F32) for i in range(2)]
                      oc = [sb(f"oc{i}", [128, 1536], F32) for i in range(2)]
                      t3 = sb("t3", [128, 1536], F32); t4 = sb("t4", [128, 1536], F32)
                      LD(cw_[:].rearrange("p a b -> p (a b)"), I["convw"][li], w=["cw_"]); LD(cb_[:], I["convb"][li], w=["cb_"])
                      tiles = list(range(NT if not last else 32))
                      for tt in tiles:
                          b = tt % 2
                          first = tt in (0, 32); lastt = tt in (31, 33)
                          r0 = tt * 128
                          if first:
                              V_(lambda e: e.memset(um[b][:], 0.0), w=[f"um{b}"])
                              LD(um[b][1:128, :], U_d[r0:r0 + 127, :], w=[f"um{b}"])
                          else:
                              LD(um[b][:], U_d[r0 - 1:r0 + 127, :], w=[f"um{b}"])
                          LD(u0[b][:], U_d[r0:r0 + 128, :], w=[f"u0{b}"])
                          if lastt:
                              V_(lambda e: e.memset(up[b][:], 0.0), w=[f"up{b}"])
                              LD(up[b][0:127, :], U_d[r0 + 1:r0 + 128, :], w=[f"up{b}"])
                          else:
                              LD(up[b][:], U_d[r0 + 1:r0 + 129, :], w=[f"up{b}"])
                          V_(lambda e: e.tensor_tensor(oc[b][:], u0[b][:], cw_[:, 1, :], op=ALU.mult), [f"u0{b}", "cw_"], [f"oc{b}"])
                          GP(lambda e: e.tensor_tensor(t3[:], um[b][:], cw_[:, 0, :], op=ALU.mult), [f"um{b}", "cw_"], ["t3"])
                          GP(lambda e: e.tensor_tensor(t4[:], up[b][:], cw_[:, 2, :], op=ALU.mult), [f"up{b}", "cw_"], ["t4"])
                          GP(lambda e: e.tensor_tensor(t3[:], t3[:], t4[:], op=ALU.add), ["t3", "t4"], ["t3"])
                          V_(lambda e: e.tensor_tensor(oc[b][:], oc[b][:], cb_[:], op=ALU.add), [f"oc{b}", "cb_"], [f"oc{b}"])
                          V_(lambda e: e.tensor_tensor(oc[b][:], oc[b][:], t3[:], op=ALU.add), [f"oc{b}", "t3"], [f"oc{b}"])
                          LD(XV_d[r0:r0 + 128, :], oc[b][:], [f"oc{b}"], ["XV_d"])
                  P.barrier()

                  chk(4.1)
                  seqs = [("lat", 0, 64)] + ([] if last else [("ctx", S, 4)])
                  with ExitStack() as es:
                      sb = lambda n, s, d: es.enter_context(nc.sbuf_tensor(uniq(n), list(s), d))
                      w3 = sb("w3", [64, 2048], BF16)
                      h2 = sb("h2", [64, 2, 4096], BF16)
                      nt_ = sb("nt_", [64, 2, 64], F32); vm_ = sb("vm_", [64, 2, 64], F32); dl = sb("dl", [64, 512], F32)
                      LDC(w3[:], I["pe_w3"][li], w=["w3"]); LD(dl[:], I["delta"][:, :], w=["dl"])
                      for si, (sname, tok0, pmax) in enumerate(seqs):
                          LD(nt_[:], I[f"nt_{sname}"].rearrange("d p j -> p d j"), w=["nt_"]); LD(vm_[:], I[f"vm_{sname}"].rearrange("d p j -> p d j"), w=["vm_"])
                          scope(f"L{li}_f{si}_mlp")
                          with ExitStack() as es2:
                              sb2 = lambda n, s, d: es2.enter_context(nc.sbuf_tensor(uniq(n), list(s), d))
                              w1 = sb2("w1", [33, 64], F32); w2 = sb2("w2", [64, 64], F32)
                              pv = sb2("pv", [64, 4], F32); pvb = sb2("pvb", [64, 2], F32)
                              zT = sb2("zT", [33, 4096], F32)
                              h1 = sb2("h1", [64, 512], F32); harg = sb2("harg", [64, 512], F32); hw1 = sb2("hw1", [64, 512], F32); hw2 = sb2("hw2", [64, 512], F32)
                              LD(w1[:], I["pe_w1"][li], w=["w1"]); LD(w2[:], I["pe_w2"][li], w=["w2"]); LD(pv[:], I["pe_v"][li], w=["pv"])
                              V_(lambda e: e.tensor_tensor(pvb[:, 0:1], pv[:, 0:1], pv[:, 1:2], op=ALU.mult), ["pv"], ["pvb"])
                              V_(lambda e: e.tensor_tensor(pvb[:, 1:2], pv[:, 2:3], pv[:, 3:4], op=ALU.mult), ["pv"], ["pvb"])

                              def sin_layer(ps, fcol, bcol, out_ap, okey):
                                  A_(lambda e: e.activation(out=harg[:], in_=ps, func=AF.Identity, scale=pv[:, fcol:fcol + 1], bias=pvb[:, bcol:bcol + 1]), [PF[0], "pv", "pvb"], ["harg"])
                                  for _ in range(2):
                                      V_(lambda e: e.tensor_scalar(hw1[:], harg[:], PI, -2 * PI, op0=ALU.is_gt, op1=ALU.mult), ["harg"], ["hw1"])
                                      V_(lambda e: e.tensor_scalar(hw2[:], harg[:], -PI, 2 * PI, op0=ALU.is_lt, op1=ALU.mult), ["harg"], ["hw2"])
                                      V_(lambda e: e.tensor_tensor(hw1[:], hw1[:], hw2[:], op=ALU.add), ["hw1", "hw2"], ["hw1"])
                                      V_(lambda e: e.tensor_tensor(harg[:], harg[:], hw1[:], op=ALU.add), ["harg", "hw1"], ["harg"])
                                  A_(lambda e: e.activation(out=out_ap, in_=harg[:], func=AF.Sin), ["harg"], [okey])

                              for dr in range(2):
                                  LD(zT[:], I[f"z_{sname}"][dr], w=["zT"])
                                  for ck in range(8):
                                      M_(lambda e: e.matmul(psf[0][0:64, :], w1[:], zT[:, ck * 512:(ck + 1) * 512], start=True, stop=True), ["w1", "zT"], [PF[0]])
                                      sin_layer(psf[0][0:64, :], 0, 0, h1[:], "h1")
                                      M_(lambda e: e.matmul(psf[0][0:64, :], w2[:], h1[:], start=True, stop=True), ["w2", "h1"], [PF[0]])
                                      sin_layer(psf[0][0:64, :], 2, 1, h2[:, dr, ck * 512:(ck + 1) * 512], "h2")
                          P.barrier()
                          for o in range(2):
                              scope(f"L{li}_f{si}{o}_s1")
                              with ExitStack() as es2:
                                  sb2 = lambda n, s, d: es2.enter_context(nc.sbuf_tensor(uniq(n), list(s), d))
                                  mlo = sb2("mlo", [64, 64, 2, 128], BF16); mhi = sb2("mhi", [64, 64, 2, 128], BF16)
                                  kfb = [sb2(f"kfb{i}", [64, 2, 512], BF16) for i in range(2)]
                                  asb = [sb2(f"asb{i}", [128, 4, 2, 512], BF16) for i in range(2)]
                                  LDC(mlo[:], I["mlo"][:, :, :, :], w=["mlo"]); LDC(mhi[:], I["mhi"][:, :, :, :], w=["mhi"])
                                  wps = [[psf[0], psf[1]], [psf[2], psf[3]]]; wpk = [[PF[0], PF[1]], [PF[2], PF[3]]]
                                  sps = [[psf[4][:], psf[5][:]], [psb[0][:].bitcast(F32), psb[1][:].bitcast(F32)]]; spk = [[PF[4], PF[5]], [PB[0], PB[1]]]
                                  win = [[sb2(f"win{i}{d_}", [64, 512], F32) for d_ in range(2)] for i in range(2)]

                                  def fs1_a(j):
                                      b = j % 2
                                      for dr in range(2):
                                          A_(lambda e: e.activation(out=win[b][dr][:], in_=dl[:], func=AF.Exp, scale=nt_[:, dr, j:j + 1]), ["dl", "nt_"], [f"win{b}{dr}"])
                                          M_(lambda e: e.matmul(wps[b][dr][0:64, :], h2[:, dr, :].rearrange("k (p j) -> k j p", j=64)[:, j, :],
                                                                w3[:, o * 1024 + dr * 512:o * 1024 + (dr + 1) * 512], start=True, stop=True), ["h2", "w3"], [wpk[b][dr]])
                                          V_(lambda e: e.scalar_tensor_tensor(out=kfb[b][:, dr, :], in0=win[b][dr][:], scalar=vm_[:, dr, j:j + 1], in1=wps[b][dr][0:64, :], op0=ALU.mult, op1=ALU.mult),
                                             [f"win{b}{dr}", "vm_", wpk[b][dr]], [f"kfb{b}{dr}"])

                                  def fs1_b(j):
                                      b = j % 2
                                      for ri in range(2):
                                          M_(lambda e: e.matmul(sps[b][ri], mlo[:, j, ri, :], kfb[b][:, 0, :], start=True, stop=False), ["mlo", f"kfb{b}0"], [spk[b][ri]])
                                          M_(lambda e: e.matmul(sps[b][ri], mhi[:, j, ri, :], kfb[b][:, 1, :], start=False, stop=True), ["mhi", f"kfb{b}1"], [spk[b][ri]])
                                      ab_ = (j // 4) % 2; jl = j % 4
                                      A_(lambda e: e.activation(out=asb[ab_][:, jl, 0, :], in_=sps[b][0], func=AF.Identity), [spk[b][0]], [f"asbr{ab_}"])
                                      V_(lambda e: e.tensor_scalar(asb[ab_][:, jl, 1, :], sps[b][1], 1.0, None, op0=ALU.mult), [spk[b][1]], [f"asbi{ab_}"])
                                      if jl == 3:
                                          LD(A_d[:, j - 3:j + 1, :, :].rearrange("f j r c -> f (j r c)"), asb[ab_][:].rearrange("f j r c -> f (j r c)"), [f"asbr{ab_}", f"asbi{ab_}"], ["A_d"])

                                  fs1_a(0)
                                  for j in range(64):
                                      if j + 1 < 64:
                                          fs1_a(j + 1)
                                      fs1_b(j)
                              P.barrier()
                              scope(f"L{li}_f{si}{o}_s3")
                              with ExitStack() as es2:
                                  sb2 = lambda n, s, d: es2.enter_context(nc.sbuf_tensor(uniq(n), list(s), d))
                                  w64 = sb2("w64", [64, 8, 128], BF16)
                                  bsb = [sb2(f"bsb{i}", [64, 4, 2, 512], BF16) for i in range(2)]
                                  ksb = [sb2(f"ksb{i}", [128, 4, 2, 512], BF16) for i in range(2)]
                                  LDC(w64[:], I["w64"][:, :, :], w=["w64"])
                                  for fc in range(32):
                                      b = fc % 2
                                      LD(bsb[b][:], A_d[fc * 4:(fc + 1) * 4].rearrange("f j r c -> j f r c"), w=[f"bsb{b}"])
                                      for fl in range(4):
                                          f1i = fc * 4 + fl
                                          kb2 = f1i % 2
                                          for ab in range(2):
                                              pt = psf[ab + 2 * kb2]; pk = PF[ab + 2 * kb2]
                                              M_(lambda e: e.matmul(pt[:], w64[:, 4 + 2 * ab, :], bsb[b][:, fl, 0, :], start=True, stop=False), ["w64", f"bsb{b}"], [pk])
                                              M_(lambda e: e.matmul(pt[:], w64[:, 5 + 2 * ab, :], bsb[b][:, fl, 1, :], start=False, stop=True), ["w64", f"bsb{b}"], [pk])
                                          A_(lambda e: e.activation(out=ksb[b][:, fl, 0, :], in_=psf[2 * kb2][:], func=AF.Identity), [PF[2 * kb2]], [f"ksba{b}"])
                                          V_(lambda e: e.tensor_scalar(ksb[b][:, fl, 1, :], psf[1 + 2 * kb2][:], 1.0, None, op0=ALU.mult), [PF[1 + 2 * kb2]], [f"ksbb{b}"])
                                          if fl == 3:
                                              LD(KH_d[si, o, fc * 4:(fc + 1) * 4].rearrange("f a r c -> r (f a) c"), ksb[b][:].rearrange("r f a c -> r (f a) c"), [f"ksba{b}", f"ksbb{b}"], ["KH_d"])
                              P.barrier()
                  P.barrier()

                  chk(4.2)
                  with ExitStack() as es:
                      sb = lambda n, s, d: es.enter_context(nc.sbuf_tensor(uniq(n), list(s), d))
                      mlo = sb("mlo", [64, 64, 2, 128], BF16); minvT = sb("minvT", [128, 64, 2, 64], BF16)
                      w64 = sb("w64", [64, 4, 128], BF16); wi1 = sb("wi1", [128, 128], BF16)
                      skb = sb("skb", [64, 1024], F32)
                      vz = sb("vz", [64, 64, CW], BF16); zz = sb("zz", [64, 64, CW], BF16)
                      asb = [sb(f"asb{i}", [128, 4, 2, CW], BF16) for i in range(2)]
                      bsb = [sb(f"bsb{i}", [64, 8, 2, CW], BF16) for i in range(2)]
                      ksb = [sb(f"ksb{i}", [128, 2, CW], BF16) for i in range(4)]
                      y1 = [sb(f"y1{i}", [128, CW], F32) for i in range(2)]
                      y2 = [sb(f"y2{i}", [128, CW], F32) for i in range(2)]
                      yb = [sb(f"yb{i}", [128, CW], BF16) for i in range(2)]
                      csb = [sb(f"csb{i}", [128, 8, CW], BF16) for i in range(2)]
                      cj = [sb(f"cj{i}", [128, 2, CW], BF16) for i in range(2)]
                      xg = [sb(f"xg{i}", [64, CW], F32) for i in range(2)]
                      tg = [sb(f"tg{i}", [64, CW], F32) for i in range(2)]
                      yh = [sb(f"yh{i}", [64, 4, CW], BF16) for i in range(2)]
                      LDC(mlo[:], I["mlo"][:, :, :, :], w=["mlo"]); LDC(minvT[:], I["minvT"][:, :, :, :], w=["minvT"])
                      LDC(w64[:], I["w64"][:, 0:4, :], w=["w64"]); LDC(wi1[:], I["wi1"][:, :], w=["wi1"])
                      LD(skb[:], I["skipb"][li], w=["skb"])
                      skp = sb("skp", [64, CW], F32)
                      passes = [[(0, 0, 64, k * 256, 256, 0)] for k in range(2)]
                      if not last:
                          passes += [[(1, S, 4, k * 256, 256, 0)] for k in range(2)]
                      for segs in passes:
                          if True:
                              for b in range(2):
                                  V_(lambda e: e.memset(xg[b][:], 0.0), w=[f"xg{b}"])
                              if segs[0][2] < 64:
                                  V_(lambda e: e.memset(vz[:], 0.0), w=["vz"])
                              for (si, tok0, pmax, c0, w_, off) in segs:
                                  LDC(vz[0:pmax, :, off:off + w_], XV_d[tok0:tok0 + pmax * 64, 1024 + c0:1024 + c0 + w_].rearrange("(p j) c -> p j c", j=64), w=["vz"])
                              for o in range(2):
                                  scope(f"L{li}_c{segs[0][0]}{segs[0][3]}{o}_s1")
                                  src = vz if o == 0 else zz
                                  skey = "vz" if o == 0 else "zz"
                                  for j in range(64):
                                      b = j % 2
                                      for ri in range(2):
                                          M_(lambda e: e.matmul(psf[ri + 2 * b][:, 0:CW], mlo[:, j, ri, :], src[:, j, :], start=True, stop=True), ["mlo", skey], [PF[ri + 2 * b]])
                                      ab_ = (j // 4) % 2; jl = j % 4
                                      A_(lambda e: e.activation(out=asb[ab_][:, jl, 0, :], in_=psf[2 * b][:, 0:CW], func=AF.Identity), [PF[2 * b]], [f"asbr{ab_}"])
                                      V_(lambda e: e.tensor_scalar(asb[ab_][:, jl, 1, :], psf[1 + 2 * b][:, 0:CW], 1.0, None, op0=ALU.mult), [PF[1 + 2 * b]], [f"asbi{ab_}"])
                                      if jl == 3:
                                          LD(A2_d[:, j - 3:j + 1, :, :].rearrange("f j r c -> f (j r c)"), asb[ab_][:].rearrange("f j r c -> f (j r c)"), [f"asbr{ab_}", f"asbi{ab_}"], ["A_d"])
                                  def s3_mm(f1i):
                                      fc, fl = divmod(f1i, 8)
                                      b = fc % 2; par = f1i % 2; k3 = f1i % 4
                                      if fl == 0:
                                          LD(bsb[b][:].rearrange("j f r c -> j f (r c)"), A2_d[fc * 8:(fc + 1) * 8].rearrange("f j r c -> j f (r c)"), w=[f"bsb{b}"])
                                      for (si, tok0, pmax, c0, w_, off) in segs:
                                          LD(ksb[k3][:, :, off:off + w_], KH_d[si, o, f1i, :, :, c0:c0 + w_].rearrange("a r c -> r a c"), w=[f"ksb{k3}"])
                                      for pq in range(2):
                                          pi = pq + 2 * par
                                          M_(lambda e: e.matmul(psf[pi][:, 0:CW], w64[:, 2 * pq, :], bsb[b][:, fl, 0, :], start=True, stop=False), ["w64", f"bsb{b}"], [PF[pi]])
                                          M_(lambda e: e.matmul(psf[pi][:, 0:CW], w64[:, 2 * pq + 1, :], bsb[b][:, fl, 1, :], start=False, stop=True), ["w64", f"bsb{b}"], [PF[pi]])

                                  def s3_post(f1i):
                                      fc, fl = divmod(f1i, 8)
                                      b = fc % 2; par = f1i % 2; k3 = f1i % 4; b2 = par
                                      V_(lambda e: e.tensor_tensor(y1[b2][:], psf[2 * par][:, 0:CW], ksb[k3][:, 0, :], op=ALU.mult), [PF[2 * par], f"ksb{k3}"], [f"y1{b2}"])
                                      V_(lambda e: e.tensor_tensor(y2[b2][:], psf[1 + 2 * par][:, 0:CW], ksb[k3][:, 1, :], op=ALU.mult), [PF[1 + 2 * par], f"ksb{k3}"], [f"y2{b2}"])
                                      GP(lambda e: e.tensor_tensor(yb[b2][:], y1[b2][:], y2[b2][:], op=ALU.add), [f"y1{b2}", f"y2{b2}"], [f"yb{b2}"])
                                      M_(lambda e: e.matmul(psf[4 + par][:, 0:CW], wi1[:], yb[b2][:], start=True, stop=True), ["wi1", f"yb{b2}"], [PF[4 + par]])
                                      A_(lambda e: e.activation(out=csb[b][:, fl, :], in_=psf[4 + par][:, 0:CW], func=AF.Identity), [PF[4 + par]], [f"csb{b}"])
                                      if fl == 7:
                                          LD(C_d[:, fc * 8:(fc + 1) * 8, :], csb[b][:], [f"csb{b}"], ["C_d"])

                                  P.barrier()
                                  scope(f"L{li}_c{segs[0][0]}{segs[0][3]}{o}_s3")
                                  s3_mm(0)
                                  for f1i in range(128):
                                      if f1i + 1 < 128:
                                          s3_mm(f1i + 1)
                                      s3_post(f1i)
                                  P.barrier()
                                  scope(f"L{li}_c{segs[0][0]}{segs[0][3]}{o}_i2")
                                  Cv = C_d.rearrange("(r j) f c -> j f r c", j=64)
                                  for (si, tok0, pmax, c0, w_, off) in segs:
                                      V_(lambda e: e.tensor_scalar(skp[:, off:off + w_], skb[:, o * 512 + c0:o * 512 + c0 + w_], 1.0, None, op0=ALU.mult), ["skb"], ["skp"])
                                  for j in range(64):
                                      b = j % 2
                                      LD(cj[b][:], Cv[j], w=[f"cj{b}"])
                                      M_(lambda e: e.matmul(psf[4 + b][0:64, 0:CW], minvT[:, j, 0, :], cj[b][:, 0, :], start=True, stop=False), ["minvT", f"cj{b}"], [PF[4 + b]])
                                      M_(lambda e: e.matmul(psf[4 + b][0:64, 0:CW], minvT[:, j, 1, :], cj[b][:, 1, :], start=False, stop=True), ["minvT", f"cj{b}"], [PF[4 + b]])
                                      for (si, tok0, pmax, c0, w_, off) in segs:
                                          xcol = (0 if o == 0 else 512) + c0
                                          LD(xg[b][0:pmax, off:off + w_], XV_d[tok0:tok0 + pmax * 64, xcol:xcol + w_].rearrange("(p j) c -> p j c", j=64)[:, j, :], w=[f"xg{b}"])
                                      G_(lambda e: e.tensor_tensor(tg[b][:], src[:, j, :], skp[:], op=ALU.mult), [skey, "skp"], [f"tg{b}"])
                                      V_(lambda e: e.tensor_tensor(tg[b][:], psf[4 + b][0:64, 0:CW], tg[b][:], op=ALU.add), [PF[4 + b], f"tg{b}"], [f"tg{b}"])
                                      if o == 0:
                                          V_(lambda e: e.tensor_tensor(zz[:, j, :], tg[b][:], xg[b][:], op=ALU.mult), [f"tg{b}", f"xg{b}"], ["zz"])
                                      else:
                                          yb_ = (j // 4) % 2; jl = j % 4
                                          V_(lambda e: e.tensor_tensor(yh[yb_][:, jl, :], tg[b][:], xg[b][:], op=ALU.mult), [f"tg{b}", f"xg{b}"], [f"yh{yb_}"])
                                          if jl == 3:
                                              for (si, tok0, pmax, c0, w_, off) in segs:
                                                  LD(YH_d[tok0:tok0 + pmax * 64, c0:c0 + w_].rearrange("(p j) c -> p j c", j=64)[:, j - 3:j + 1, :], yh[yb_][0:pmax, :, off:off + w_], [f"yh{yb_}"], ["YH_d"])
                                  P.barrier()
                  P.barrier()

              scope(None)
              if stop == 5:
                  break
              scope(f"L{li}_merge")
              with ExitStack() as es:
                  sb = lambda n, s, d: es.enter_context(nc.sbuf_tensor(uniq(n), list(s), d))
                  wap = sb("wap", [128, 8, D], BF16); whp = sb("whp", [128, 4, D], BF16); wo = sb("wo", [128, 8, D], BF16)
                  rw = sb("rw", [128, 8, NE], F32); rbb = sb("rbb", [128, NE], F32)
                  ya = [sb(f"ya{i}", [128, 8, 512], BF16) for i in range(2)]
                  yht = [sb(f"yht{i}", [128, 512], BF16) for i in range(2)]
                  yhT = sb("yhT", [128, 4, 512], BF16)
                  gt = [sb("gt0", [128, 16, 512], BF16)] * 2
                  mT = sb("mT", [128, 8, 512], BF16)
                  m1 = [sb(f"m1{i}", [128, 512], F32) for i in range(2)]
                  m2 = [sb(f"m2{i}", [128, 512], F32) for i in range(2)]
                  xt = [sb(f"xt{i}", [128, D], F32) for i in range(2)]
                  xo = [sb(f"xo{i}", [128, D], F32) for i in range(2)]
                  junk = sb("junk", [128, D], BF16)
                  st = [sb(f"st{i}", [128, 4], F32) for i in range(2)]
                  xn = [sb(f"xn{i}", [128, D], F32) for i in range(2)]
                  h32 = sb("h32", [128, 8, 128], F32)
                  h2b = sb("h2b", [128, 8, 512], BF16)
                  lg = sb("lg", [128, NE], F32); sc_ = sb("sc_", [128, NE], F32); bi_ = sb("bi_", [128, NE], F32)
                  t8 = sb("t8", [128, 8, 8], F32); gs = sb("gs", [128, 8], F32); gm = sb("gm", [128, 8], F32)
                  sm = sb("sm", [128, 4], F32); em = sb("em", [128, NE], F32); gate = [sb(f"gate{i}", [128, NE], F32) for i in range(2)]
                  LDC(wap[:], I["w_ap"][li].rearrange("(kt k) n -> k kt n", k=128), w=["wap"])
                  LDC(whp[:], I["w_hp"][li].rearrange("(kt k) n -> k kt n", k=128), w=["whp"])
                  LDC(wo[:], I["w_out"][li].rearrange("(kt k) n -> k kt n", k=128), w=["wo"])
                  LD(rw[:], I["router_w"].rearrange("(kt k) n -> k kt n", k=128), w=["rw"]); LD(rbb[:], I["router_bb"][:, :], w=["rbb"])
                  ntiles = NT if not last else 32
                  nblk = (ntiles * 128 + 511) // 512
                  for blk in range(nblk):
                      tts = list(range(blk * 4, min(blk * 4 + 4, ntiles)))
                      ntok = len(tts) * 128
                      t0 = blk * 512
                      b = blk % 2
                      LD(ya[b][:, :, 0:ntok], YA_d[:, t0:t0 + ntok].rearrange("(h p) t -> p h t", p=128), w=[f"ya{b}"])
                      LD(gt[b][:, :, 0:ntok], G_d[:, t0:t0 + ntok].rearrange("(h p) t -> p h t", p=128), w=["gt"])
                      for ti, tt in enumerate(tts):
                          yb_ = tt % 2
                          LD(yht[yb_][:], YH_d[tt * 128:(tt + 1) * 128, :], w=[f"yht{yb_}"])
                          for ct in range(4):
                              M_(lambda e: e.transpose(psb[0][:, ct * 128:(ct + 1) * 128], yht[yb_][:, ct * 128:(ct + 1) * 128], idb[:]), [f"yht{yb_}", "idb"], [PB[0]])
                          A_(lambda e: e.activation(out=yhT[:, :, ti * 128:(ti + 1) * 128], in_=psb[0][:, 0:512].rearrange("p (c t) -> p c t", t=128), func=AF.Identity), [PB[0]], ["yhT"])
                      for ncn in range(8):
                          pa = psf[ncn % 2]; pak = PF[ncn % 2]; ph = psf[2 + ncn % 2]; phk = PF[2 + ncn % 2]
                          mb = ncn % 2
                          for kt in range(8):
                              M_(lambda e: e.matmul(pa[:, 0:ntok], wap[:, kt, ncn * 128:(ncn + 1) * 128], ya[b][:, kt, 0:ntok], start=(kt == 0), stop=(kt == 7)), ["wap", f"ya{b}"], [pak])
                          for ct in range(4):
                              M_(lambda e: e.matmul(ph[:, 0:ntok], whp[:, ct, ncn * 128:(ncn + 1) * 128], yhT[:, ct, 0:ntok], start=(ct == 0), stop=(ct == 3)), ["whp", "yhT"], [phk])
                          V_(lambda e: e.tensor_tensor(m1[mb][:, 0:ntok], pa[:, 0:ntok], gt[b][:, ncn, 0:ntok], op=ALU.mult), [pak, "gt"], [f"m1{mb}"])
                          V_(lambda e: e.tensor_tensor(m2[mb][:, 0:ntok], ph[:, 0:ntok], gt[b][:, 8 + ncn, 0:ntok], op=ALU.mult), [phk, "gt"], [f"m2{mb}"])
                          G_(lambda e: e.tensor_tensor(mT[:, ncn, 0:ntok], m1[mb][:, 0:ntok], m2[mb][:, 0:ntok], op=ALU.add), [f"m1{mb}", f"m2{mb}"], ["mT"])
                      for ti, tt in enumerate(tts):
                          m = 0 if tt < 32 else 1
                          xb_ = tt % 2
                          LD(xt[xb_][:], cur[tt * 128:(tt + 1) * 128, :], w=[f"xt{xb_}"])
                          for hf in range(2):
                              po = psf[4 + hf]; pok = PF[4 + hf]
                              for kt in range(8):
                                  M_(lambda e: e.matmul(po[:], mT[:, kt, ti * 128:(ti + 1) * 128], wo[:, kt, hf * 512:(hf + 1) * 512], start=(kt == 0), stop=(kt == 7)), ["mT", "wo"], [pok])
                              V_(lambda e: e.tensor_tensor(xo[xb_][:, hf * 512:(hf + 1) * 512], po[:], gb[:, m, hf * 512:(hf + 1) * 512], op=ALU.mult), [pok, "gb"], [f"xo{xb_}"])
                          G_(lambda e: e.tensor_tensor(xo[xb_][:], xo[xb_][:], xt[xb_][:], op=ALU.add), [f"xo{xb_}", f"xt{xb_}"], [f"xo{xb_}"])
                          LD(mid[tt * 128:(tt + 1) * 128, :], xo[xb_][:], [f"xo{xb_}"], ["mid"])
                          A_(lambda e: e.activation(out=junk[:], in_=xo[xb_][:], func=AF.Square, accum_out=st[xb_][:, 0:1]), [f"xo{xb_}"], ["junk", f"st{xb_}"])
                          A_(lambda e: e.activation(out=st[xb_][:, 1:2], in_=st[xb_][:, 0:1], func=AF.Sqrt, scale=1.0 / D, bias=epsc[:, 0:1]), [f"st{xb_}", "epsc"], [f"st{xb_}"])
                          V_(lambda e: e.reciprocal(st[xb_][:, 2:3], st[xb_][:, 1:2]), [f"st{xb_}"], [f"st{xb_}"])
                          V_(lambda e: e.tensor_scalar(xn[xb_][:], xo[xb_][:], st[xb_][:, 2:3], None, op0=ALU.mult), [f"xo{xb_}", f"st{xb_}"], [f"xn{xb_}"])
                          for dt in range(8):
                              M_(lambda e: e.transpose(psf[dt // 4][:, (dt % 4) * 128:(dt % 4 + 1) * 128], xn[xb_][:, dt * 128:(dt + 1) * 128], idf[:]), [f"xn{xb_}", "idf"], [PF[dt // 4]])
                          for dt in range(8):
                              A_(lambda e: e.activation(out=h32[:, dt, :], in_=psf[dt // 4][:, (dt % 4) * 128:(dt % 4 + 1) * 128], func=AF.Identity,
                                                        scale=scB[:, dt, m:m + 1], bias=mods[:, 24 + dt, m:m + 1]), [PF[dt // 4], "scB", "mods"], ["h32"])
                          G_(lambda e: e.tensor_scalar(h2b[:, :, ti * 128:(ti + 1) * 128], h32[:], 1.0, None, op0=ALU.mult), ["h32"], ["h2b"])
                          for dt in range(8):
                              M_(lambda e: e.matmul(psf[2][:, 0:NE], h32[:, dt, :], rw[:, dt, :], start=(dt == 0), stop=(dt == 7)), ["h32", "rw"], [PF[2]])
                          gb_ = tt % 2
                          A_(lambda e: e.activation(out=sc_[:], in_=psf[2][:, 0:NE], func=AF.Sigmoid), [PF[2]], ["sc_"])
                          V_(lambda e: e.tensor_tensor(bi_[:], sc_[:], rbb[:], op=ALU.add), ["sc_", "rbb"], ["bi_"])
                          for g in range(8):
                              V_(lambda e: e.max(out=t8[:, g, :], in_=bi_[:, g * 8:(g + 1) * 8]), ["bi_"], ["t8"])
                          V_(lambda e: e.tensor_tensor(gs[:], t8[:, :, 0], t8[:, :, 1], op=ALU.add), ["t8"], ["gs"])
                          V_(lambda e: e.tensor_reduce(sm[:, 0:1], gs[:], axis=AX.X, op=ALU.max), ["gs"], ["sm"])
                          V_(lambda e: e.tensor_scalar(gm[:], gs[:], sm[:, 0:1], None, op0=ALU.is_equal), ["gs", "sm"], ["gm"])
                          V_(lambda e: e.tensor_tensor(gs[:], gm[:], t8[:, :, 1], op=ALU.mult), ["gm", "t8"], ["gs"])
                          V_(lambda e: e.tensor_reduce(sm[:, 1:2], gs[:], axis=AX.X, op=ALU.add), ["gs"], ["sm"])
                          V_(lambda e: e.tensor_scalar(em[:], bi_[:], sm[:, 1:2], None, op0=ALU.is_ge), ["bi_", "sm"], ["em"])
                          V_(lambda e: e.tensor_tensor(em[:].rearrange("p (g k) -> p g k", k=8), em[:].rearrange("p (g k) -> p g k", k=8),
                                                       gm[:].unsqueeze(2).to_broadcast([128, 8, 8]), op=ALU.mult), ["em", "gm"], ["em"])
                          V_(lambda e: e.tensor_tensor(em[:], em[:], sc_[:], op=ALU.mult), ["em", "sc_"], ["em"])
                          V_(lambda e: e.tensor_reduce(sm[:, 2:3], em[:], axis=AX.X, op=ALU.add), ["em"], ["sm"])
                          V_(lambda e: e.reciprocal(sm[:, 3:4], sm[:, 2:3]), ["sm"], ["sm"])
                          V_(lambda e: e.tensor_scalar(gate[gb_][:], em[:], sm[:, 3:4], None, op0=ALU.mult), ["em", "sm"], [f"gate{gb_}"])
                          LD(GATE_d[tt * 128:(tt + 1) * 128, :], gate[gb_][:], [f"gate{gb_}"], ["GATE_d"])
                      LD(H2T_d[:, t0:t0 + ntok].rearrange("(kt p) t -> p kt t", p=128), h2b[:, :, 0:ntok], ["h2b"], ["H2T_d"])
              P.barrier()

              if stop == 6:
                  break
              scope(f"L{li}_moe")
              ntiles = NT if not last else 32
              with ExitStack() as es:
                  sb = lambda n, s, d: es.enter_context(nc.sbuf_tensor(uniq(n), list(s), d))
                  SBK = 1024
                  hT2 = sb("hT2", [128, 8, SBK], BF16)
                  acc = sb("acc", [128, SBK // 128, D], F32)
                  gat = sb("gat", [128, SBK // 128, NE], F32)
                  w1 = [sb(f"ew1{i}", [128, 8, 512], BF16) for i in range(2)]
                  w3 = [sb(f"ew3{i}", [128, 8, 512], BF16) for i in range(2)]
                  w2 = [sb(f"ew2{i}", [128, 4, D], BF16) for i in range(2)]
                  s1 = [sb(f"s1{i}", [128, 512], F32) for i in range(2)]
                  gT = [sb(f"gT{i}", [128, 4, 512], BF16) for i in range(2)]
                  xt = [sb(f"xt{i}", [128, D], F32) for i in range(2)]
                  xo = [sb(f"xo{i}", [128, D], F32) for i in range(2)]
                  junk = sb("junk", [128, D], BF16); st = [sb(f"st{i}", [128, 4], F32) for i in range(2)]
                  fnw = sb("fnw", [128, D], F32)
                  LD(fnw[:], I["fnw"][:, :], w=["fnw"])
                  ntok_all = ntiles * 128
                  for s0 in range(0, ntok_all, SBK):
                      sn = min(SBK, ntok_all - s0)
                      stl = sn // 128
                      LD(hT2[:, :, 0:sn], H2T_d[:, s0:s0 + sn].rearrange("(kt p) t -> p kt t", p=128), w=["hT2"])
                      LD(gat[:, 0:stl, :], GATE_d[s0:s0 + sn, :].rearrange("(t p) e -> p t e", p=128), w=["gat"])
                      V_(lambda e: e.memset(acc[:], 0.0), w=["acc"])
                      if do_moe:
                          blist = [(ex, b0, min(512, sn - b0)) for ex in range(NE) for b0 in range(0, sn, 512)]

                          def moe_a(bi):
                              ex, b0, bn = blist[bi]
                              wb = ex % 2; gbi = bi % 2
                              if b0 == 0:
                                  LDC(w1[wb][:], I["exp_w1"][li, ex].rearrange("(kt k) n -> k kt n", k=128), w=[f"ew1{wb}"])
                                  LDC(w3[wb][:], I["exp_w3"][li, ex].rearrange("(kt k) n -> k kt n", k=128), w=[f"ew3{wb}"])
                                  LDC(w2[wb][:], I["exp_w2"][li, ex].rearrange("(kt k) n -> k kt n", k=128), w=[f"ew2{wb}"])
                              for fcn in range(4):
                                  p1 = psf[fcn % 2]; p1k = PF[fcn % 2]; p3 = psf[2 + fcn % 2]; p3k = PF[2 + fcn % 2]
                                  sbi = fcn % 2
                                  for kt in range(8):
                                      M_(lambda e: e.matmul(p1[:, 0:bn], w1[wb][:, kt, fcn * 128:(fcn + 1) * 128], hT2[:, kt, b0:b0 + bn], start=(kt == 0), stop=(kt == 7)), [f"ew1{wb}", "hT2"], [p1k])
                                  for kt in range(8):
                                      M_(lambda e: e.matmul(p3[:, 0:bn], w3[wb][:, kt, fcn * 128:(fcn + 1) * 128], hT2[:, kt, b0:b0 + bn], start=(kt == 0), stop=(kt == 7)), [f"ew3{wb}", "hT2"], [p3k])
                                  A_(lambda e: e.activation(out=s1[sbi][:, 0:bn], in_=p1[:, 0:bn], func=AF.Silu), [p1k], [f"s1{sbi}"])
                                  V_(lambda e: e.tensor_tensor(gT[gbi][:, fcn, 0:bn], p3[:, 0:bn], s1[sbi][:, 0:bn], op=ALU.mult), [p3k, f"s1{sbi}"], [f"gT{gbi}"])

                          def moe_b(bi):
                              ex, b0, bn = blist[bi]
                              wb = ex % 2; gbi = bi % 2
                              for ti in range(bn // 128):
                                  tl = (b0 // 128) + ti
                                  for hf in range(2):
                                      po = psf[4 + hf]; pok = PF[4 + hf]
                                      for ft in range(4):
                                          M_(lambda e: e.matmul(po[:], gT[gbi][:, ft, ti * 128:(ti + 1) * 128], w2[wb][:, ft, hf * 512:(hf + 1) * 512], start=(ft == 0), stop=(ft == 3)), [f"gT{gbi}", f"ew2{wb}"], [pok])
                                      V_(lambda e: e.scalar_tensor_tensor(out=acc[:, tl, hf * 512:(hf + 1) * 512], in0=po[:], scalar=gat[:, tl, ex:ex + 1],
                                                                          in1=acc[:, tl, hf * 512:(hf + 1) * 512], op0=ALU.mult, op1=ALU.add), [pok, "gat", "acc"], ["acc"])

                          moe_a(0)
                          for bi in range(len(blist)):
                              if bi + 1 < len(blist):
                                  moe_a(bi + 1)
                              moe_b(bi)
                      for tl in range(stl):
                          tt = s0 // 128 + tl
                          m = 0 if tt < 32 else 1
                          xb_ = tt % 2
                          LD(xt[xb_][:], mid[tt * 128:(tt + 1) * 128, :], w=[f"xt{xb_}"])
                          V_(lambda e: e.tensor_tensor(xo[xb_][:], acc[:, tl, :], gb[:, 2 + m, :], op=ALU.mult), ["acc", "gb"], [f"xo{xb_}"])
                          G_(lambda e: e.tensor_tensor(xo[xb_][:], xo[xb_][:], xt[xb_][:], op=ALU.add), [f"xo{xb_}", f"xt{xb_}"], [f"xo{xb_}"])
                          if not last:
                              LD(cur[tt * 128:(tt + 1) * 128, :], xo[xb_][:], [f"xo{xb_}"], ["cur"])
                          else:
                              A_(lambda e: e.activation(out=junk[:], in_=xo[xb_][:], func=AF.Square, accum_out=st[xb_][:, 0:1]), [f"xo{xb_}"], ["junk", f"st{xb_}"])
                              A_(lambda e: e.activation(out=st[xb_][:, 1:2], in_=st[xb_][:, 0:1], func=AF.Sqrt, scale=1.0 / D, bias=epsc[:, 0:1]), [f"st{xb_}", "epsc"], [f"st{xb_}"])
                              V_(lambda e: e.reciprocal(st[xb_][:, 2:3], st[xb_][:, 1:2]), [f"st{xb_}"], [f"st{xb_}"])
                              V_(lambda e: e.scalar_tensor_tensor(out=xt[xb_][:], in0=xo[xb_][:], scalar=st[xb_][:, 2:3], in1=fnw[:], op0=ALU.mult, op1=ALU.mult),
                                 [f"xo{xb_}", f"st{xb_}", "fnw"], [f"xt{xb_}"])
                              LD(OUT[tt * 128:(tt + 1) * 128, :], xt[xb_][:], [f"xt{xb_}"], ["OUT"])
              P.barrier()
          except _Stop:
              break
        P.dead = False
        P.barrier()
        nops = P.nops
    return nc, nops


def make_in_maps(inputs, cores=range(8)):
    f = lambda a: np.ascontiguousarray(np.asarray(a, dtype=np.float32))
    consts = _consts()
    shared = {}
    shared["w_ada"] = f(inputs["w_ada"])
    shared["b_adaT"] = f(np.asarray(inputs["b_ada"]).reshape(2, 48, 128).transpose(0, 2, 1))
    shared["n1T"] = f(np.asarray(inputs["norm1_w"]).reshape(2, 8, 128).transpose(0, 2, 1))
    shared["n2T"] = f(np.asarray(inputs["norm2_w"]).reshape(2, 8, 128).transpose(0, 2, 1))
    shared["w_in"] = f(inputs["w_in"])
    qw = np.asarray(inputs["q_norm_w"]); kw = np.asarray(inputs["k_norm_w"])
    qkw = np.concatenate([np.tile(qw, (1, 8)), np.tile(kw, (1, 2))], 1)
    shared["qkw"] = f(np.broadcast_to(qkw[:, None, :], (2, 128, 1280)))
    cw = np.asarray(inputs["hy_conv_w"]).reshape(2, 3 * 1536)
    shared["convw"] = f(np.broadcast_to(cw[:, None, :], (2, 128, 3 * 1536)))
    shared["convb"] = f(np.broadcast_to(np.asarray(inputs["hy_conv_b"])[:, None, :], (2, 128, 1536)))
    shared["pe_w1"] = f(inputs["hy_pe_w1"]); shared["pe_w2"] = f(inputs["hy_pe_w2"]); shared["pe_w3"] = f(inputs["hy_pe_w3"])
    shared["pe_v"] = f(np.stack([np.asarray(inputs["hy_freq1"]), np.asarray(inputs["hy_pe_b1"]),
                                 np.asarray(inputs["hy_freq2"]), np.asarray(inputs["hy_pe_b2"])], -1))
    sk = np.asarray(inputs["hy_skip"]).reshape(2, 1024)
    shared["skipb"] = f(np.broadcast_to(sk[:, None, :], (2, 64, 1024)))
    shared["w_ap"] = f(inputs["w_att_proj"]); shared["w_hp"] = f(inputs["w_hy_proj"]); shared["w_out"] = f(inputs["w_out"])
    shared["router_w"] = f(inputs["router_w"])
    shared["router_bb"] = f(np.broadcast_to(np.asarray(inputs["router_b"])[None, :], (128, NE)))
    shared["exp_w1"] = f(inputs["exp_w1"]); shared["exp_w3"] = f(inputs["exp_w3"]); shared["exp_w2"] = f(inputs["exp_w2"])
    shared["fnw"] = f(np.broadcast_to(np.asarray(inputs["final_norm_w"])[None, :], (128, D)))
    for k, v in consts.items():
        shared["c_" + k] = f(v)
    x = np.asarray(inputs["x"]); c = np.asarray(inputs["c"]); ctx = np.asarray(inputs["ctx"]); c_ctx = np.asarray(inputs["c_ctx"])
    maps = []
    for b in cores:
        m = dict(shared)
        m["x"] = f(x[b]); m["ctx"] = f(ctx[b])
        cc = np.stack([c[b].reshape(8, 128).T, c_ctx.reshape(8, 128).T], -1)
        m["cc"] = f(cc)
        maps.append(m)
    return maps


_NC_CACHE = {}


def kernel(**inputs):
    if "nc" not in _NC_CACHE:
        _NC_CACHE["nc"] = build()[0]
    nc = _NC_CACHE["nc"]
    maps = make_in_maps(inputs)
    res = run_bass_kernel_spmd(nc, maps, core_ids=list(range(8)))
    out = np.stack([np.asarray(r["out"]) for r in res.results], 0).astype(np.float32)
    return out
```

```python
import math
import numpy as np
import concourse.bass as bass
import concourse.mybir as mybir
from concourse.bass_utils import run_bass_kernel_spmd
from contextlib import ExitStack

F32 = mybir.dt.float32
BF16 = mybir.dt.bfloat16
AF = mybir.ActivationFunctionType
ALU = mybir.AluOpType
AX = mybir.AxisListType

S = 4096
C = 256
T = S + C
NT = T // 128
D = 1024
NE = 64
EPS = 1e-6
NFFT = 8192
CW = 256
PI = float(np.pi)


class _Stop(Exception):
    pass


class Prog:
    EPOCH = 30000
    NDMA = 24

    def __init__(self, nc, es):
        self.nc = nc
        self.es = es
        self.eng = {"pe": nc.tensor, "act": nc.scalar, "dve": nc.vector, "pool": nc.gpsimd, "sp": nc.sync}
        self.sems = {e: [es.enter_context(nc.semaphore(f"s_{e}_0"))] for e in self.eng}
        self.cnt = {e: 0 for e in self.eng}
        self.ep = {e: 0 for e in self.eng}
        self.dsem = [es.enter_context(nc.semaphore(f"s_dma_{i}")) for i in range(self.NDMA)]
        self.dcnt = [0] * self.NDMA
        self.dnext = 0
        self.waited = {e: {} for e in self.eng}
        self.W = {}
        self.R = {}
        self.nops = 0
        self.dead = False

    def _wait(self, e, tok):
        sem, val, src = tok
        if src == e and e == "pe":
            return
        w = self.waited[e]
        k = id(sem)
        if w.get(k, 0) >= val:
            return
        self.eng[e].wait_ge(sem, val)
        w[k] = val

    def _deps(self, reads, writes):
        toks = []
        for k in reads:
            toks.extend(self.W.get(k, {}).values())
        for k in writes:
            toks.extend(self.W.get(k, {}).values())
            toks.extend(self.R.get(k, {}).values())
        return toks

    def _commit(self, tok, reads, writes):
        sid = id(tok[0])
        for k in reads:
            d = self.R.setdefault(k, {})
            if sid not in d or d[sid][1] < tok[1]:
                d[sid] = tok
        for k in writes:
            d = self.W.setdefault(k, {})
            if sid not in d or d[sid][1] < tok[1]:
                d[sid] = tok

    def op(self, e, fn, reads=(), writes=()):
        if self.dead:
            return None
        for t in self._deps(reads, writes):
            self._wait(e, t)
        if self.cnt[e] >= self.EPOCH:
            self.ep[e] += 1
            self.sems[e].append(self.es.enter_context(self.nc.semaphore(f"s_{e}_{self.ep[e]}")))
            self.cnt[e] = 0
        inst = fn(self.eng[e])
        self.cnt[e] += 1
        sem = self.sems[e][-1]
        inst.then_inc(sem, 1)
        tok = (sem, self.cnt[e], e)
        self._commit(tok, reads, writes)
        self.nops += 1
        return tok

    def dma(self, q, fn, reads=(), writes=()):
        if self.dead:
            return None
        for t in self._deps(reads, writes):
            self._wait(q, t)
        i = self.dnext
        self.dnext = (self.dnext + 1) % self.NDMA
        sem = self.dsem[i]
        if self.dcnt[i] > 0:
            self._wait(q, (sem, 16 * self.dcnt[i], None))
        inst = fn(self.eng[q])
        self.dcnt[i] += 1
        inst.then_inc(sem, 16)
        tok = (sem, 16 * self.dcnt[i], None)
        self._commit(tok, reads, writes)
        self.nops += 1
        return tok

    def barrier(self):
        if self.dead:
            return
        toks = []
        for e in self.eng:
            if self.cnt[e] > 0:
                toks.append((self.sems[e][-1], self.cnt[e], e))
        for i in range(self.NDMA):
            if self.dcnt[i] > 0:
                toks.append((self.dsem[i], 16 * self.dcnt[i], None))
        for e in self.eng:
            for t in toks:
                if t[2] != e:
                    self._wait(e, t)
        self.W = {}
        self.R = {}


def _consts():
    c = {}
    c["ident"] = np.eye(128, dtype=np.float32)
    t = np.arange(S)
    row = (t // 64).astype(np.float32)
    col = (t % 64).astype(np.float32)
    n = 32
    inv = (10000.0 ** (-np.arange(n, dtype=np.float32) / n)).astype(np.float32)
    ang = np.concatenate([row[:, None] * inv, col[:, None] * inv], -1).astype(np.float32)
    cs = np.ones((T, 64), np.float32)
    sn = np.zeros((T, 64), np.float32)
    cs[:S] = np.cos(ang)
    sn[:S] = np.sin(ang)
    c["ropec"] = np.ascontiguousarray(cs.reshape(NT, 128, 64).transpose(1, 0, 2))
    c["ropes"] = np.ascontiguousarray(sn.reshape(NT, 128, 64).transpose(1, 0, 2))
    p = np.arange(128, dtype=np.float64)[:, None, None]
    j = np.arange(64, dtype=np.float64)[None, :, None]
    f1 = np.arange(128, dtype=np.float64)[None, None, :]
    M = np.exp(-2j * np.pi * (p * f1 / 128.0 + j * f1 / NFFT))
    Mri = np.stack([M.real, M.imag], 2)
    c["mlo"] = np.ascontiguousarray(Mri[:64]).astype(np.float32)
    c["mhi"] = np.ascontiguousarray(Mri[64:]).astype(np.float32)
    Mi = np.stack([M.real[:64], M.imag[:64]], 0) / NFFT
    c["minvT"] = np.ascontiguousarray(Mi.transpose(3, 2, 0, 1)).astype(np.float32)
    jj = np.arange(64, dtype=np.float64)[:, None]
    f2 = np.arange(64, dtype=np.float64)[None, :]
    Wre = np.cos(2 * np.pi * jj * f2 / 64.0)
    Wim = -np.sin(2 * np.pi * jj * f2 / 64.0)
    cat = lambda a, b: np.concatenate([a, b], 1)
    st = [cat(Wre, Wim), cat(-Wim, Wre), cat(Wim, Wre), cat(Wre, -Wim),
          cat(Wre, Wre), cat(-Wim, -Wim), cat(-Wim, Wim), cat(-Wre, Wre)]
    c["w64"] = np.ascontiguousarray(np.stack(st, 1)).astype(np.float32)
    wi = np.zeros((128, 128))
    wi[:64, :64] = Wre.T
    wi[:64, 64:] = -Wim.T
    wi[64:, :64] = Wim.T
    wi[64:, 64:] = Wre.T
    c["wi1"] = wi.astype(np.float32)
    deltas = np.abs(np.linspace(math.log(1e-2) / 1.5, math.log(1e-2) / 0.3, 512, dtype=np.float32))
    c["delta"] = np.ascontiguousarray(np.broadcast_to(deltas[None, :], (64, 512))).astype(np.float32)

    def zfeat(pos, L):
        t01 = (np.linspace(0.0, 1.0, L, dtype=np.float32))[pos][:, None]
        posf = pos.astype(np.float32)[:, None]
        bands = np.linspace(1e-4, 15, 16, dtype=np.float32)[None, :]
        f = (2.0 * math.pi * posf * bands / L).astype(np.float32)
        return np.concatenate([t01, np.cos(f), -np.sin(f)], -1).astype(np.float32), t01[:, 0]

    for name, L in (("lat", S), ("ctx", C)):
        q = np.arange(4096)
        vf = q < L
        posf = np.where(vf, q, 0)
        zf, t01f = zfeat(posf, L)
        d = 4096 - q
        vb = (d >= 1) & (d < L)
        posb = np.where(vb, d, 0)
        zb, t01b = zfeat(posb, L)
        c[f"z_{name}"] = np.ascontiguousarray(np.stack([zf.T, zb.T], 0))
        c[f"nt_{name}"] = np.ascontiguousarray(np.stack([-t01f.reshape(64, 64), -t01b.reshape(64, 64)], 0))
        c[f"vm_{name}"] = np.ascontiguousarray(np.stack([vf.reshape(64, 64), vb.reshape(64, 64)], 0).astype(np.float32))
    return c


_CONST_SHAPES = None


def build(debug=(), nlayers=2, do_moe=True, do_hyena=True, do_attn=True, stop=99, do_scopes=False):
    nc = bass.Bass("TRN2", target_bir_lowering=False)
    consts = _consts()
    _u = [0]

    def uniq(n):
        _u[0] += 1
        return f"t{_u[0]}_{n}"

    def din(name, shape, dt=F32):
        return nc.dram_tensor(name, list(shape), dt, kind="ExternalInput").ap()

    def dscr(name, shape, dt):
        kind = "ExternalOutput" if name in debug else "Internal"
        return nc.dram_tensor(name, list(shape), dt, kind=kind).ap()

    I = {}
    I["x"] = din("x", [S, D]); I["ctx"] = din("ctx", [C, D]); I["cc"] = din("cc", [128, 8, 2])
    I["w_ada"] = din("w_ada", [2, D, 6 * D]); I["b_adaT"] = din("b_adaT", [2, 128, 48])
    I["n1T"] = din("n1T", [2, 128, 8]); I["n2T"] = din("n2T", [2, 128, 8])
    I["w_in"] = din("w_in", [2, D, 5120])
    I["qkw"] = din("qkw", [2, 128, 1280])
    I["convw"] = din("convw", [2, 128, 3 * 1536]); I["convb"] = din("convb", [2, 128, 1536])
    I["pe_w1"] = din("pe_w1", [2, 33, 64]); I["pe_w2"] = din("pe_w2", [2, 64, 64]); I["pe_w3"] = din("pe_w3", [2, 64, 2048])
    I["pe_v"] = din("pe_v", [2, 64, 4])
    I["skipb"] = din("skipb", [2, 64, 1024])
    I["w_ap"] = din("w_ap", [2, D, D]); I["w_hp"] = din("w_hp", [2, 512, D]); I["w_out"] = din("w_out", [2, D, D])
    I["router_w"] = din("router_w", [D, NE]); I["router_bb"] = din("router_bb", [128, NE])
    if do_moe:
        I["exp_w1"] = din("exp_w1", [2, NE, D, 512]); I["exp_w3"] = din("exp_w3", [2, NE, D, 512]); I["exp_w2"] = din("exp_w2", [2, NE, 512, D])
    I["fnw"] = din("fnw", [128, D])
    for k, v in consts.items():
        I[k] = din("c_" + k, v.shape)
    OUT = nc.dram_tensor("out", [S, D], F32, kind="ExternalOutput").ap()

    X0 = dscr("X0", [T, D], F32); X1 = dscr("X1", [T, D], F32)
    QT_d = dscr("QT_d", [8, 128, T], BF16); KT_d = dscr("KT_d", [2, 128, T], BF16)
    V_d = dscr("V_d", [T, 256], BF16); U_d = dscr("U_d", [T, 1536], F32)
    G_d = dscr("G_d", [2048, T], BF16); YA_d = dscr("YA_d", [D, T], BF16)
    XV_d = dscr("XV_d", [T, 1536], F32)
    A_d = dscr("A_d", [128, 64, 2, 512], BF16); C_d = dscr("C_d", [128, 128, CW], BF16)
    KH_d = dscr("KH_d", [2, 2, 128, 2, 128, 512], BF16)
    YH_d = dscr("YH_d", [T, 512], BF16)
    A2_d = dscr("A2_d", [128, 64, 2, CW], BF16)
    H2T_d = dscr("H2T_d", [D, T], BF16); GATE_d = dscr("GATE_d", [T, NE], F32)
    HT_d = dscr("HT_d", [D, T], BF16)

    with ExitStack() as es0:
        P = Prog(nc, es0)
        V_ = lambda fn, r=(), w=(): P.op("dve", fn, r, w)
        A_ = lambda fn, r=(), w=(): P.op("act", fn, r, w)
        M_ = lambda fn, r=(), w=(): P.op("pe", fn, r, w)
        G_ = lambda fn, r=(), w=(): P.op("dve", fn, r, w)
        GP = lambda fn, r=(), w=(): P.op("dve", fn, r, w)
        def LD(out, in_, r=(), w=()):
            q = "pool" if (str(out.space) == "DRAM" and str(in_.space) != "DRAM") else "sp"
            return P.dma(q, lambda e: e.dma_start(out=out, in_=in_), r, w)
        LDC = lambda out, in_, r=(), w=(): P.dma("pool", lambda e: e.dma_start(out=out, in_=in_), r, w)

        psf = [es0.enter_context(nc.psum_tensor(f"psf{i}", [128, 512], F32)) for i in range(6)]
        psb = [es0.enter_context(nc.psum_tensor(f"psb{i}", [128, 1024], BF16)) for i in range(2)]
        PF = [f"psf{i}" for i in range(6)]
        PB = [f"psb{i}" for i in range(2)]

        def sbp(name, shape, dt):
            return es0.enter_context(nc.sbuf_tensor(uniq(name), list(shape), dt))
        idf = sbp("idf", [128, 128], F32); idb = sbp("idb", [128, 128], BF16)
        onesf = sbp("onesf", [128, 128], F32); onesb = sbp("onesb", [128, 128], BF16)
        epsc = sbp("epsc", [128, 1], F32)
        mods = sbp("mods", [128, 48, 2], F32)
        scA = sbp("scA", [128, 8, 2], F32); scB = sbp("scB", [128, 8, 2], F32)
        gb = sbp("gb", [128, 4, D], F32)
        LD(idf[:], I["ident"][:, :], w=["idf"]); LDC(idb[:], I["ident"][:, :], w=["idb"])
        V_(lambda e: e.memset(onesf[:], 1.0), w=["onesf"]); V_(lambda e: e.memset(onesb[:], 1.0), w=["onesb"])
        V_(lambda e: e.memset(epsc[:], EPS), w=["epsc"])
        LD(X0[0:S, :], I["x"][:, :], w=["X0"]); LD(X0[S:T, :], I["ctx"][:, :], w=["X0"])
        P.barrier()

        _sc = [None]

        def scope(name):
            if not do_scopes:
                return
            if _sc[0] is not None:
                nc.leave_named_scope(_sc[0]) if False else _sc[0].__exit__(None, None, None)
                _sc[0] = None
            if name is not None:
                cm = nc.named_scope(name)
                cm.__enter__()
                _sc[0] = cm

        def chk(x):
            if stop == x and not P.dead:
                P.barrier()
                P.dead = True

        for li in range(nlayers):
          try:
              last = li == 1
              if stop == 0:
                  break
              ntl = 32 if False else NT

              scope(f"L{li}_adaln")
              with ExitStack() as es:
                  sb = lambda n, s, d: es.enter_context(nc.sbuf_tensor(uniq(n), list(s), d))
                  ccs = sb("ccs", [128, 8, 2], F32)
                  wa = [sb(f"wa{i}", [128, 8, 512], F32) for i in range(2)]
                  bT = sb("bT", [128, 48], F32); n1 = sb("n1", [128, 8], F32); n2 = sb("n2", [128, 8], F32)
                  dg = sb("dg", [128, 128], F32); tmp = sb("tmpa", [128, 8, 2], F32)
                  LD(ccs[:], I["cc"][:, :, :], w=["ccs"])
                  A_(lambda e: e.activation(out=ccs[:], in_=ccs[:], func=AF.Silu), ["ccs"], ["ccs"])
                  LD(bT[:], I["b_adaT"][li], w=["bT"]); LD(n1[:], I["n1T"][li], w=["n1"]); LD(n2[:], I["n2T"][li], w=["n2"])
                  wsrc = I["w_ada"][li].rearrange("(kt k) n -> k kt n", k=128)
                  for g in range(12 if stop != 0.3 else 0):
                      w = wa[g % 2]
                      LD(w[:], wsrc[:, :, g * 512:(g + 1) * 512], w=[f"wa{g % 2}"])
                      for sub in range(4):
                          ch = g * 4 + sub
                          for kt in range(8):
                              M_(lambda e: e.matmul(psf[0][:, ch * 2:ch * 2 + 2], w[:, kt, sub * 128:(sub + 1) * 128], ccs[:, kt, :],
                                                    start=(kt == 0), stop=(kt == 7)), [f"wa{g % 2}", "ccs"], [PF[0]])
                  if stop in (0.3, 0.5):
                      break
                  V_(lambda e: e.tensor_tensor(mods[:], psf[0][:, 0:96].rearrange("p (c m) -> p c m", m=2),
                                               bT[:].unsqueeze(2).to_broadcast([128, 48, 2]), op=ALU.add), [PF[0], "bT"], ["mods"])
                  V_(lambda e: e.tensor_scalar(tmp[:], mods[:, 8:16, :], 1.0, None, op0=ALU.add), ["mods"], ["tmpa"])
                  V_(lambda e: e.tensor_tensor(scA[:], tmp[:], n1[:].unsqueeze(2).to_broadcast([128, 8, 2]), op=ALU.mult), ["tmpa", "n1"], ["scA"])
                  V_(lambda e: e.tensor_scalar(tmp[:], mods[:, 32:40, :], 1.0, None, op0=ALU.add), ["mods", "scA"], ["tmpa"])
                  V_(lambda e: e.tensor_tensor(scB[:], tmp[:], n2[:].unsqueeze(2).to_broadcast([128, 8, 2]), op=ALU.mult), ["tmpa", "n2"], ["scB"])
                  if stop == 0.7:
                      break
                  for gi, base in enumerate((16, 40)):
                      for m in range(2):
                          for dt in range(8):
                              V_(lambda e: e.tensor_scalar(dg[:], idf[:], mods[:, base + dt, m:m + 1], None, op0=ALU.mult), ["idf", "mods"], ["dg"])
                              M_(lambda e: e.matmul(psf[1][:, 0:128], onesf[:], dg[:], start=True, stop=True), ["onesf", "dg"], [PF[1]])
                              A_(lambda e: e.activation(out=gb[:, gi * 2 + m, dt * 128:(dt + 1) * 128], in_=psf[1][:, 0:128], func=AF.Identity), [PF[1]], ["gb"])
              P.barrier()

              if stop == 1:
                  break
              scope(f"L{li}_inproj")
              cur, mid = X0, X1
              with ExitStack() as es:
                  sb = lambda n, s, d: es.enter_context(nc.sbuf_tensor(uniq(n), list(s), d))
                  wq = sb("wq", [128, 8, 3072], BF16)
                  qkw = sb("qkw", [128, 10, 128], F32)
                  rc = sb("rc", [128, 2, 64], F32); rs = sb("rs", [128, 2, 64], F32)
                  xt = [sb(f"xt{i}", [128, D], F32) for i in range(2)]
                  xn = [sb(f"xn{i}", [128, D], BF16) for i in range(2)]
                  junk = sb("junk", [128, 1280], BF16)
                  st = [sb(f"st{i}", [128, 4], F32) for i in range(2)]
                  hT = [sb(f"hT{i}", [128, 8, 512], BF16) for i in range(2)]
                  qs = sb("qs", [128, 1280], F32); q2 = sb("q2", [128, 1280], F32)
                  hs = sb("hs", [128, 16], F32)
                  r1 = sb("r1", [128, 10, 64], F32); r2 = sb("r2", [128, 10, 64], F32)
                  qr = [sb(f"qr{i}", [128, 10, 128], BF16) for i in range(2)]
                  qT = [sb(f"qT{i}", [128, 10, 512], BF16) for i in range(2)]
                  vsb = [sb(f"vsb{i}", [128, 256], BF16) for i in range(2)]
                  usb = [sb(f"usb{i}", [128, 1536], F32) for i in range(2)]
                  wsrc = I["w_in"][li].rearrange("(kt k) n -> k kt n", k=128)
                  for cgi in range(3):
                      LDC(wq[:, :, cgi * 1024:(cgi + 1) * 1024], wsrc[:, :, cgi * 1024:(cgi + 1) * 1024], w=["wq"])
                  LD(qkw[:].rearrange("p a b -> p (a b)"), I["qkw"][li], w=["qkw"])
                  nblk = (T + 511) // 512
                  for blk in range(nblk):
                      tts = list(range(blk * 4, min(blk * 4 + 4, NT)))
                      ntok = len(tts) * 128
                      hb = blk % 2
                      for ti, tt in enumerate(tts):
                          m = 0 if tt < 32 else 1
                          b = tt % 2
                          LD(xt[b][:], cur[tt * 128:(tt + 1) * 128, :], ["X0", "X1"] if False else [], [f"xt{b}"])
                          A_(lambda e: e.activation(out=junk[:, 0:D], in_=xt[b][:], func=AF.Square, accum_out=st[b][:, 0:1]), [f"xt{b}"], ["junk", f"st{b}"])
                          A_(lambda e: e.activation(out=st[b][:, 1:2], in_=st[b][:, 0:1], func=AF.Sqrt, scale=1.0 / D, bias=epsc[:, 0:1]), [f"st{b}", "epsc"], [f"st{b}"])
                          V_(lambda e: e.reciprocal(st[b][:, 2:3], st[b][:, 1:2]), [f"st{b}"], [f"st{b}"])
                          V_(lambda e: e.tensor_scalar(xn[b][:], xt[b][:], st[b][:, 2:3], None, op0=ALU.mult), [f"xt{b}", f"st{b}"], [f"xn{b}"])
                          for dt in range(8):
                              M_(lambda e: e.transpose(psb[0][:, dt * 128:(dt + 1) * 128], xn[b][:, dt * 128:(dt + 1) * 128], idb[:]), [f"xn{b}", "idb"], [PB[0]])
                          for dt in range(8):
                              A_(lambda e: e.activation(out=hT[hb][:, dt, ti * 128:(ti + 1) * 128], in_=psb[0][:, dt * 128:(dt + 1) * 128], func=AF.Identity,
                                                        scale=scA[:, dt, m:m + 1], bias=mods[:, dt, m:m + 1]), [PB[0], "scA", "mods"], [f"hT{hb}"])
                          chk(1.2)
                      for ti, tt in enumerate(tts):
                          b = tt % 2
                          for cgi in range(6):
                              pb = PF[cgi % 3]; pt = psf[cgi % 3]
                              for kt in range(8):
                                  M_(lambda e: e.matmul(pt[:], hT[hb][:, kt, ti * 128:(ti + 1) * 128], wq[:, kt, cgi * 512:(cgi + 1) * 512],
                                                        start=(kt == 0), stop=(kt == 7)), [f"hT{hb}", "wq"], [pb])
                              chk(1.25)
                              if cgi == 1:
                                  chk(1.2515)
                              if cgi < 2:
                                  A_(lambda e: e.activation(out=qs[:, cgi * 512:(cgi + 1) * 512], in_=pt[:], func=AF.Identity), [pb], ["qs"])
                                  chk(1.251 + 0.001 * cgi)
                              elif cgi == 2:
                                  A_(lambda e: e.activation(out=qs[:, 1024:1280], in_=pt[:, 0:256], func=AF.Identity), [pb], ["qs"])
                                  chk(1.253)
                                  A_(lambda e: e.activation(out=vsb[b][:], in_=pt[:, 256:512], func=AF.Identity), [pb], [f"vsb{b}"])
                                  chk(1.26)
                                  LD(V_d[tt * 128:(tt + 1) * 128, :], vsb[b][:], [f"vsb{b}"], ["V_d"])
                                  chk(1.27)
                              else:
                                  if cgi % 2 == 0:
                                      A_(lambda e: e.activation(out=usb[b][:, (cgi - 3) * 512:(cgi - 2) * 512], in_=pt[:], func=AF.Identity), [pb], [f"usb{b}"])
                                  else:
                                      V_(lambda e: e.tensor_scalar(usb[b][:, (cgi - 3) * 512:(cgi - 2) * 512], pt[:], 1.0, None, op0=ALU.mult), [pb], [f"usb{b}"])
                          LD(U_d[tt * 128:(tt + 1) * 128, :], usb[b][:], [f"usb{b}"], ["U_d"])
                          chk(1.3)
                          q3 = qs[:].rearrange("p (h d) -> p h d", d=128)
                          A_(lambda e: e.activation(out=q2[:], in_=qs[:], func=AF.Square), ["qs"], ["q2"])
                          V_(lambda e: e.tensor_reduce(hs[:, 0:10], q2[:].rearrange("p (h d) -> p h d", d=128), axis=AX.X, op=ALU.add), ["q2"], ["hs"])
                          A_(lambda e: e.activation(out=hs[:, 0:10], in_=hs[:, 0:10], func=AF.Sqrt, scale=1.0 / 128, bias=epsc[:, 0:1]), ["hs", "epsc"], ["hs"])
                          V_(lambda e: e.reciprocal(hs[:, 0:10], hs[:, 0:10]), ["hs"], ["hs"])
                          V_(lambda e: e.tensor_tensor(q2[:].rearrange("p (h d) -> p h d", d=128), q3, hs[:, 0:10].unsqueeze(2).to_broadcast([128, 10, 128]), op=ALU.mult), ["qs", "hs"], ["q2"])
                          G_(lambda e: e.tensor_tensor(qs[:], q2[:], qkw[:].rearrange("p a b -> p (a b)"), op=ALU.mult), ["q2", "qkw"], ["qs"])
                          chk(1.4)
                          LD(rc[:, b, :], I["ropec"][:, tt, :], w=["rc"]); LD(rs[:, b, :], I["ropes"][:, tt, :], w=["rs"])
                          cb = rc[:, b, :].unsqueeze(1).to_broadcast([128, 10, 64]); sbb = rs[:, b, :].unsqueeze(1).to_broadcast([128, 10, 64])
                          x1v = q3[:, :, 0:64]; x2v = q3[:, :, 64:128]
                          V_(lambda e: e.tensor_tensor(r1[:], x1v, cb, op=ALU.mult), ["qs", "rc"], ["r1"])
                          G_(lambda e: e.tensor_tensor(r2[:], x2v, sbb, op=ALU.mult), ["qs", "rs"], ["r2"])
                          V_(lambda e: e.tensor_tensor(qr[b][:, :, 0:64], r1[:], r2[:], op=ALU.subtract), ["r1", "r2"], [f"qr{b}"])
                          V_(lambda e: e.tensor_tensor(r1[:], x1v, sbb, op=ALU.mult), ["qs", "rs", f"qr{b}"], ["r1"])
                          G_(lambda e: e.tensor_tensor(r2[:], x2v, cb, op=ALU.mult), ["qs", "rc", f"qr{b}"], ["r2"])
                          V_(lambda e: e.tensor_tensor(qr[b][:, :, 64:128], r1[:], r2[:], op=ALU.add), ["r1", "r2"], [f"qr{b}"])
                          chk(1.5)
                          for h in range(10):
                              pbi = 1 if h < 8 else 0
                              off = (h % 8) * 128
                              M_(lambda e: e.transpose(psb[1][:, off:off + 128] if h < 8 else psb[0][:, off:off + 128], qr[b][:, h, :], idb[:]),
                                 [f"qr{b}", "idb"], [PB[pbi]])
                          V_(lambda e: e.tensor_scalar(qT[hb][:, 0:8, ti * 128:(ti + 1) * 128], psb[1][:].rearrange("p (h t) -> p h t", t=128), 1.0, None, op0=ALU.mult), [PB[1]], [f"qT{hb}"])
                          A_(lambda e: e.activation(out=qT[hb][:, 8:10, ti * 128:(ti + 1) * 128], in_=psb[0][:, 0:256].rearrange("p (h t) -> p h t", t=128), func=AF.Identity), [PB[0]], [f"qT{hb}"])
                          chk(1.6)
                      t0 = blk * 512
                      LD(QT_d[:, :, t0:t0 + ntok].rearrange("h p t -> p h t"), qT[hb][:, 0:8, 0:ntok], [f"qT{hb}"], ["QT_d"])
                      LD(KT_d[:, :, t0:t0 + ntok].rearrange("h p t -> p h t"), qT[hb][:, 8:10, 0:ntok], [f"qT{hb}"], ["KT_d"])
                      LD(HT_d[:, t0:t0 + ntok].rearrange("(kt p) t -> p kt t", p=128), hT[hb][:, :, 0:ntok], [f"hT{hb}"], ["HT_d"])
              P.barrier()
              if stop == 2:
                  break
              scope(f"L{li}_gates")
              with ExitStack() as es:
                  sb = lambda n, s, d: es.enter_context(nc.sbuf_tensor(uniq(n), list(s), d))
                  wg = sb("wg", [128, 8, 2048], BF16)
                  hT = [sb(f"hT{i}", [128, 8, 512], BF16) for i in range(2)]
                  gsb = [sb(f"gsb{i}", [128, 16, 512], BF16) for i in range(2)]
                  wsrc = I["w_in"][li].rearrange("(kt k) n -> k kt n", k=128)
                  for cgi in range(2):
                      LDC(wg[:, :, cgi * 1024:(cgi + 1) * 1024], wsrc[:, :, 3072 + cgi * 1024:3072 + (cgi + 1) * 1024], w=["wg"])
                  ntg = T if not last else S
                  for blk in range((ntg + 511) // 512):
                      t0 = blk * 512
                      ntok = min(512, ntg - t0)
                      hb = blk % 2
                      LD(hT[hb][:, :, 0:ntok], HT_d[:, t0:t0 + ntok].rearrange("(kt p) t -> p kt t", p=128), w=[f"hT{hb}"])
                      for ncn in range(16):
                          pb = PF[3 + ncn % 2]; pt = psf[3 + ncn % 2]
                          for kt in range(8):
                              M_(lambda e: e.matmul(pt[:, 0:ntok], wg[:, kt, ncn * 128:(ncn + 1) * 128], hT[hb][:, kt, 0:ntok],
                                                    start=(kt == 0), stop=(kt == 7)), [f"hT{hb}", "wg"], [pb])
                          gi = blk % 2
                          A_(lambda e: e.activation(out=gsb[gi][:, ncn, 0:ntok], in_=pt[:, 0:ntok], func=AF.Sigmoid), [pb], [f"gsb{gi}"])
                      LD(G_d[:, t0:t0 + ntok].rearrange("(h p) t -> p h t", p=128), gsb[blk % 2][:, :, 0:ntok], [f"gsb{blk % 2}"], ["G_d"])
              P.barrier()

              if stop == 3:
                  break
              scope(f"L{li}_attn")
              if do_attn:
                  with ExitStack() as es:
                      sb = lambda n, s, d: es.enter_context(nc.sbuf_tensor(uniq(n), list(s), d))
                      kT = sb("kT", [128, 2, T], BF16); vv = sb("vv", [128, NT, 256], BF16)
                      qb = [sb(f"qb{i}", [128, 512], BF16) for i in range(2)]
                      pT = [sb(f"pT{i}", [128, 512], BF16) for i in range(4)]
                      stb = [psf[0][:], psf[1][:], psb[0][:].bitcast(F32), psb[1][:].bitcast(F32)]
                      stk = [PF[0], PF[1], PB[0], PB[1]]
                      rcp = sb("rcp", [128, 512], F32)
                      yo = [sb(f"yo{i}", [128, 512], BF16) for i in range(2)]
                      for h in range(2):
                          LD(kT[:, h, :], KT_d[h], w=["kT"])
                      LD(vv[:], V_d.rearrange("(t p) c -> p t c", p=128), w=["vv"])
                      scale = 128 ** -0.5
                      it = 0
                      blocks = [(h, q0, 512, 0, NT) for h in range(8) for q0 in range(0, S, 512)]
                      if not last:
                          blocks += [(h, S, 256, 32, NT) for h in range(8)]
                      for bi, (h, q0, nq, k0, k1) in enumerate(blocks):
                          kv = h // 4
                          b = bi % 2
                          LD(qb[b][:, 0:nq], QT_d[h, :, q0:q0 + nq], w=[f"qb{b}"])
                          po = psf[4 + b]; pok = PF[4 + b]
                          psm = psf[2 + b]; psmk = PF[2 + b]
                          def qk_(kt, g):
                              sbk = g % 4
                              M_(lambda e: e.matmul(stb[sbk][:, 0:nq], kT[:, kv, kt * 128:(kt + 1) * 128], qb[b][:, 0:nq], start=True, stop=True),
                                 ["kT", f"qb{b}"], [stk[sbk]])

                          def ex_(kt, g):
                              sbk = g % 4; pb3 = g % 4
                              A_(lambda e: e.activation(out=pT[pb3][:, 0:nq], in_=stb[sbk][:, 0:nq], func=AF.Exp, scale=scale), [stk[sbk]], [f"pT{pb3}"])

                          def pv_(kt, g):
                              pb3 = g % 4
                              M_(lambda e: e.matmul(po[:, 0:nq], vv[:, kt, kv * 128:(kv + 1) * 128], pT[pb3][:, 0:nq], start=(kt == k0), stop=(kt == k1 - 1)),
                                 ["vv", f"pT{pb3}"], [pok])
                              M_(lambda e: e.matmul(psm[:, 0:nq], onesb[:], pT[pb3][:, 0:nq], start=(kt == k0), stop=(kt == k1 - 1)),
                                 ["onesb", f"pT{pb3}"], [psmk])

                          g0 = it
                          it += (k1 - k0)
                          qk_(k0, g0)
                          if k0 + 1 < k1:
                              qk_(k0 + 1, g0 + 1)
                          for kt in range(k0, k1):
                              g = g0 + (kt - k0)
                              if kt + 2 < k1:
                                  qk_(kt + 2, g + 2)
                              ex_(kt, g)
                              pv_(kt, g)
                          V_(lambda e: e.reciprocal(rcp[:, 0:nq], psm[:, 0:nq]), [psmk], ["rcp"])
                          V_(lambda e: e.tensor_tensor(yo[b][:, 0:nq], po[:, 0:nq], rcp[:, 0:nq], op=ALU.mult), [pok, "rcp"], [f"yo{b}"])
                          LD(YA_d[h * 128:(h + 1) * 128, q0:q0 + nq], yo[b][:, 0:nq], [f"yo{b}"], ["YA_d"])
                  P.barrier()

              if stop == 4:
                  break
              if do_hyena:
                  scope(f"L{li}_shortconv")
                  with ExitStack() as es:
                      sb = lambda n, s, d: es.enter_context(nc.sbuf_tensor(uniq(n), list(s), d))
                      cw_ = sb("cw_", [128, 3, 1536], F32); cb_ = sb("cb_", [128, 1536], F32)
                      um = [sb(f"um{i}", [128, 1536], F32) for i in range(2)]
                      u0 = [sb(f"u0{i}", [128, 1536], F32) for i in range(2)]
                      up = [sb(f"up{i}", [128, 1536], F32) for i in range(2)]
                      oc = [sb(f"oc{i}", [128, 1536], F32) for i in range(2)]
                      t3 = sb("t3", [128, 1536], F32); t4 = sb("t4", [128, 1536], F32)
                      LD(cw_[:].rearrange("p a b -> p (a b)"), I["convw"][li], w=["cw_"]); LD(cb_[:], I["convb"][li], w=["cb_"])
                      tiles = list(range(NT if not last else 32))
                      for tt in tiles:
                          b = tt % 2
                          first = tt in (0, 32); lastt = tt in (31, 33)
                          r0 = tt * 128
                          if first:
                              V_(lambda e: e.memset(um[b][:], 0.0), w=[f"um{b}"])
                              LD(um[b][1:128, :], U_d[r0:r0 + 127, :], w=[f"um{b}"])
                          else:
                              LD(um[b][:], U_d[r0 - 1:r0 + 127, :], w=[f"um{b}"])
                          LD(u0[b][:], U_d[r0:r0 + 128, :], w=[f"u0{b}"])
                          if lastt:
                              V_(lambda e: e.memset(up[b][:], 0.0), w=[f"up{b}"])
                              LD(up[b][0:127, :], U_d[r0 + 1:r0 + 128, :], w=[f"up{b}"])
                          else:
                              LD(up[b][:], U_d[r0 + 1:r0 + 129, :], w=[f"up{b}"])
                          V_(lambda e: e.tensor_tensor(oc[b][:], u0[b][:], cw_[:, 1, :], op=ALU.mult), [f"u0{b}", "cw_"], [f"oc{b}"])
                          GP(lambda e: e.tensor_tensor(t3[:], um[b][:], cw_[:, 0, :], op=ALU.mult), [f"um{b}", "cw_"], ["t3"])
                          GP(lambda e: e.tensor_tensor(t4[:], up[b][:], cw_[:, 2, :], op=ALU.mult), [f"up{b}", "cw_"], ["t4"])
                          GP(lambda e: e.tensor_tensor(t3[:], t3[:], t4[:], op=ALU.add), ["t3", "t4"], ["t3"])
                          V_(lambda e: e.tensor_tensor(oc[b][:], oc[b][:], cb_[:], op=ALU.add), [f"oc{b}", "cb_"], [f"oc{b}"])
                          V_(lambda e: e.tensor_tensor(oc[b][:], oc[b][:], t3[:], op=ALU.add), [f"oc{b}", "t3"], [f"oc{b}"])
                          LD(XV_d[r0:r0 + 128, :], oc[b][:], [f"oc{b}"], ["XV_d"])
                  P.barrier()

                  chk(4.1)
                  seqs = [("lat", 0, 64)] + ([] if last else [("ctx", S, 4)])
                  with ExitStack() as es:
                      sb = lambda n, s, d: es.enter_context(nc.sbuf_tensor(uniq(n), list(s), d))
                      w3 = sb("w3", [64, 2048], BF16)
                      h2 = sb("h2", [64, 2, 4096], BF16)
                      nt_ = sb("nt_", [64, 2, 64], F32); vm_ = sb("vm_", [64, 2, 64], F32); dl = sb("dl", [64, 512], F32)
                      LDC(w3[:], I["pe_w3"][li], w=["w3"]); LD(dl[:], I["delta"][:, :], w=["dl"])
                      for si, (sname, tok0, pmax) in enumerate(seqs):
                          LD(nt_[:], I[f"nt_{sname}"].rearrange("d p j -> p d j"), w=["nt_"]); LD(vm_[:], I[f"vm_{sname}"].rearrange("d p j -> p d j"), w=["vm_"])
                          scope(f"L{li}_f{si}_mlp")
                          with ExitStack() as es2:
                              sb2 = lambda n, s, d: es2.enter_context(nc.sbuf_tensor(uniq(n), list(s), d))
                              w1 = sb2("w1", [33, 64], F32); w2 = sb2("w2", [64, 64], F32)
                              pv = sb2("pv", [64, 4], F32); pvb = sb2("pvb", [64, 2], F32)
                              zT = sb2("zT", [33, 4096], F32)
                              h1 = sb2("h1", [64, 512], F32); harg = sb2("harg", [64, 512], F32); hw1 = sb2("hw1", [64, 512], F32); hw2 = sb2("hw2", [64, 512], F32)
                              LD(w1[:], I["pe_w1"][li], w=["w1"]); LD(w2[:], I["pe_w2"][li], w=["w2"]); LD(pv[:], I["pe_v"][li], w=["pv"])
                              V_(lambda e: e.tensor_tensor(pvb[:, 0:1], pv[:, 0:1], pv[:, 1:2], op=ALU.mult), ["pv"], ["pvb"])
                              V_(lambda e: e.tensor_tensor(pvb[:, 1:2], pv[:, 2:3], pv[:, 3:4], op=ALU.mult), ["pv"], ["pvb"])

                              def sin_layer(ps, fcol, bcol, out_ap, okey):
                                  A_(lambda e: e.activation(out=harg[:], in_=ps, func=AF.Identity, scale=pv[:, fcol:fcol + 1], bias=pvb[:, bcol:bcol + 1]), [PF[0], "pv", "pvb"], ["harg"])
                                  for _ in range(2):
                                      V_(lambda e: e.tensor_scalar(hw1[:], harg[:], PI, -2 * PI, op0=ALU.is_gt, op1=ALU.mult), ["harg"], ["hw1"])
                                      V_(lambda e: e.tensor_scalar(hw2[:], harg[:], -PI, 2 * PI, op0=ALU.is_lt, op1=ALU.mult), ["harg"], ["hw2"])
                                      V_(lambda e: e.tensor_tensor(hw1[:], hw1[:], hw2[:], op=ALU.add), ["hw1", "hw2"], ["hw1"])
                                      V_(lambda e: e.tensor_tensor(harg[:], harg[:], hw1[:], op=ALU.add), ["harg", "hw1"], ["harg"])
                                  A_(lambda e: e.activation(out=out_ap, in_=harg[:], func=AF.Sin), ["harg"], [okey])

                              for dr in range(2):
                                  LD(zT[:], I[f"z_{sname}"][dr], w=["zT"])
                                  for ck in range(8):
                                      M_(lambda e: e.matmul(psf[0][0:64, :], w1[:], zT[:, ck * 512:(ck + 1) * 512], start=True, stop=True), ["w1", "zT"], [PF[0]])
                                      sin_layer(psf[0][0:64, :], 0, 0, h1[:], "h1")
                                      M_(lambda e: e.matmul(psf[0][0:64, :], w2[:], h1[:], start=True, stop=True), ["w2", "h1"], [PF[0]])
                                      sin_layer(psf[0][0:64, :], 2, 1, h2[:, dr, ck * 512:(ck + 1) * 512], "h2")
                          P.barrier()
                          for o in range(2):
                              scope(f"L{li}_f{si}{o}_s1")
                              with ExitStack() as es2:
                                  sb2 = lambda n, s, d: es2.enter_context(nc.sbuf_tensor(uniq(n), list(s), d))
                                  mlo = sb2("mlo", [64, 64, 2, 128], BF16); mhi = sb2("mhi", [64, 64, 2, 128], BF16)
                                  kfb = [sb2(f"kfb{i}", [64, 2, 512], BF16) for i in range(2)]
                                  asb = [sb2(f"asb{i}", [128, 4, 2, 512], BF16) for i in range(2)]
                                  LDC(mlo[:], I["mlo"][:, :, :, :], w=["mlo"]); LDC(mhi[:], I["mhi"][:, :, :, :], w=["mhi"])
                                  wps = [[psf[0], psf[1]], [psf[2], psf[3]]]; wpk = [[PF[0], PF[1]], [PF[2], PF[3]]]
                                  sps = [[psf[4][:], psf[5][:]], [psb[0][:].bitcast(F32), psb[1][:].bitcast(F32)]]; spk = [[PF[4], PF[5]], [PB[0], PB[1]]]
                                  win = [[sb2(f"win{i}{d_}", [64, 512], F32) for d_ in range(2)] for i in range(2)]

                                  def fs1_a(j):
                                      b = j % 2
                                      for dr in range(2):
                                          A_(lambda e: e.activation(out=win[b][dr][:], in_=dl[:], func=AF.Exp, scale=nt_[:, dr, j:j + 1]), ["dl", "nt_"], [f"win{b}{dr}"])
                                          M_(lambda e: e.matmul(wps[b][dr][0:64, :], h2[:, dr, :].rearrange("k (p j) -> k j p", j=64)[:, j, :],
                                                                w3[:, o * 1024 + dr * 512:o * 1024 + (dr + 1) * 512], start=True, stop=True), ["h2", "w3"], [wpk[b][dr]])
                                          V_(lambda e: e.scalar_tensor_tensor(out=kfb[b][:, dr, :], in0=win[b][dr][:], scalar=vm_[:, dr, j:j + 1], in1=wps[b][dr][0:64, :], op0=ALU.mult, op1=ALU.mult),
                                             [f"win{b}{dr}", "vm_", wpk[b][dr]], [f"kfb{b}{dr}"])

                                  def fs1_b(j):
                                      b = j % 2
                                      for ri in range(2):
                                          M_(lambda e: e.matmul(sps[b][ri], mlo[:, j, ri, :], kfb[b][:, 0, :], start=True, stop=False), ["mlo", f"kfb{b}0"], [spk[b][ri]])
                                          M_(lambda e: e.matmul(sps[b][ri], mhi[:, j, ri, :], kfb[b][:, 1, :], start=False, stop=True), ["mhi", f"kfb{b}1"], [spk[b][ri]])
                                      ab_ = (j // 4) % 2; jl = j % 4
                                      A_(lambda e: e.activation(out=asb[ab_][:, jl, 0, :], in_=sps[b][0], func=AF.Identity), [spk[b][0]], [f"asbr{ab_}"])
                                      V_(lambda e: e.tensor_scalar(asb[ab_][:, jl, 1, :], sps[b][1], 1.0, None, op0=ALU.mult), [spk[b][1]], [f"asbi{ab_}"])
                                      if jl == 3:
                                          LD(A_d[:, j - 3:j + 1, :, :].rearrange("f j r c -> f (j r c)"), asb[ab_][:].rearrange("f j r c -> f (j r c)"), [f"asbr{ab_}", f"asbi{ab_}"], ["A_d"])

                                  fs1_a(0)
                                  for j in range(64):
                                      if j + 1 < 64:
                                          fs1_a(j + 1)
                                      fs1_b(j)
                              P.barrier()
                              scope(f"L{li}_f{si}{o}_s3")
                              with ExitStack() as es2:
                                  sb2 = lambda n, s, d: es2.enter_context(nc.sbuf_tensor(uniq(n), list(s), d))
                                  w64 = sb2("w64", [64, 8, 128], BF16)
                                  bsb = [sb2(f"bsb{i}", [64, 4, 2, 512], BF16) for i in range(2)]
                                  ksb = [sb2(f"ksb{i}", [128, 4, 2, 512], BF16) for i in range(2)]
                                  LDC(w64[:], I["w64"][:, :, :], w=["w64"])
                                  for fc in range(32):
                                      b = fc % 2
                                      LD(bsb[b][:], A_d[fc * 4:(fc + 1) * 4].rearrange("f j r c -> j f r c"), w=[f"bsb{b}"])
                                      for fl in range(4):
                                          f1i = fc * 4 + fl
                                          kb2 = f1i % 2
                                          for ab in range(2):
                                              pt = psf[ab + 2 * kb2]; pk = PF[ab + 2 * kb2]
                                              M_(lambda e: e.matmul(pt[:], w64[:, 4 + 2 * ab, :], bsb[b][:, fl, 0, :], start=True, stop=False), ["w64", f"bsb{b}"], [pk])
                                              M_(lambda e: e.matmul(pt[:], w64[:, 5 + 2 * ab, :], bsb[b][:, fl, 1, :], start=False, stop=True), ["w64", f"bsb{b}"], [pk])
                                          A_(lambda e: e.activation(out=ksb[b][:, fl, 0, :], in_=psf[2 * kb2][:], func=AF.Identity), [PF[2 * kb2]], [f"ksba{b}"])
                                          V_(lambda e: e.tensor_scalar(ksb[b][:, fl, 1, :], psf[1 + 2 * kb2][:], 1.0, None, op0=ALU.mult), [PF[1 + 2 * kb2]], [f"ksbb{b}"])
                                          if fl == 3:
                                              LD(KH_d[si, o, fc * 4:(fc + 1) * 4].rearrange("f a r c -> r (f a) c"), ksb[b][:].rearrange("r f a c -> r (f a) c"), [f"ksba{b}", f"ksbb{b}"], ["KH_d"])
                              P.barrier()
                  P.barrier()

                  chk(4.2)
                  with ExitStack() as es:
                      sb = lambda n, s, d: es.enter_context(nc.sbuf_tensor(uniq(n), list(s), d))
                      mlo = sb("mlo", [64, 64, 2, 128], BF16); minvT = sb("minvT", [128, 64, 2, 64], BF16)
                      w64 = sb("w64", [64, 4, 128], BF16); wi1 = sb("wi1", [128, 128], BF16)
                      skb = sb("skb", [64, 1024], F32)
                      vz = sb("vz", [64, 64, CW], BF16); zz = sb("zz", [64, 64, CW], BF16)
                      asb = [sb(f"asb{i}", [128, 4, 2, CW], BF16) for i in range(2)]
                      bsb = [sb(f"bsb{i}", [64, 8, 2, CW], BF16) for i in range(2)]
                      ksb = [sb(f"ksb{i}", [128, 4, 2, CW], BF16) for i in range(3)]
                      y1 = [sb(f"y1{i}", [128, CW], BF16) for i in range(2)]
                      y2 = [sb(f"y2{i}", [128, CW], BF16) for i in range(2)]
                      yb = [sb(f"yb{i}", [128, CW], BF16) for i in range(2)]
                      csb = [sb(f"csb{i}", [128, 8, CW], BF16) for i in range(2)]
                      cj = [sb(f"cj{i}", [128, 2, CW], BF16) for i in range(2)]
                      xg = [sb(f"xg{i}", [64, CW], F32) for i in range(2)]
                      tg = [sb(f"tg{i}", [64, CW], F32) for i in range(2)]
                      yh = [sb(f"yh{i}", [64, 4, CW], BF16) for i in range(2)]
                      LDC(mlo[:], I["mlo"][:, :, :, :], w=["mlo"]); LDC(minvT[:], I["minvT"][:, :, :, :], w=["minvT"])
                      LDC(w64[:], I["w64"][:, 0:4, :], w=["w64"]); LDC(wi1[:], I["wi1"][:, :], w=["wi1"])
                      LD(skb[:], I["skipb"][li], w=["skb"])
                      skp = sb("skp", [64, CW], F32)
                      passes = [[(0, 0, 64, k * 256, 256, 0)] for k in range(2)]
                      if not last:
                          passes += [[(1, S, 4, k * 256, 256, 0)] for k in range(2)]
                      for segs in passes:
                          if True:
                              for b in range(2):
                                  V_(lambda e: e.memset(xg[b][:], 0.0), w=[f"xg{b}"])
                              if segs[0][2] < 64:
                                  V_(lambda e: e.memset(vz[:], 0.0), w=["vz"])
                              for (si, tok0, pmax, c0, w_, off) in segs:
                                  LDC(vz[0:pmax, :, off:off + w_], XV_d[tok0:tok0 + pmax * 64, 1024 + c0:1024 + c0 + w_].rearrange("(p j) c -> p j c", j=64), w=["vz"])
                              for o in range(2):
                                  scope(f"L{li}_c{segs[0][0]}{segs[0][3]}{o}_s1")
                                  src = vz if o == 0 else zz
                                  skey = "vz" if o == 0 else "zz"
                                  for j in range(64):
                                      b = j % 2
                                      for ri in range(2):
                                          M_(lambda e: e.matmul(psf[ri + 2 * b][:, 0:CW], mlo[:, j, ri, :], src[:, j, :], start=True, stop=True), ["mlo", skey], [PF[ri + 2 * b]])
                                      ab_ = (j // 4) % 2; jl = j % 4
                                      A_(lambda e: e.activation(out=asb[ab_][:, jl, 0, :], in_=psf[2 * b][:, 0:CW], func=AF.Identity), [PF[2 * b]], [f"asbr{ab_}"])
                                      V_(lambda e: e.tensor_scalar(asb[ab_][:, jl, 1, :], psf[1 + 2 * b][:, 0:CW], 1.0, None, op0=ALU.mult), [PF[1 + 2 * b]], [f"asbi{ab_}"])
                                      if jl == 3:
                                          LD(A2_d[:, j - 3:j + 1, :, :].rearrange("f j r c -> f (j r c)"), asb[ab_][:].rearrange("f j r c -> f (j r c)"), [f"asbr{ab_}", f"asbi{ab_}"], ["A_d"])
                                  def s3_mm(f1i):
                                      fc, fl = divmod(f1i, 8)
                                      b = fc % 2; par = f1i % 2; k3 = f1i % 4
                                      if fl == 0:
                                          LD(bsb[b][:].rearrange("j f r c -> j f (r c)"), A2_d[fc * 8:(fc + 1) * 8].rearrange("f j r c -> j f (r c)"), w=[f"bsb{b}"])
                                      k3 = (f1i // 4) % 3
                                      if f1i % 4 == 0:
                                          for (si, tok0, pmax, c0, w_, off) in segs:
                                              LD(ksb[k3][:, :, :, off:off + w_].rearrange("r f a c -> r (f a) c"), KH_d[si, o, f1i:f1i + 4, :, :, c0:c0 + w_].rearrange("f a r c -> r (f a) c"), w=[f"ksb{k3}"])
                                      for pq in range(2):
                                          pi = pq + 2 * par
                                          M_(lambda e: e.matmul(psf[pi][:, 0:CW], w64[:, 2 * pq, :], bsb[b][:, fl, 0, :], start=True, stop=False), ["w64", f"bsb{b}"], [PF[pi]])
                                          M_(lambda e: e.matmul(psf[pi][:, 0:CW], w64[:, 2 * pq + 1, :], bsb[b][:, fl, 1, :], start=False, stop=True), ["w64", f"bsb{b}"], [PF[pi]])

                                  def s3_post(f1i):
                                      fc, fl = divmod(f1i, 8)
                                      b = fc % 2; par = f1i % 2; k3 = (f1i // 4) % 3; b2 = par; kf = f1i % 4
                                      V_(lambda e: e.tensor_tensor(y1[b2][:], psf[2 * par][:, 0:CW], ksb[k3][:, kf, 0, :], op=ALU.mult), [PF[2 * par], f"ksb{k3}"], [f"y1{b2}"])
                                      V_(lambda e: e.tensor_tensor(y2[b2][:], psf[1 + 2 * par][:, 0:CW], ksb[k3][:, kf, 1, :], op=ALU.mult), [PF[1 + 2 * par], f"ksb{k3}"], [f"y2{b2}"])
                                      M_(lambda e: e.matmul(psf[4 + par][:, 0:CW], wi1[:], y1[b2][:], start=True, stop=False), ["wi1", f"y1{b2}"], [PF[4 + par]])
                                      M_(lambda e: e.matmul(psf[4 + par][:, 0:CW], wi1[:], y2[b2][:], start=False, stop=True), ["wi1", f"y2{b2}"], [PF[4 + par]])
                                      A_(lambda e: e.activation(out=csb[b][:, fl, :], in_=psf[4 + par][:, 0:CW], func=AF.Identity), [PF[4 + par]], [f"csb{b}"])
                                      if fl == 7:
                                          LD(C_d[:, fc * 8:(fc + 1) * 8, :], csb[b][:], [f"csb{b}"], ["C_d"])

                                  P.barrier()
                                  scope(f"L{li}_c{segs[0][0]}{segs[0][3]}{o}_s3")
                                  s3_mm(0)
                                  for f1i in range(128):
                                      if f1i + 1 < 128:
                                          s3_mm(f1i + 1)
                                      s3_post(f1i)
                                  P.barrier()
                                  scope(f"L{li}_c{segs[0][0]}{segs[0][3]}{o}_i2")
                                  Cv = C_d.rearrange("(r j) f c -> j f r c", j=64)
                                  for (si, tok0, pmax, c0, w_, off) in segs:
                                      V_(lambda e: e.tensor_scalar(skp[:, off:off + w_], skb[:, o * 512 + c0:o * 512 + c0 + w_], 1.0, None, op0=ALU.mult), ["skb"], ["skp"])
                                  for j in range(64):
                                      b = j % 2
                                      LD(cj[b][:], Cv[j], w=[f"cj{b}"])
                                      M_(lambda e: e.matmul(psf[4 + b][0:64, 0:CW], minvT[:, j, 0, :], cj[b][:, 0, :], start=True, stop=False), ["minvT", f"cj{b}"], [PF[4 + b]])
                                      M_(lambda e: e.matmul(psf[4 + b][0:64, 0:CW], minvT[:, j, 1, :], cj[b][:, 1, :], start=False, stop=True), ["minvT", f"cj{b}"], [PF[4 + b]])
                                      for (si, tok0, pmax, c0, w_, off) in segs:
                                          xcol = (0 if o == 0 else 512) + c0
                                          LD(xg[b][0:pmax, off:off + w_], XV_d[tok0:tok0 + pmax * 64, xcol:xcol + w_].rearrange("(p j) c -> p j c", j=64)[:, j, :], w=[f"xg{b}"])
                                      G_(lambda e: e.tensor_tensor(tg[b][:], src[:, j, :], skp[:], op=ALU.mult), [skey, "skp"], [f"tg{b}"])
                                      V_(lambda e: e.tensor_tensor(tg[b][:], psf[4 + b][0:64, 0:CW], tg[b][:], op=ALU.add), [PF[4 + b], f"tg{b}"], [f"tg{b}"])
                                      if o == 0:
                                          V_(lambda e: e.tensor_tensor(zz[:, j, :], tg[b][:], xg[b][:], op=ALU.mult), [f"tg{b}", f"xg{b}"], ["zz"])
                                      else:
                                          yb_ = (j // 4) % 2; jl = j % 4
                                          V_(lambda e: e.tensor_tensor(yh[yb_][:, jl, :], tg[b][:], xg[b][:], op=ALU.mult), [f"tg{b}", f"xg{b}"], [f"yh{yb_}"])
                                          if jl == 3:
                                              for (si, tok0, pmax, c0, w_, off) in segs:
                                                  LD(YH_d[tok0:tok0 + pmax * 64, c0:c0 + w_].rearrange("(p j) c -> p j c", j=64)[:, j - 3:j + 1, :], yh[yb_][0:pmax, :, off:off + w_], [f"yh{yb_}"], ["YH_d"])
                                  P.barrier()
                  P.barrier()

              scope(None)
              if stop == 5:
                  break
              scope(f"L{li}_merge")
              with ExitStack() as es:
                  sb = lambda n, s, d: es.enter_context(nc.sbuf_tensor(uniq(n), list(s), d))
                  wap = sb("wap", [128, 8, D], BF16); whp = sb("whp", [128, 4, D], BF16); wo = sb("wo", [128, 8, D], BF16)
                  rw = sb("rw", [128, 8, NE], F32); rbb = sb("rbb", [128, NE], F32)
                  ya = [sb(f"ya{i}", [128, 8, 512], BF16) for i in range(2)]
                  yht = [sb(f"yht{i}", [128, 512], BF16) for i in range(2)]
                  yhT = sb("yhT", [128, 4, 512], BF16)
                  gt = [sb("gt0", [128, 16, 512], BF16)] * 2
                  mT = sb("mT", [128, 8, 512], BF16)
                  m1 = [sb(f"m1{i}", [128, 512], F32) for i in range(2)]
                  m2 = [sb(f"m2{i}", [128, 512], F32) for i in range(2)]
                  xt = [sb(f"xt{i}", [128, D], F32) for i in range(2)]
                  xo = [sb(f"xo{i}", [128, D], F32) for i in range(2)]
                  junk = sb("junk", [128, D], BF16)
                  st = [sb(f"st{i}", [128, 4], F32) for i in range(2)]
                  xn = [sb(f"xn{i}", [128, D], F32) for i in range(2)]
                  h32 = sb("h32", [128, 8, 128], F32)
                  h2b = sb("h2b", [128, 8, 512], BF16)
                  lg = sb("lg", [128, NE], F32); sc_ = sb("sc_", [128, NE], F32); bi_ = sb("bi_", [128, NE], F32)
                  t8 = sb("t8", [128, 8, 8], F32); gs = sb("gs", [128, 8], F32); gm = sb("gm", [128, 8], F32)
                  sm = sb("sm", [128, 4], F32); em = sb("em", [128, NE], F32); gate = [sb(f"gate{i}", [128, NE], F32) for i in range(2)]
                  LDC(wap[:], I["w_ap"][li].rearrange("(kt k) n -> k kt n", k=128), w=["wap"])
                  LDC(whp[:], I["w_hp"][li].rearrange("(kt k) n -> k kt n", k=128), w=["whp"])
                  LDC(wo[:], I["w_out"][li].rearrange("(kt k) n -> k kt n", k=128), w=["wo"])
                  LD(rw[:], I["router_w"].rearrange("(kt k) n -> k kt n", k=128), w=["rw"]); LD(rbb[:], I["router_bb"][:, :], w=["rbb"])
                  ntiles = NT if not last else 32
                  nblk = (ntiles * 128 + 511) // 512
                  for blk in range(nblk):
                      tts = list(range(blk * 4, min(blk * 4 + 4, ntiles)))
                      ntok = len(tts) * 128
                      t0 = blk * 512
                      b = blk % 2
                      LD(ya[b][:, :, 0:ntok], YA_d[:, t0:t0 + ntok].rearrange("(h p) t -> p h t", p=128), w=[f"ya{b}"])
                      LD(gt[b][:, :, 0:ntok], G_d[:, t0:t0 + ntok].rearrange("(h p) t -> p h t", p=128), w=["gt"])
                      for ti, tt in enumerate(tts):
                          yb_ = tt % 2
                          LD(yht[yb_][:], YH_d[tt * 128:(tt + 1) * 128, :], w=[f"yht{yb_}"])
                          for ct in range(4):
                              M_(lambda e: e.transpose(psb[0][:, ct * 128:(ct + 1) * 128], yht[yb_][:, ct * 128:(ct + 1) * 128], idb[:]), [f"yht{yb_}", "idb"], [PB[0]])
                          A_(lambda e: e.activation(out=yhT[:, :, ti * 128:(ti + 1) * 128], in_=psb[0][:, 0:512].rearrange("p (c t) -> p c t", t=128), func=AF.Identity), [PB[0]], ["yhT"])
                      for ncn in range(8):
                          pa = psf[ncn % 2]; pak = PF[ncn % 2]; ph = psf[2 + ncn % 2]; phk = PF[2 + ncn % 2]
                          mb = ncn % 2
                          for kt in range(8):
                              M_(lambda e: e.matmul(pa[:, 0:ntok], wap[:, kt, ncn * 128:(ncn + 1) * 128], ya[b][:, kt, 0:ntok], start=(kt == 0), stop=(kt == 7)), ["wap", f"ya{b}"], [pak])
                          for ct in range(4):
                              M_(lambda e: e.matmul(ph[:, 0:ntok], whp[:, ct, ncn * 128:(ncn + 1) * 128], yhT[:, ct, 0:ntok], start=(ct == 0), stop=(ct == 3)), ["whp", "yhT"], [phk])
                          V_(lambda e: e.tensor_tensor(m1[mb][:, 0:ntok], pa[:, 0:ntok], gt[b][:, ncn, 0:ntok], op=ALU.mult), [pak, "gt"], [f"m1{mb}"])
                          V_(lambda e: e.tensor_tensor(m2[mb][:, 0:ntok], ph[:, 0:ntok], gt[b][:, 8 + ncn, 0:ntok], op=ALU.mult), [phk, "gt"], [f"m2{mb}"])
                          G_(lambda e: e.tensor_tensor(mT[:, ncn, 0:ntok], m1[mb][:, 0:ntok], m2[mb][:, 0:ntok], op=ALU.add), [f"m1{mb}", f"m2{mb}"], ["mT"])
                      for ti, tt in enumerate(tts):
                          m = 0 if tt < 32 else 1
                          xb_ = tt % 2
                          LD(xt[xb_][:], cur[tt * 128:(tt + 1) * 128, :], w=[f"xt{xb_}"])
                          for hf in range(2):
                              po = psf[4 + hf]; pok = PF[4 + hf]
                              for kt in range(8):
                                  M_(lambda e: e.matmul(po[:], mT[:, kt, ti * 128:(ti + 1) * 128], wo[:, kt, hf * 512:(hf + 1) * 512], start=(kt == 0), stop=(kt == 7)), ["mT", "wo"], [pok])
                              V_(lambda e: e.tensor_tensor(xo[xb_][:, hf * 512:(hf + 1) * 512], po[:], gb[:, m, hf * 512:(hf + 1) * 512], op=ALU.mult), [pok, "gb"], [f"xo{xb_}"])
                          G_(lambda e: e.tensor_tensor(xo[xb_][:], xo[xb_][:], xt[xb_][:], op=ALU.add), [f"xo{xb_}", f"xt{xb_}"], [f"xo{xb_}"])
                          LD(mid[tt * 128:(tt + 1) * 128, :], xo[xb_][:], [f"xo{xb_}"], ["mid"])
                          A_(lambda e: e.activation(out=junk[:], in_=xo[xb_][:], func=AF.Square, accum_out=st[xb_][:, 0:1]), [f"xo{xb_}"], ["junk", f"st{xb_}"])
                          A_(lambda e: e.activation(out=st[xb_][:, 1:2], in_=st[xb_][:, 0:1], func=AF.Sqrt, scale=1.0 / D, bias=epsc[:, 0:1]), [f"st{xb_}", "epsc"], [f"st{xb_}"])
                          V_(lambda e: e.reciprocal(st[xb_][:, 2:3], st[xb_][:, 1:2]), [f"st{xb_}"], [f"st{xb_}"])
                          V_(lambda e: e.tensor_scalar(xn[xb_][:], xo[xb_][:], st[xb_][:, 2:3], None, op0=ALU.mult), [f"xo{xb_}", f"st{xb_}"], [f"xn{xb_}"])
                          for dt in range(8):
                              M_(lambda e: e.transpose(psf[dt // 4][:, (dt % 4) * 128:(dt % 4 + 1) * 128], xn[xb_][:, dt * 128:(dt + 1) * 128], idf[:]), [f"xn{xb_}", "idf"], [PF[dt // 4]])
                          for dt in range(8):
                              A_(lambda e: e.activation(out=h32[:, dt, :], in_=psf[dt // 4][:, (dt % 4) * 128:(dt % 4 + 1) * 128], func=AF.Identity,
                                                        scale=scB[:, dt, m:m + 1], bias=mods[:, 24 + dt, m:m + 1]), [PF[dt // 4], "scB", "mods"], ["h32"])
                          G_(lambda e: e.tensor_scalar(h2b[:, :, ti * 128:(ti + 1) * 128], h32[:], 1.0, None, op0=ALU.mult), ["h32"], ["h2b"])
                          for dt in range(8):
                              M_(lambda e: e.matmul(psf[2][:, 0:NE], h32[:, dt, :], rw[:, dt, :], start=(dt == 0), stop=(dt == 7)), ["h32", "rw"], [PF[2]])
                          gb_ = tt % 2
                          A_(lambda e: e.activation(out=sc_[:], in_=psf[2][:, 0:NE], func=AF.Sigmoid), [PF[2]], ["sc_"])
                          V_(lambda e: e.tensor_tensor(bi_[:], sc_[:], rbb[:], op=ALU.add), ["sc_", "rbb"], ["bi_"])
                          for g in range(8):
                              V_(lambda e: e.max(out=t8[:, g, :], in_=bi_[:, g * 8:(g + 1) * 8]), ["bi_"], ["t8"])
                          V_(lambda e: e.tensor_tensor(gs[:], t8[:, :, 0], t8[:, :, 1], op=ALU.add), ["t8"], ["gs"])
                          V_(lambda e: e.tensor_reduce(sm[:, 0:1], gs[:], axis=AX.X, op=ALU.max), ["gs"], ["sm"])
                          V_(lambda e: e.tensor_scalar(gm[:], gs[:], sm[:, 0:1], None, op0=ALU.is_equal), ["gs", "sm"], ["gm"])
                          V_(lambda e: e.tensor_tensor(gs[:], gm[:], t8[:, :, 1], op=ALU.mult), ["gm", "t8"], ["gs"])
                          V_(lambda e: e.tensor_reduce(sm[:, 1:2], gs[:], axis=AX.X, op=ALU.add), ["gs"], ["sm"])
                          V_(lambda e: e.tensor_scalar(em[:], bi_[:], sm[:, 1:2], None, op0=ALU.is_ge), ["bi_", "sm"], ["em"])
                          V_(lambda e: e.tensor_tensor(em[:].rearrange("p (g k) -> p g k", k=8), em[:].rearrange("p (g k) -> p g k", k=8),
                                                       gm[:].unsqueeze(2).to_broadcast([128, 8, 8]), op=ALU.mult), ["em", "gm"], ["em"])
                          V_(lambda e: e.tensor_tensor(em[:], em[:], sc_[:], op=ALU.mult), ["em", "sc_"], ["em"])
                          V_(lambda e: e.tensor_reduce(sm[:, 2:3], em[:], axis=AX.X, op=ALU.add), ["em"], ["sm"])
                          V_(lambda e: e.reciprocal(sm[:, 3:4], sm[:, 2:3]), ["sm"], ["sm"])
                          V_(lambda e: e.tensor_scalar(gate[gb_][:], em[:], sm[:, 3:4], None, op0=ALU.mult), ["em", "sm"], [f"gate{gb_}"])
                          LD(GATE_d[tt * 128:(tt + 1) * 128, :], gate[gb_][:], [f"gate{gb_}"], ["GATE_d"])
                      LD(H2T_d[:, t0:t0 + ntok].rearrange("(kt p) t -> p kt t", p=128), h2b[:, :, 0:ntok], ["h2b"], ["H2T_d"])
              P.barrier()

              if stop == 6:
                  break
              scope(f"L{li}_moe")
              ntiles = NT if not last else 32
              with ExitStack() as es:
                  sb = lambda n, s, d: es.enter_context(nc.sbuf_tensor(uniq(n), list(s), d))
                  SBK = 1536
                  hT2 = sb("hT2", [128, 8, SBK], BF16)
                  acc = sb("acc", [128, SBK // 128, D], F32)
                  gat = sb("gat", [128, SBK // 128, NE], F32)
                  w1 = [sb(f"ew1{i}", [128, 8, 512], BF16) for i in range(2)]
                  w3 = [sb(f"ew3{i}", [128, 8, 512], BF16) for i in range(2)]
                  w2 = [sb(f"ew2{i}", [128, 4, D], BF16) for i in range(2)]
                  s1 = [sb(f"s1{i}", [128, 512], F32) for i in range(2)]
                  gT = [sb(f"gT{i}", [128, 4, 512], BF16) for i in range(2)]
                  xt = [sb(f"xt{i}", [128, D], F32) for i in range(2)]
                  xo = [sb(f"xo{i}", [128, D], F32) for i in range(2)]
                  junk = sb("junk", [128, D], BF16); st = [sb(f"st{i}", [128, 4], F32) for i in range(2)]
                  fnw = sb("fnw", [128, D], F32)
                  LD(fnw[:], I["fnw"][:, :], w=["fnw"])
                  ntok_all = ntiles * 128
                  for s0 in range(0, ntok_all, SBK):
                      sn = min(SBK, ntok_all - s0)
                      stl = sn // 128
                      LD(hT2[:, :, 0:sn], H2T_d[:, s0:s0 + sn].rearrange("(kt p) t -> p kt t", p=128), w=["hT2"])
                      LD(gat[:, 0:stl, :], GATE_d[s0:s0 + sn, :].rearrange("(t p) e -> p t e", p=128), w=["gat"])
                      V_(lambda e: e.memset(acc[:], 0.0), w=["acc"])
                      if do_moe:
                          blist = [(ex, b0, min(512, sn - b0)) for ex in range(NE) for b0 in range(0, sn, 512)]

                          def moe_a(bi):
                              ex, b0, bn = blist[bi]
                              wb = ex % 2; gbi = bi % 2
                              if b0 == 0:
                                  LDC(w1[wb][:], I["exp_w1"][li, ex].rearrange("(kt k) n -> k kt n", k=128), w=[f"ew1{wb}"])
                                  LDC(w3[wb][:], I["exp_w3"][li, ex].rearrange("(kt k) n -> k kt n", k=128), w=[f"ew3{wb}"])
                                  LDC(w2[wb][:], I["exp_w2"][li, ex].rearrange("(kt k) n -> k kt n", k=128), w=[f"ew2{wb}"])
                              for fcn in range(4):
                                  p1 = psf[fcn % 2]; p1k = PF[fcn % 2]; p3 = psf[2 + fcn % 2]; p3k = PF[2 + fcn % 2]
                                  sbi = fcn % 2
                                  for kt in range(8):
                                      M_(lambda e: e.matmul(p1[:, 0:bn], w1[wb][:, kt, fcn * 128:(fcn + 1) * 128], hT2[:, kt, b0:b0 + bn], start=(kt == 0), stop=(kt == 7)), [f"ew1{wb}", "hT2"], [p1k])
                                  for kt in range(8):
                                      M_(lambda e: e.matmul(p3[:, 0:bn], w3[wb][:, kt, fcn * 128:(fcn + 1) * 128], hT2[:, kt, b0:b0 + bn], start=(kt == 0), stop=(kt == 7)), [f"ew3{wb}", "hT2"], [p3k])
                                  A_(lambda e: e.activation(out=s1[sbi][:, 0:bn], in_=p1[:, 0:bn], func=AF.Silu), [p1k], [f"s1{sbi}"])
                                  V_(lambda e: e.tensor_tensor(gT[gbi][:, fcn, 0:bn], p3[:, 0:bn], s1[sbi][:, 0:bn], op=ALU.mult), [p3k, f"s1{sbi}"], [f"gT{gbi}"])

                          def moe_b(bi):
                              ex, b0, bn = blist[bi]
                              wb = ex % 2; gbi = bi % 2
                              for ti in range(bn // 128):
                                  tl = (b0 // 128) + ti
                                  for hf in range(2):
                                      po = psf[4 + hf]; pok = PF[4 + hf]
                                      for ft in range(4):
                                          M_(lambda e: e.matmul(po[:], gT[gbi][:, ft, ti * 128:(ti + 1) * 128], w2[wb][:, ft, hf * 512:(hf + 1) * 512], start=(ft == 0), stop=(ft == 3)), [f"gT{gbi}", f"ew2{wb}"], [pok])
                                      V_(lambda e: e.scalar_tensor_tensor(out=acc[:, tl, hf * 512:(hf + 1) * 512], in0=po[:], scalar=gat[:, tl, ex:ex + 1],
                                                                          in1=acc[:, tl, hf * 512:(hf + 1) * 512], op0=ALU.mult, op1=ALU.add), [pok, "gat", "acc"], ["acc"])

                          moe_a(0)
                          for bi in range(len(blist)):
                              if bi + 1 < len(blist):
                                  moe_a(bi + 1)
                              moe_b(bi)
                      for tl in range(stl):
                          tt = s0 // 128 + tl
                          m = 0 if tt < 32 else 1
                          xb_ = tt % 2
                          LD(xt[xb_][:], mid[tt * 128:(tt + 1) * 128, :], w=[f"xt{xb_}"])
                          V_(lambda e: e.tensor_tensor(xo[xb_][:], acc[:, tl, :], gb[:, 2 + m, :], op=ALU.mult), ["acc", "gb"], [f"xo{xb_}"])
                          G_(lambda e: e.tensor_tensor(xo[xb_][:], xo[xb_][:], xt[xb_][:], op=ALU.add), [f"xo{xb_}", f"xt{xb_}"], [f"xo{xb_}"])
                          if not last:
                              LD(cur[tt * 128:(tt + 1) * 128, :], xo[xb_][:], [f"xo{xb_}"], ["cur"])
                          else:
                              A_(lambda e: e.activation(out=junk[:], in_=xo[xb_][:], func=AF.Square, accum_out=st[xb_][:, 0:1]), [f"xo{xb_}"], ["junk", f"st{xb_}"])
                              A_(lambda e: e.activation(out=st[xb_][:, 1:2], in_=st[xb_][:, 0:1], func=AF.Sqrt, scale=1.0 / D, bias=epsc[:, 0:1]), [f"st{xb_}", "epsc"], [f"st{xb_}"])
                              V_(lambda e: e.reciprocal(st[xb_][:, 2:3], st[xb_][:, 1:2]), [f"st{xb_}"], [f"st{xb_}"])
                              V_(lambda e: e.scalar_tensor_tensor(out=xt[xb_][:], in0=xo[xb_][:], scalar=st[xb_][:, 2:3], in1=fnw[:], op0=ALU.mult, op1=ALU.mult),
                                 [f"xo{xb_}", f"st{xb_}", "fnw"], [f"xt{xb_}"])
                              LD(OUT[tt * 128:(tt + 1) * 128, :], xt[xb_][:], [f"xt{xb_}"], ["OUT"])
              P.barrier()
          except _Stop:
              break
        P.dead = False
        P.barrier()
        nops = P.nops
    return nc, nops


def make_in_maps(inputs, cores=range(8)):
    f = lambda a: np.ascontiguousarray(np.asarray(a, dtype=np.float32))
    consts = _consts()
    shared = {}
    shared["w_ada"] = f(inputs["w_ada"])
    shared["b_adaT"] = f(np.asarray(inputs["b_ada"]).reshape(2, 48, 128).transpose(0, 2, 1))
    shared["n1T"] = f(np.asarray(inputs["norm1_w"]).reshape(2, 8, 128).transpose(0, 2, 1))
    shared["n2T"] = f(np.asarray(inputs["norm2_w"]).reshape(2, 8, 128).transpose(0, 2, 1))
    shared["w_in"] = f(inputs["w_in"])
    qw = np.asarray(inputs["q_norm_w"]); kw = np.asarray(inputs["k_norm_w"])
    qkw = np.concatenate([np.tile(qw, (1, 8)), np.tile(kw, (1, 2))], 1)
    shared["qkw"] = f(np.broadcast_to(qkw[:, None, :], (2, 128, 1280)))
    cw = np.asarray(inputs["hy_conv_w"]).reshape(2, 3 * 1536)
    shared["convw"] = f(np.broadcast_to(cw[:, None, :], (2, 128, 3 * 1536)))
    shared["convb"] = f(np.broadcast_to(np.asarray(inputs["hy_conv_b"])[:, None, :], (2, 128, 1536)))
    shared["pe_w1"] = f(inputs["hy_pe_w1"]); shared["pe_w2"] = f(inputs["hy_pe_w2"]); shared["pe_w3"] = f(inputs["hy_pe_w3"])
    shared["pe_v"] = f(np.stack([np.asarray(inputs["hy_freq1"]), np.asarray(inputs["hy_pe_b1"]),
                                 np.asarray(inputs["hy_freq2"]), np.asarray(inputs["hy_pe_b2"])], -1))
    sk = np.asarray(inputs["hy_skip"]).reshape(2, 1024)
    shared["skipb"] = f(np.broadcast_to(sk[:, None, :], (2, 64, 1024)))
    shared["w_ap"] = f(inputs["w_att_proj"]); shared["w_hp"] = f(inputs["w_hy_proj"]); shared["w_out"] = f(inputs["w_out"])
    shared["router_w"] = f(inputs["router_w"])
    shared["router_bb"] = f(np.broadcast_to(np.asarray(inputs["router_b"])[None, :], (128, NE)))
    shared["exp_w1"] = f(inputs["exp_w1"]); shared["exp_w3"] = f(inputs["exp_w3"]); shared["exp_w2"] = f(inputs["exp_w2"])
    shared["fnw"] = f(np.broadcast_to(np.asarray(inputs["final_norm_w"])[None, :], (128, D)))
    for k, v in consts.items():
        shared["c_" + k] = f(v)
    x = np.asarray(inputs["x"]); c = np.asarray(inputs["c"]); ctx = np.asarray(inputs["ctx"]); c_ctx = np.asarray(inputs["c_ctx"])
    maps = []
    for b in cores:
        m = dict(shared)
        m["x"] = f(x[b]); m["ctx"] = f(ctx[b])
        cc = np.stack([c[b].reshape(8, 128).T, c_ctx.reshape(8, 128).T], -1)
        m["cc"] = f(cc)
        maps.append(m)
    return maps


_NC_CACHE = {}


def kernel(**inputs):
    if "nc" not in _NC_CACHE:
        _NC_CACHE["nc"] = build()[0]
    nc = _NC_CACHE["nc"]
    maps = make_in_maps(inputs)
    res = run_bass_kernel_spmd(nc, maps, core_ids=list(range(8)))
    out = np.stack([np.asarray(r["out"]) for r in res.results], 0).astype(np.float32)
    return out
```
